# Optimizing a Trainium2 kernel written in Bass

```python
import jax, jax.numpy as jnp
from jax import lax
import numpy as np

D_MODEL = 1024
BATCH = 8
SEQ = 4096
DEPTH = 2

N_MIXERS = 2
N_GLA = (DEPTH + 1) // 2
N_NSA = DEPTH // 2
NORM_EPS = 1e-6
ROPE_THETA = 10000.0
NEG = -1e30
BIG = 1e9

GLA_HEADS = 4
GLA_DK = D_MODEL // 2 // GLA_HEADS
GLA_DV = D_MODEL // GLA_HEADS
GLA_QK = GLA_HEADS * GLA_DK
GLA_V = GLA_HEADS * GLA_DV
GLA_GATE_RANK = 16
GLA_GATE_NORMALIZER = 16.0
GLA_CHUNK = 64
GLA_IN = 2 * GLA_QK + GLA_V + GLA_GATE_RANK + GLA_V

NSA_HEADS = 16
NSA_GROUPS = 4
NSA_HPG = NSA_HEADS // NSA_GROUPS
NSA_DH = D_MODEL // NSA_HEADS
NSA_Q = NSA_HEADS * NSA_DH
NSA_KV = NSA_GROUPS * NSA_DH
CMP_BLOCK = 32
CMP_STRIDE = 16
CMP_HIDDEN = 256
SEL_BLOCK = 64
SEL_TOP = 16
WINDOW = 512
Q_BLOCK = 64
NSA_IN = NSA_Q + 6 * NSA_KV + 3 * NSA_HEADS + NSA_Q

kernel_name = "hybrid_gla_nsa_sandwich"


def rms_norm(x, g):
    xf = x.astype(jnp.float32)
    y = xf * lax.rsqrt(jnp.mean(xf * xf, axis=-1, keepdims=True) + NORM_EPS)
    return (y * g.astype(jnp.float32)).astype(x.dtype)


def rope(x, positions):
    dh = x.shape[-1]
    inv = ROPE_THETA ** (-jnp.arange(0, dh, 2, dtype=jnp.float32) / dh)
    ang = positions.astype(jnp.float32)[..., None] * inv
    cos = jnp.cos(ang)[:, :, None, :]
    sin = jnp.sin(ang)[:, :, None, :]
    x1, x2 = jnp.split(x, 2, axis=-1)
    return jnp.concatenate([x1 * cos - x2 * sin, x2 * cos + x1 * sin], axis=-1)


def gla_chunked(q, k, v, g):
    B, H, S, dk = q.shape
    dv = v.shape[-1]
    C = GLA_CHUNK
    N = S // C
    q = q.reshape(B, H, N, C, dk)
    k = k.reshape(B, H, N, C, dk)
    v = v.reshape(B, H, N, C, dv)
    b = jnp.cumsum(g.reshape(B, H, N, C, dk), axis=3)
    b_last = b[:, :, :, -1:]
    q_e = q * jnp.exp(b)
    k_e = k * jnp.exp(-b)
    k_d = k * jnp.exp(b_last - b)
    causal = jnp.tril(jnp.ones((C, C), dtype=bool))
    a = jnp.where(causal, jnp.einsum('bhnid,bhnjd->bhnij', q_e, k_e), 0.0)
    o_intra = jnp.einsum('bhnij,bhnjv->bhniv', a, v)

    def step(state, xs):
        qe, kd, vc, dec = xs
        o = jnp.einsum('bhcd,bhdv->bhcv', qe, state)
        state = dec[..., None] * state + jnp.einsum('bhcd,bhcv->bhdv', kd, vc)
        return state, o

    xs = (jnp.moveaxis(q_e, 2, 0), jnp.moveaxis(k_d, 2, 0), jnp.moveaxis(v, 2, 0),
          jnp.moveaxis(jnp.exp(b_last[:, :, :, 0]), 2, 0))
    _, o_inter = lax.scan(step, jnp.zeros((B, H, dk, dv), jnp.float32), xs)
    return (o_intra + jnp.moveaxis(o_inter, 0, 2)).reshape(B, H, S, dv)


def gla_mixer(h, w_in, w_gk_up, b_gk, head_norm, w_out):
    B, S, _ = h.shape
    p = (h @ w_in).astype(jnp.float32)
    q, k, v, glr, z = jnp.split(p, [GLA_QK, 2 * GLA_QK, 2 * GLA_QK + GLA_V,
                                    2 * GLA_QK + GLA_V + GLA_GATE_RANK], axis=-1)
    gk = jax.nn.log_sigmoid(glr @ w_gk_up.astype(jnp.float32) + b_gk.astype(jnp.float32)) / GLA_GATE_NORMALIZER

    def heads(t, d):
        return t.reshape(B, S, GLA_HEADS, d).transpose(0, 2, 1, 3)

    o = gla_chunked(heads(q, GLA_DK) * GLA_DK ** -0.5, heads(k, GLA_DK),
                    heads(v, GLA_DV), heads(gk, GLA_DK))
    o = rms_norm(o, head_norm)
    o = o.transpose(0, 2, 1, 3).reshape(B, S, GLA_V) * jax.nn.silu(z)
    return o @ w_out.astype(jnp.float32)


def compress(t, pe, w1, w2):
    B, S, G, DH = t.shape
    r = CMP_BLOCK // CMP_STRIDE
    nc = (S - CMP_BLOCK) // CMP_STRIDE + 1
    pieces = t.reshape(B, S // CMP_STRIDE, CMP_STRIDE, G, DH)
    blocks = jnp.concatenate([pieces[:, i:i + nc] for i in range(r)], axis=2)
    blocks = blocks + pe.astype(jnp.float32)[None, None, :, None, :]
    flat = blocks.transpose(0, 3, 1, 2, 4).reshape(B, G, nc, CMP_BLOCK * DH)
    return jax.nn.silu(flat @ w1.astype(jnp.float32)) @ w2.astype(jnp.float32)


def nsa_mixer(h, positions, w_in, b_gate, pe_k, pe_v, ck_w1, ck_w2, cv_w1, cv_w2, w_out):
    B, S, _ = h.shape
    G, HPG, DH = NSA_GROUPS, NSA_HPG, NSA_DH
    p = (h @ w_in).astype(jnp.float32)
    splits = [NSA_Q + i * NSA_KV for i in range(7)] + [NSA_Q + 6 * NSA_KV + 3 * NSA_HEADS]
    q, kc, vc, ks, vs, kw, vw, gl, z = jnp.split(p, splits, axis=-1)

    q = rope(q.reshape(B, S, NSA_HEADS, DH), positions) * DH ** -0.5
    kc = rope(kc.reshape(B, S, G, DH), positions)
    ks = rope(ks.reshape(B, S, G, DH), positions)
    kw = rope(kw.reshape(B, S, G, DH), positions)
    vc = vc.reshape(B, S, G, DH)
    vs = vs.reshape(B, S, G, DH)
    vw = vw.reshape(B, S, G, DH)

    kcmp = compress(kc, pe_k, ck_w1, ck_w2)
    vcmp = compress(vc, pe_v, cv_w1, cv_w2)
    nc = kcmp.shape[2]
    ns = S // SEL_BLOCK
    nq = S // Q_BLOCK
    n_top = min(SEL_TOP, ns)

    ksb = ks.reshape(B, ns, SEL_BLOCK, G, DH).transpose(0, 3, 1, 2, 4)
    vsb = vs.reshape(B, ns, SEL_BLOCK, G, DH).transpose(0, 3, 1, 2, 4)
    pad = ((0, 0), (0, 0), (WINDOW, 0), (0, 0))
    kwp = jnp.pad(kw.transpose(0, 2, 1, 3), pad)
    vwp = jnp.pad(vw.transpose(0, 2, 1, 3), pad)

    cmp_start = jnp.arange(nc) * CMP_STRIDE
    cmp_end = cmp_start + CMP_BLOCK - 1
    sel_start = jnp.arange(ns) * SEL_BLOCK
    overlap = ((cmp_start[:, None] < sel_start[None, :] + SEL_BLOCK) &
               (cmp_start[:, None] + CMP_BLOCK > sel_start[None, :])).astype(jnp.float32)
    jb = jnp.arange(ns)
    bidx = jnp.arange(B)[:, None, None, None]
    gidx = jnp.arange(G)[None, :, None, None]

    qblocks = q.reshape(B, nq, Q_BLOCK, G, HPG, DH).transpose(1, 0, 3, 4, 2, 5)

    def block(args):
        n, qb = args
        t = n * Q_BLOCK + jnp.arange(Q_BLOCK)
        s = jnp.einsum('bghtd,bgcd->bghtc', qb, kcmp)
        valid = cmp_end[None, :] <= t[:, None]
        pc = jax.nn.softmax(jnp.where(valid, s, NEG), axis=-1)
        pc = jnp.where(jnp.any(valid, axis=-1)[:, None], pc, 0.0)
        o_c = jnp.einsum('bghtc,bgcd->bghtd', pc, vcmp)
        imp = jnp.einsum('bghtc,cj->bgtj', pc, overlap)
        cur = (t // SEL_BLOCK)[:, None]
        forced = (jb[None] == 0) | (jb[None] == cur) | (jb[None] == cur - 1)
        causal_blk = sel_start[None, :] <= t[:, None]
        imp = jnp.where(forced, BIG, jnp.where(causal_blk, imp, NEG))
        _, idx = lax.top_k(imp, n_top)
        k_sel = ksb[bidx, gidx, idx]
        v_sel = vsb[bidx, gidx, idx]
        pos = idx[..., None] * SEL_BLOCK + jnp.arange(SEL_BLOCK)
        smask = (pos <= t[:, None, None])[:, :, None]
        s = jnp.where(smask, jnp.einsum('bghtd,bgtksd->bghtks', qb, k_sel), NEG)
        ps = jax.nn.softmax(s.reshape(B, G, HPG, Q_BLOCK, n_top * SEL_BLOCK), axis=-1).reshape(s.shape)
        o_s = jnp.einsum('bghtks,bgtksd->bghtd', ps, v_sel)
        kwin = lax.dynamic_slice_in_dim(kwp, n * Q_BLOCK, WINDOW + Q_BLOCK, axis=2)
        vwin = lax.dynamic_slice_in_dim(vwp, n * Q_BLOCK, WINDOW + Q_BLOCK, axis=2)
        wpos = n * Q_BLOCK - WINDOW + jnp.arange(WINDOW + Q_BLOCK)
        dlt = t[:, None] - wpos[None, :]
        wmask = (dlt >= 0) & (dlt < WINDOW) & (wpos[None, :] >= 0)
        s = jnp.where(wmask, jnp.einsum('bghtd,bgsd->bghts', qb, kwin), NEG)
        o_w = jnp.einsum('bghts,bgsd->bghtd', jax.nn.softmax(s, axis=-1), vwin)
        return jnp.stack([o_c, o_s, o_w], axis=0)

    out = lax.map(block, (jnp.arange(nq), qblocks))
    out = out.transpose(2, 0, 5, 3, 4, 1, 6).reshape(B, S, NSA_HEADS, 3, DH)
    gates = jax.nn.sigmoid(gl + b_gate.astype(jnp.float32)).reshape(B, S, NSA_HEADS, 3)
    o = jnp.einsum('bshc,bshcd->bshd', gates, out).reshape(B, S, NSA_Q) * jax.nn.silu(z)
    return o @ w_out.astype(jnp.float32)


def setup_inputs(seed: int = 0) -> dict:
    key = jax.random.key(seed)
    ks = jax.random.split(key, 20)
    nrm = jax.random.normal
    f32 = jnp.float32
    return {
        "x": nrm(ks[0], (BATCH, SEQ, D_MODEL), f32),
        "positions": jnp.broadcast_to(jnp.arange(SEQ, dtype=jnp.int32), (BATCH, SEQ)),
        "pre_norm": 1.0 + 0.05 * nrm(ks[1], (DEPTH, D_MODEL), f32),
        "post_norm": 1.0 + 0.05 * nrm(ks[2], (DEPTH, D_MODEL), f32),
        "gla_w_in": nrm(ks[3], (N_GLA, D_MODEL, GLA_IN), f32) * D_MODEL ** -0.5,
        "gla_w_gk_up": nrm(ks[4], (N_GLA, GLA_GATE_RANK, GLA_QK), f32) * GLA_GATE_RANK ** -0.5,
        "gla_b_gk": 0.02 * nrm(ks[5], (N_GLA, GLA_QK), f32),
        "gla_head_norm": 1.0 + 0.05 * nrm(ks[6], (N_GLA, GLA_DV), f32),
        "gla_w_out": nrm(ks[7], (N_GLA, GLA_V, D_MODEL), f32) * GLA_V ** -0.5,
        "nsa_w_in": nrm(ks[8], (N_NSA, D_MODEL, NSA_IN), f32) * D_MODEL ** -0.5,
        "nsa_b_gate": 0.02 * nrm(ks[9], (N_NSA, 3 * NSA_HEADS), f32),
        "nsa_pe_k": 0.02 * nrm(ks[10], (N_NSA, CMP_BLOCK, NSA_DH), f32),
        "nsa_pe_v": 0.02 * nrm(ks[11], (N_NSA, CMP_BLOCK, NSA_DH), f32),
        "nsa_ck_w1": nrm(ks[12], (N_NSA, CMP_BLOCK * NSA_DH, CMP_HIDDEN), f32) * (CMP_BLOCK * NSA_DH) ** -0.5,
        "nsa_ck_w2": nrm(ks[13], (N_NSA, CMP_HIDDEN, NSA_DH), f32) * CMP_HIDDEN ** -0.5,
        "nsa_cv_w1": nrm(ks[14], (N_NSA, CMP_BLOCK * NSA_DH, CMP_HIDDEN), f32) * (CMP_BLOCK * NSA_DH) ** -0.5,
        "nsa_cv_w2": nrm(ks[15], (N_NSA, CMP_HIDDEN, NSA_DH), f32) * CMP_HIDDEN ** -0.5,
        "nsa_w_out": nrm(ks[16], (N_NSA, NSA_Q, D_MODEL), f32) * NSA_Q ** -0.5,
    }


def reference(x, positions, pre_norm, post_norm, gla_w_in, gla_w_gk_up, gla_b_gk, gla_head_norm,
              gla_w_out, nsa_w_in, nsa_b_gate, nsa_pe_k, nsa_pe_v, nsa_ck_w1, nsa_ck_w2,
              nsa_cv_w1, nsa_cv_w2, nsa_w_out):
    for i in range(DEPTH):
        h = rms_norm(x, pre_norm[i])
        j = i // N_MIXERS
        if i % N_MIXERS == 0:
            y = gla_mixer(h, gla_w_in[j], gla_w_gk_up[j], gla_b_gk[j], gla_head_norm[j], gla_w_out[j])
        else:
            y = nsa_mixer(h, positions, nsa_w_in[j], nsa_b_gate[j], nsa_pe_k[j], nsa_pe_v[j],
                          nsa_ck_w1[j], nsa_ck_w2[j], nsa_cv_w1[j], nsa_cv_w2[j], nsa_w_out[j])
        x = x + rms_norm(y.astype(x.dtype), post_norm[i])
    return x
```

```python
import math
from contextlib import ExitStack
import numpy as np
import concourse.bass as bass
import concourse.mybir as mybir
from concourse.bass_utils import run_bass_kernel_spmd

F32 = mybir.dt.float32
BF16 = mybir.dt.bfloat16
I32 = mybir.dt.int32
AF = mybir.ActivationFunctionType
ALU = mybir.AluOpType
AX = mybir.AxisListType

ENGS = ("tensor", "vector", "scalar", "gpsimd", "sync")
OUT_KEYS = ("out", "accum_out")


class Buf:
    def __init__(self, h, name):
        self.h = h
        self.name = name
        self.w = None
        self.r = []
        self.sem = None
        self.semcnt = 0

    def __getitem__(self, idx):
        return V(self, self.h[idx])


class V:
    def __init__(self, buf, ap):
        self.buf = buf
        self.ap = ap

    def __getitem__(self, idx):
        return V(self.buf, self.ap[idx])

    def rearrange(self, *a, **k):
        return V(self.buf, self.ap.rearrange(*a, **k))

    def bitcast(self, dt):
        return V(self.buf, self.ap.bitcast(dt))

    def to_broadcast(self, shape):
        return V(self.buf, self.ap.to_broadcast(shape))


class Op:
    __slots__ = ("eng", "fn", "deps", "signal", "idx", "dma_sem", "dma_cnt", "semval")

    def __init__(self, eng, fn):
        self.eng = eng
        self.fn = fn
        self.deps = []
        self.signal = False
        self.dma_sem = None
        self.dma_cnt = 0
        self.semval = 0


class Prog:
    def __init__(self, nc):
        self.nc = nc
        self.esem = {e: nc.alloc_semaphore(f"s_{e}") for e in ENGS}
        self.ecount = {e: 0 for e in ENGS}
        self.ops = {e: [] for e in ENGS}
        self.nbuf = 0
        self.dma_sems = []
        self.all_bufs = []

    def sb(self, stack, name, shape, dtype):
        h = stack.enter_context(self.nc.sbuf_tensor(name, list(shape), dtype))
        b = Buf(h, name)
        self.all_bufs.append(b)
        return b

    def ps(self, stack, name, shape, dtype=F32):
        h = stack.enter_context(self.nc.psum_tensor(name, list(shape), dtype))
        b = Buf(h, name)
        b.psum = True
        self.all_bufs.append(b)
        return b

    def track(self, h, name):
        b = Buf(h, name)
        self.all_bufs.append(b)
        return b

    def _collect(self, kw):
        reads, writes, real = [], [], {}
        for k, v in kw.items():
            if isinstance(v, V):
                (writes if k in OUT_KEYS else reads).append(v.buf)
                real[k] = v.ap
            else:
                real[k] = v
        return reads, writes, real

    def _add(self, eng, fn, reads, writes, is_dma=False, dma_buf=None, extra_reads=(), extra_writes=()):
        self.nops = getattr(self, "nops", 0) + 1
        if self.nops > getattr(self, "maxops", 1 << 60):
            return None
        op = Op(eng, fn)
        reads = list(reads) + list(extra_reads)
        writes = list(writes) + list(extra_writes)
        deps = []
        for b in reads:
            if b.w is not None:
                deps.append(("raw", b.w))
            if getattr(b, "psum", False):
                for t in b.r:
                    if t[0] == "c" and t[1] != eng:
                        deps.append(("rar", t))
        for b in writes:
            if b.w is not None:
                deps.append(("waw", b.w))
            for t in b.r:
                deps.append(("war", t))
        opidx = len(self.ops[eng])
        keep = []
        for kind, t in deps:
            if t[0] == "c":
                if t[1] == eng and not is_dma:
                    if kind != "raw" or eng == "tensor":
                        continue
            keep.append(t)
        op.deps = keep
        self.ops[eng].append(op)
        if is_dma:
            b = dma_buf
            if b.sem is None:
                b.sem = self.nc.alloc_semaphore(f"d_{b.name}")
                self.dma_sems.append(b)
            b.semcnt += 16
            op.dma_sem = b.sem
            ticket = ("d", b, b.semcnt)
        else:
            ticket = ("c", eng, opidx)
        for b in reads:
            b.r.append(ticket)
        for b in writes:
            b.w = ticket
            b.r = []
        return op

    def do(self, eng, name, r=(), w=(), **kw):
        reads, writes, real = self._collect(kw)
        self.last_desc = (eng, name)
        fn = lambda e, name=name, real=real: getattr(e, name)(**real)
        return self._add(eng, fn, reads, writes, extra_reads=r, extra_writes=w)

    def dma(self, eng, out, in_, **kw):
        reads, writes = [], []
        oa, ia = out, in_
        if isinstance(out, V):
            writes.append(out.buf)
            oa = out.ap
        if isinstance(in_, V):
            reads.append(in_.buf)
            ia = in_.ap
        dbuf = writes[0] if writes else reads[0]
        fn = lambda e, oa=oa, ia=ia, kw=kw: e.dma_start(out=oa, in_=ia, **kw)
        return self._add(eng, fn, reads, writes, is_dma=True, dma_buf=dbuf)

    def flush(self, final_wait_bufs=()):
        nc = self.nc
        for e in ENGS:
            for op in self.ops[e]:
                for t in op.deps:
                    if t[0] == "c":
                        self.ops[t[1]][t[2]].signal = True
        for e in ENGS:
            for op in reversed(self.ops[e]):
                if op.dma_sem is None:
                    op.signal = True
                    break
        for e in ENGS:
            c = self.ecount[e]
            for op in self.ops[e]:
                if op.signal:
                    c += 1
                op.semval = c
            self.ecount[e] = c
        ops = self.ops
        esem = self.esem
        ecount = dict(self.ecount)
        dsems = list(self.dma_sems)

        def emit(e):
            def body(eng):
                waited = {}
                for op in ops[e]:
                    need = {}
                    for t in op.deps:
                        if t[0] == "c":
                            key = ("c", t[1])
                            val = ops[t[1]][t[2]].semval
                            sem = esem[t[1]]
                        else:
                            key = ("d", id(t[1]))
                            val = t[2]
                            sem = t[1].sem
                        if waited.get(key, -1) >= val:
                            continue
                        if key not in need or need[key][1] < val:
                            need[key] = (sem, val)
                    for key, (sem, val) in need.items():
                        eng.wait_ge(sem, val)
                        waited[key] = val
                    ins = op.fn(eng)
                    if op.dma_sem is not None:
                        ins.then_inc(op.dma_sem, 16)
                    elif op.signal:
                        ins.then_inc(esem[e], 1)
                for e2 in ENGS:
                    if e2 != e and ecount[e2] > 0 and waited.get(("c", e2), -1) < ecount[e2]:
                        eng.wait_ge(esem[e2], ecount[e2])
                for b in dsems:
                    if waited.get(("d", id(b)), -1) < b.semcnt:
                        eng.wait_ge(b.sem, b.semcnt)
            return body

        with nc.Block() as blk:
            for e in ENGS:
                getattr(blk, e)(emit(e))
        self.ops = {e: [] for e in ENGS}
        for b in self.all_bufs:
            b.w = None
            b.r = []


S = 4096
D = 1024
NT = S // 128
EPS = 1e-6


def make_ident(P, st, pfx="g"):
    identf = P.sb(st, pfx + "_identf", [128, 128], F32)
    ident = P.sb(st, pfx + "_ident", [128, 128], BF16)
    P.do("gpsimd", "memset", ap=identf[:], constant=1.0, w=[identf])
    P.do("gpsimd", "affine_select", out=identf[:], in_=identf[:], pattern=[[-1, 128]],
         compare_op=ALU.is_equal, fill=0.0, base=0, channel_multiplier=1)
    P.do("vector", "tensor_copy", out=ident[:], in_=identf[:])
    return ident, identf


def gla_layer(P, nc, x_in, x_out, pre_g, post_g, w_in, w_up, b_gk, hnorm, w_out, ntiles=NT):
    with ExitStack() as st:
        ident, identf = make_ident(P, st)
        Win = P.sb(st, "g_Win", [128, 8, 3088], BF16)
        Wout = P.sb(st, "g_Wout", [128, 8, 1024], BF16)
        stage = [P.sb(st, f"g_stage{i}", [128, 3088], F32) for i in range(2)]
        gpre = P.sb(st, "g_gpre", [128, 8], F32)
        gpost = P.sb(st, "g_gpost", [128, 1024], F32)
        wupf = P.sb(st, "g_wupf", [16, 512], F32)
        wup = P.sb(st, "g_wup", [16, 512], BF16)
        negb = P.sb(st, "g_negb", [128, 4], F32)
        hn = P.sb(st, "g_hn", [128, 2], F32)
        onesb = P.sb(st, "g_ones", [128, 128], BF16)
        rmask = P.sb(st, "g_rmask", [128, 512], F32)
        bdmask = P.sb(st, "g_bdmask", [128, 512], F32)
        one1 = P.sb(st, "g_one1", [128, 1], F32)

        P.dma("sync", gpre[:], pre_g.rearrange("(c p o) -> p c o", p=128, o=1), allow_slow_non_contiguous=True)
        P.dma("sync", negb[:], b_gk.rearrange("(c p o) -> p c o", p=128, o=1), allow_slow_non_contiguous=True)
        P.dma("sync", hn[:], hnorm.rearrange("(c p o) -> p c o", p=128, o=1), allow_slow_non_contiguous=True)
        P.dma("sync", wupf[:], w_up)
        P.dma("sync", gpost[:], post_g.rearrange("(o n) -> o n", o=1).to_broadcast([128, 1024]))
        P.do("vector", "tensor_scalar", out=negb[:], in0=negb[:], scalar1=-1.0, scalar2=None, op0=ALU.mult)
        P.do("vector", "tensor_copy", out=wup[:], in_=wupf[:])
        P.do("gpsimd", "memset", ap=onesb[:], constant=1.0, w=[onesb])
        P.do("gpsimd", "memset", ap=one1[:], constant=1.0, w=[one1])
        P.do("gpsimd", "memset", ap=rmask[:], constant=1.0, w=[rmask])
        P.do("gpsimd", "memset", ap=rmask[:].rearrange("p (a b) -> p a b", b=64)[:, :, 0:1], constant=0.0, w=[rmask])
        P.do("gpsimd", "memset", ap=bdmask[:], constant=1.0, w=[bdmask])
        for hh in range(4):
            sl = bdmask[:, hh * 128:(hh + 1) * 128]
            P.do("gpsimd", "affine_select", out=sl, in_=sl, pattern=[[1, 128]], compare_op=ALU.is_ge,
                 fill=0.0, base=0, channel_multiplier=-1)
            sl2 = bdmask[64:128, hh * 128:hh * 128 + 64]
            P.do("gpsimd", "memset", ap=sl2, constant=0.0, w=[bdmask])
            sl3 = bdmask[0:64, hh * 128 + 64:(hh + 1) * 128]
            P.do("gpsimd", "memset", ap=sl3, constant=0.0, w=[bdmask])
        for c in range(8):
            sg = stage[c % 2]
            P.dma("sync" if c % 2 == 0 else "gpsimd", sg[:], w_in[c * 128:(c + 1) * 128, :])
            P.do("vector" if c % 2 == 0 else "gpsimd", "tensor_scalar", out=Win[:, c, :], in0=sg[:],
                 scalar1=gpre[:, c:c + 1], scalar2=None, op0=ALU.mult)
        for c in range(8):
            sg = stage[c % 2]
            P.dma("sync" if c % 2 == 0 else "gpsimd", sg[:, 0:1024], w_out[c * 128:(c + 1) * 128, :])
            P.do("vector" if c % 2 == 0 else "gpsimd", "tensor_copy", out=Wout[:, c, :], in_=sg[:, 0:1024])

        xt = [P.sb(st, f"g_xt{i}", [128, 1024], F32) for i in range(2)]
        xo = [P.sb(st, f"g_xo{i}", [128, 1024], F32) for i in range(2)]
        sq = P.sb(st, "g_sq", [128, 1024], F32)
        ss = P.sb(st, "g_ss", [128, 4], F32)
        rstd = P.sb(st, "g_rstd", [128, 2], F32)
        hb = P.sb(st, "g_hb", [128, 1024], BF16)
        hT = P.sb(st, "g_hT", [128, 1024], BF16)
        glrT = P.sb(st, "g_glrT", [16, 128], BF16)
        e1 = P.sb(st, "g_e1", [128, 512], F32)
        yv = P.sb(st, "g_yv", [128, 512], F32)
        Bc = P.sb(st, "g_Bc", [128, 512], F32)
        eb = P.sb(st, "g_eb", [128, 512], F32)
        enb = P.sb(st, "g_enb", [128, 512], F32)
        qeT = P.sb(st, "g_qeT", [128, 512], BF16)
        keT = P.sb(st, "g_keT", [128, 512], BF16)
        kdT = P.sb(st, "g_kdT", [128, 512], BF16)
        kdz = [P.sb(st, f"g_kd{i}", [128, 512], BF16) for i in range(2)]
        for i in range(2):
            P.do("gpsimd", "memset", ap=kdz[i][:], constant=0.0, w=[kdz[i]])
        zs = P.sb(st, "g_zs", [128, 1024], BF16)
        vb = P.sb(st, "g_vb", [128, 1024], BF16)
        ATm = P.sb(st, "g_ATm", [128, 512], BF16)
        oT = P.sb(st, "g_oT", [128, 1024], F32)
        osq = P.sb(st, "g_osq", [128, 1024], BF16)
        rs = P.sb(st, "g_rs", [128, 512], F32)
        tmpo = P.sb(st, "g_tmpo", [128, 1024], F32)
        ogT = P.sb(st, "g_ogT", [128, 1024], BF16)
        S32 = [P.sb(st, f"g_S32_{h}", [128, 256], F32) for h in range(4)]
        Sbf = [[P.sb(st, f"g_Sbf_{h}_{i}", [128, 256], BF16) for i in range(2)] for h in range(4)]
        t1 = P.sb(st, "g_t1", [128, 1024], F32)

        bank = [P.ps(st, f"g_bank{i}", [128, 512], F32) for i in range(7)]
        bS = P.ps(st, "g_bankS", [128, 512], F32)
        pS = [bS[:, i * 256:(i + 1) * 256] for i in range(2)]
        bA, bQ, bK, bZ0, bZ1, bV0, bV1 = bank
        bG = bA
        pTb = bA[:].bitcast(BF16)

        sidx = [0, 0, 0, 0]
        nupd = 0
        for t in range(ntiles):
            x_t = xt[t % 2]
            P.dma("sync", x_t[:], x_in[t * 128:(t + 1) * 128, :])
            P.do("scalar", "activation", out=sq[:], in_=x_t[:], func=AF.Square, accum_out=ss[:, 0:1])
            P.do("vector", "tensor_scalar", out=rstd[:, 0:1], in0=ss[:, 0:1], scalar1=1.0 / D, scalar2=EPS,
                 op0=ALU.mult, op1=ALU.add)
            P.do("scalar", "activation", out=rstd[:, 0:1], in_=rstd[:, 0:1], func=AF.Sqrt)
            P.do("vector", "reciprocal", out=rstd[:, 0:1], in_=rstd[:, 0:1])
            P.do("vector", "tensor_scalar", out=hb[:], in0=x_t[:], scalar1=rstd[:, 0:1], scalar2=None, op0=ALU.mult)
            for c in range(8):
                P.do("tensor", "transpose", out=pTb[:, c * 128:(c + 1) * 128], in_=hb[:, c * 128:(c + 1) * 128],
                     identity=ident[:])
            P.do("vector", "tensor_copy", out=hT[:], in_=pTb)

            def hTc(c):
                return hT[:, c * 128:(c + 1) * 128]

            for c in range(8):
                P.do("tensor", "matmul", out=bA[0:16, 0:128], lhsT=Win[:, c, 2048:2064], rhs=hTc(c),
                     start=(c == 0), stop=(c == 7))
            P.do("scalar", "copy", out=glrT[:], in_=bA[0:16, 0:128])
            for hh in range(4):
                for c in range(8):
                    P.do("tensor", "matmul", out=bQ[:, hh * 128:(hh + 1) * 128], lhsT=Win[:, c, hh * 128:(hh + 1) * 128],
                         rhs=hTc(c), start=(c == 0), stop=(c == 7))
            for hh in range(4):
                for c in range(8):
                    P.do("tensor", "matmul", out=bK[:, hh * 128:(hh + 1) * 128],
                         lhsT=Win[:, c, 512 + hh * 128:512 + (hh + 1) * 128], rhs=hTc(c), start=(c == 0), stop=(c == 7))
            for hh in range(4):
                P.do("tensor", "matmul", out=bA[:, hh * 128:(hh + 1) * 128], lhsT=wup[:, hh * 128:(hh + 1) * 128],
                     rhs=glrT[:], start=True, stop=True)
            for zc in range(8):
                bz = bZ0 if zc < 4 else bZ1
                for c in range(8):
                    P.do("tensor", "matmul", out=bz[:, (zc % 4) * 128:(zc % 4 + 1) * 128],
                         lhsT=Win[:, c, 2064 + zc * 128:2064 + (zc + 1) * 128], rhs=hTc(c), start=(c == 0), stop=(c == 7))
            for i, bv in enumerate((bV0, bV1)):
                for c in range(8):
                    P.do("tensor", "matmul", out=bv[:], lhsT=hTc(c), rhs=Win[:, c, 1024 + i * 512:1024 + (i + 1) * 512],
                         start=(c == 0), stop=(c == 7))
            for hh in range(4):
                P.do("scalar", "activation", out=e1[:, hh * 128:(hh + 1) * 128], in_=bA[:, hh * 128:(hh + 1) * 128],
                     func=AF.Exp, scale=-1.0, bias=negb[:, hh:hh + 1])
            P.do("scalar", "activation", out=yv[:], in_=e1[:], func=AF.Ln, bias=one1[:, 0:1], scale=1.0)
            P.do("vector", "tensor_tensor_scan", out=Bc[:], data0=rmask[:], data1=yv[:], initial=0.0,
                 op0=ALU.mult, op1=ALU.add)
            P.do("scalar", "activation", out=eb[:], in_=Bc[:], func=AF.Exp, scale=-1.0 / 16.0)
            P.do("scalar", "activation", out=enb[:], in_=Bc[:], func=AF.Exp, scale=1.0 / 16.0)
            P.do("vector", "scalar_tensor_tensor", out=qeT[:], in0=bQ[:], scalar=128 ** -0.5, in1=eb[:],
                 op0=ALU.mult, op1=ALU.mult)
            P.do("vector", "tensor_tensor", out=keT[:], in0=bK[:], in1=enb[:], op=ALU.mult)
            for hh in range(4):
                for cc in range(2):
                    lo = hh * 128 + cc * 64
                    P.do("vector", "scalar_tensor_tensor", out=kdT[:, lo:lo + 64], in0=bK[:, lo:lo + 64],
                         scalar=eb[:, lo + 63:lo + 64], in1=enb[:, lo:lo + 64], op0=ALU.mult, op1=ALU.mult)
            P.do("scalar", "activation", out=zs[:, 0:512], in_=bZ0[:], func=AF.Silu)
            P.do("scalar", "activation", out=zs[:, 512:1024], in_=bZ1[:], func=AF.Silu)
            P.do("gpsimd" if False else "vector", "tensor_copy", out=vb[:, 0:512], in_=bV0[:])
            P.do("scalar", "copy", out=vb[:, 512:1024], in_=bV1[:])
            for hh in range(4):
                P.do("tensor", "transpose", out=pTb[:, hh * 128:(hh + 1) * 128], in_=kdT[:, hh * 128:(hh + 1) * 128],
                     identity=ident[:])
            P.do("vector", "tensor_copy", out=kdz[0][0:64, :], in_=pTb[0:64, 0:512])
            P.do("vector", "tensor_copy", out=kdz[1][64:128, :], in_=pTb[64:128, 0:512])
            for hh in range(4):
                P.do("tensor", "matmul", out=bQ[:, hh * 128:(hh + 1) * 128], lhsT=keT[:, hh * 128:(hh + 1) * 128],
                     rhs=qeT[:, hh * 128:(hh + 1) * 128], start=True, stop=True)
            P.do("vector", "tensor_tensor", out=ATm[:], in0=bQ[:], in1=bdmask[:], op=ALU.mult)
            bO = (bV0, bV1)
            for hh in range(4):
                for cc in range(2):
                    first = (t == 0 and cc == 0)
                    scur = Sbf[hh][sidx[hh]]
                    for vc in range(2):
                        idx = hh * 2 + vc
                        out = bO[idx // 4][:, (idx % 4) * 128 + cc * 64:(idx % 4) * 128 + cc * 64 + 64]
                        P.do("tensor", "matmul", out=out,
                             lhsT=vb[:, hh * 256 + vc * 128:hh * 256 + (vc + 1) * 128],
                             rhs=ATm[:, hh * 128 + cc * 64:hh * 128 + cc * 64 + 64],
                             start=True, stop=first)
                        if not first:
                            P.do("tensor", "matmul", out=out, lhsT=scur[:, vc * 128:(vc + 1) * 128],
                                 rhs=qeT[:, hh * 128 + cc * 64:hh * 128 + cc * 64 + 64], start=False, stop=True)
                    ps = pS[nupd % 2]
                    nupd += 1
                    P.do("tensor", "matmul", out=ps, lhsT=kdz[cc][:, hh * 128:(hh + 1) * 128],
                         rhs=vb[:, hh * 256:(hh + 1) * 256], start=True, stop=True)
                    if first:
                        P.do("vector", "tensor_copy", out=S32[hh][:], in_=ps)
                    else:
                        lo = hh * 128 + cc * 64
                        P.do("vector", "scalar_tensor_tensor", out=S32[hh][:], in0=S32[hh][:],
                             scalar=eb[:, lo + 63:lo + 64], in1=ps, op0=ALU.mult, op1=ALU.add)
                    sidx[hh] ^= 1
                    P.do("gpsimd", "tensor_copy", out=Sbf[hh][sidx[hh]][:], in_=S32[hh][:])
            P.do("scalar", "copy", out=oT[:, 0:512], in_=bV0[:])
            P.do("scalar", "copy", out=oT[:, 512:1024], in_=bV1[:])
            P.do("scalar", "activation", out=osq[:, 0:512], in_=bV0[:], func=AF.Square)
            P.do("scalar", "activation", out=osq[:, 512:1024], in_=bV1[:], func=AF.Square)
            for hh in range(4):
                for vc in range(2):
                    idx = hh * 2 + vc
                    P.do("tensor", "matmul", out=bK[:, hh * 128:(hh + 1) * 128], lhsT=onesb[:],
                         rhs=osq[:, idx * 128:(idx + 1) * 128], start=(vc == 0), stop=(vc == 1))
            P.do("vector", "tensor_scalar", out=rs[:], in0=bK[:], scalar1=1.0 / 256, scalar2=EPS, op0=ALU.mult, op1=ALU.add)
            P.do("scalar", "activation", out=rs[:], in_=rs[:], func=AF.Sqrt)
            P.do("vector", "reciprocal", out=rs[:], in_=rs[:])
            for hh in range(4):
                for vc in range(2):
                    idx = hh * 2 + vc
                    sl = slice(idx * 128, (idx + 1) * 128)
                    P.do("vector", "scalar_tensor_tensor", out=tmpo[:, sl], in0=oT[:, sl], scalar=hn[:, vc:vc + 1],
                         in1=rs[:, hh * 128:(hh + 1) * 128], op0=ALU.mult, op1=ALU.mult)
            P.do("gpsimd", "tensor_tensor", out=ogT[:], in0=tmpo[:], in1=zs[:], op=ALU.mult)
            bY = (bZ0, bZ1)
            for i in range(2):
                for fc in range(8):
                    P.do("tensor", "matmul", out=bY[i][:], lhsT=ogT[:, fc * 128:(fc + 1) * 128],
                         rhs=Wout[:, fc, i * 512:(i + 1) * 512], start=(fc == 0), stop=(fc == 7))
            P.do("scalar", "activation", out=sq[:, 0:512], in_=bZ0[:], func=AF.Square, accum_out=ss[:, 1:2])
            P.do("scalar", "activation", out=sq[:, 512:1024], in_=bZ1[:], func=AF.Square, accum_out=ss[:, 2:3])
            P.do("vector", "tensor_tensor", out=ss[:, 3:4], in0=ss[:, 1:2], in1=ss[:, 2:3], op=ALU.add)
            P.do("vector", "tensor_scalar", out=rstd[:, 1:2], in0=ss[:, 3:4], scalar1=1.0 / D, scalar2=EPS,
                 op0=ALU.mult, op1=ALU.add)
            P.do("scalar", "activation", out=rstd[:, 1:2], in_=rstd[:, 1:2], func=AF.Sqrt)
            P.do("vector", "reciprocal", out=rstd[:, 1:2], in_=rstd[:, 1:2])
            for i in range(2):
                P.do("vector", "scalar_tensor_tensor", out=t1[:, i * 512:(i + 1) * 512], in0=bY[i][:],
                     scalar=rstd[:, 1:2], in1=gpost[:, i * 512:(i + 1) * 512], op0=ALU.mult, op1=ALU.mult)
            x_o = xo[t % 2]
            P.do("gpsimd", "tensor_tensor", out=x_o[:], in0=t1[:], in1=x_t[:], op=ALU.add)
            P.dma("sync", x_out[t * 128:(t + 1) * 128, :], x_o[:])
        P.flush()


S = 4096
D = 1024
NT = S // 128
EPS = 1e-6
NEGM = -30000.0
TWO_PI = 2.0 * math.pi


def _front(P, t, x_in, xt, sq, ss, rstd, hb, hT, pTb, ident):
    x_t = xt[t % 2]
    P.dma("sync", x_t[:], x_in[t * 128:(t + 1) * 128, :])
    P.do("scalar", "activation", out=sq[:], in_=x_t[:], func=AF.Square, accum_out=ss[:, 0:1])
    P.do("vector", "tensor_scalar", out=rstd[:, 0:1], in0=ss[:, 0:1], scalar1=1.0 / D, scalar2=EPS,
         op0=ALU.mult, op1=ALU.add)
    P.do("scalar", "activation", out=rstd[:, 0:1], in_=rstd[:, 0:1], func=AF.Sqrt)
    P.do("vector", "reciprocal", out=rstd[:, 0:1], in_=rstd[:, 0:1])
    P.do("vector", "tensor_scalar", out=hb[:], in0=x_t[:], scalar1=rstd[:, 0:1], scalar2=None, op0=ALU.mult)
    for c in range(8):
        P.do("tensor", "transpose", out=pTb[:, c * 128:(c + 1) * 128], in_=hb[:, c * 128:(c + 1) * 128],
             identity=ident[:])
    P.do("vector", "tensor_copy", out=hT[:], in_=pTb)
    return x_t


def _rope(P, src, nh, cosb, sinb, tmp, dst):
    s3 = src.rearrange("p (h d) -> p h d", d=64)
    d3 = dst.rearrange("p (h d) -> p h d", d=64)
    x1, x2 = s3[:, :, 0:32], s3[:, :, 32:64]
    cb = V(cosb.buf, cosb.ap.unsqueeze(1).to_broadcast([128, nh, 32]))
    sb_ = V(sinb.buf, sinb.ap.unsqueeze(1).to_broadcast([128, nh, 32]))
    ta = tmp[0][:, 0:nh * 32].rearrange("p (h d) -> p h d", d=32)
    tb = tmp[1][:, 0:nh * 32].rearrange("p (h d) -> p h d", d=32)
    P.do("vector", "tensor_tensor", out=ta, in0=x1, in1=cb, op=ALU.mult)
    P.do("vector", "tensor_tensor", out=tb, in0=x2, in1=sb_, op=ALU.mult)
    P.do("gpsimd", "tensor_tensor", out=d3[:, :, 0:32], in0=ta, in1=tb, op=ALU.subtract)
    tc_ = tmp[2][:, 0:nh * 32].rearrange("p (h d) -> p h d", d=32)
    td = tmp[3][:, 0:nh * 32].rearrange("p (h d) -> p h d", d=32)
    P.do("vector", "tensor_tensor", out=tc_, in0=x2, in1=cb, op=ALU.mult)
    P.do("vector", "tensor_tensor", out=td, in0=x1, in1=sb_, op=ALU.mult)
    P.do("gpsimd", "tensor_tensor", out=d3[:, :, 32:64], in0=tc_, in1=td, op=ALU.add)


def nsa_layer(P, nc, x_in, x_out, pos, pre_g, post_g, w_in, b_gate, pe_k, pe_v, ck_w1, ck_w2, cv_w1, cv_w2,
              w_out, ntiles=NT):
    nblk = 8 * ntiles - 1
    with ExitStack() as sta:
        ident, identf = make_ident(P, sta, "n")
        kcmpT = P.sb(sta, "n_kcmpT", [64, 4, 256], BF16)
        vcmp = P.sb(sta, "n_vcmp", [128, 2, 4, 129], BF16)
        costab = P.sb(sta, "n_cos", [128, NT, 32], F32)
        sintab = P.sb(sta, "n_sin", [128, NT, 32], F32)
        gpre = P.sb(sta, "n_gpre", [128, 8], F32)
        xt = [P.sb(sta, f"n_xt{i}", [128, 1024], F32) for i in range(2)]
        sq = P.sb(sta, "n_sq", [128, 1024], F32)
        ss = P.sb(sta, "n_ss", [128, 4], F32)
        rstd = P.sb(sta, "n_rstd", [128, 2], F32)
        hb = P.sb(sta, "n_hb", [128, 1024], BF16)
        hT = P.sb(sta, "n_hT", [128, 1024], BF16)
        rtmp = [P.sb(sta, f"n_rtmp{i}", [128, 512], F32) for i in range(4)]
        B = [P.ps(sta, f"n_bank{i}", [128, 512], F32) for i in range(8)]
        pTb = B[0][:].bitcast(BF16)

        P.dma("sync", gpre[:], pre_g.rearrange("(c p o) -> p c o", p=128, o=1), allow_slow_non_contiguous=True)

        with ExitStack() as stc:
            posi = P.sb(stc, "n_posi", [128, NT], I32)
            posf = P.sb(stc, "n_posf", [128, NT], F32)
            invf = P.sb(stc, "n_invf", [128, 32], F32)
            ang = P.sb(stc, "n_ang", [128, NT, 32], F32)
            kf = P.sb(stc, "n_kf", [128, NT, 32], F32)
            ki = P.sb(stc, "n_ki", [128, NT, 32], I32)
            mk = P.sb(stc, "n_mk", [128, NT, 32], F32)
            P.dma("sync", posi[:], pos.rearrange("(t p o) -> p t o", p=128, o=1), allow_slow_non_contiguous=True)
            P.do("vector", "tensor_copy", out=posf[:], in_=posi[:])
            P.do("gpsimd", "iota", out=invf[:], pattern=[[1, 32]], base=0, channel_multiplier=0,
                 allow_small_or_imprecise_dtypes=True)
            P.do("scalar", "activation", out=invf[:], in_=invf[:], func=AF.Exp, scale=-math.log(10000.0) / 32.0)
            for t in range(NT):
                P.do("vector", "tensor_scalar", out=ang[:, t, :], in0=invf[:], scalar1=posf[:, t:t + 1], scalar2=None,
                     op0=ALU.mult)

            def reduce_to_pi(dst, shift):
                P.do("vector", "tensor_scalar", out=kf[:], in0=ang[:], scalar1=shift, scalar2=1.0 / TWO_PI,
                     op0=ALU.add, op1=ALU.mult)
                P.do("vector", "tensor_copy", out=ki[:], in_=kf[:])
                P.do("vector", "tensor_copy", out=kf[:], in_=ki[:])
                P.do("vector", "scalar_tensor_tensor", out=kf[:], in0=kf[:], scalar=-TWO_PI, in1=ang[:],
                     op0=ALU.mult, op1=ALU.add)
                P.do("vector", "tensor_scalar", out=kf[:], in0=kf[:], scalar1=shift, scalar2=None, op0=ALU.add)
                P.do("vector", "tensor_scalar", out=mk[:], in0=kf[:], scalar1=math.pi, scalar2=-TWO_PI,
                     op0=ALU.is_gt, op1=ALU.mult)
                P.do("vector", "tensor_tensor", out=kf[:], in0=kf[:], in1=mk[:], op=ALU.add)
                P.do("vector", "tensor_scalar", out=mk[:], in0=kf[:], scalar1=-math.pi, scalar2=TWO_PI,
                     op0=ALU.is_lt, op1=ALU.mult)
                P.do("vector", "tensor_tensor", out=kf[:], in0=kf[:], in1=mk[:], op=ALU.add)
                P.do("vector", "tensor_scalar", out=kf[:], in0=kf[:], scalar1=math.pi, scalar2=-math.pi,
                     op0=ALU.min, op1=ALU.max)
                P.do("scalar", "activation", out=dst[:], in_=kf[:], func=AF.Sin)

            reduce_to_pi(sintab, 0.0)
            reduce_to_pi(costab, math.pi / 2.0)
            P.flush()

        with ExitStack() as st0:
            WinA = P.sb(st0, "n0_WinA", [128, 8, 512], BF16)
            stg = [P.sb(st0, f"n0_stg{i}", [128, 2048], F32) for i in range(2)]
            w1 = [P.sb(st0, f"n0_w1_{i}", [64, 32, 256], BF16) for i in range(2)]
            w2 = [P.sb(st0, f"n0_w2_{i}", [128, 2, 64], BF16) for i in range(2)]
            w2f = P.sb(st0, "n0_w2f", [128, 2, 64], F32)
            peTf = P.sb(st0, "n0_peTf", [64, 32], F32)
            peT = [P.sb(st0, f"n0_peT{i}", [64, 32], BF16) for i in range(2)]
            bias = P.sb(st0, "n0_bias", [128, 4], F32)
            kcT = P.sb(st0, "n0_kcT", [64, 4, S], BF16)
            vcT = P.sb(st0, "n0_vcT", [64, 4, S], BF16)
            kcr = P.sb(st0, "n0_kcr", [128, 256], BF16)
            vcb = P.sb(st0, "n0_vcb", [128, 256], BF16)
            hidT = P.sb(st0, "n0_hidT", [128, 2, 256], BF16)
            ovf = P.sb(st0, "n0_ovf", [128, 2, 64], F32)

            for c in range(8):
                sg = stg[c % 2]
                P.dma("sync" if c % 2 == 0 else "gpsimd", sg[:, 0:512], w_in[c * 128:(c + 1) * 128, 1024:1536])
                P.do("vector" if c % 2 == 0 else "gpsimd", "tensor_scalar", out=WinA[:, c, :], in0=sg[:, 0:512],
                     scalar1=gpre[:, c:c + 1], scalar2=None, op0=ALU.mult)
            n = 0
            for kv, (wsrc, w2src, pesrc) in enumerate(((ck_w1, ck_w2, pe_k), (cv_w1, cv_w2, pe_v))):
                w1v = wsrc.rearrange("(l d) n -> d l n", d=64)
                for q4 in range(4):
                    sg = stg[n % 2]
                    P.dma("sync" if n % 2 == 0 else "gpsimd", sg[0:64, :].rearrange("p (l n) -> p l n", n=256),
                          w1v[:, q4 * 8:(q4 + 1) * 8, :])
                    P.do("vector" if n % 2 == 0 else "gpsimd", "tensor_copy",
                         out=w1[kv][:, q4 * 8:(q4 + 1) * 8, :], in_=sg[0:64, :].rearrange("p (l n) -> p l n", n=256))
                    n += 1
                P.dma("sync", w2f[:], w2src.rearrange("(c p) n -> p c n", p=128))
                P.do("vector", "tensor_copy", out=w2[kv][:], in_=w2f[:])
                P.dma("sync", peTf[:], pesrc.rearrange("l d -> d l"), allow_slow_non_contiguous=True)
                P.do("vector", "tensor_copy", out=peT[kv][:], in_=peTf[:])
                for hc in range(2):
                    for l in range(32):
                        P.do("tensor", "matmul", out=B[1][:, kv * 2 + hc:kv * 2 + hc + 1],
                             lhsT=w1[kv][:, l, hc * 128:(hc + 1) * 128], rhs=peT[kv][:, l:l + 1],
                             start=(l == 0), stop=(l == 31))
            P.do("vector", "tensor_copy", out=bias[:], in_=B[1][:, 0:4])

            P.do("gpsimd", "memset", ap=vcmp[:], constant=0.0, w=[vcmp])
            P.do("gpsimd", "memset", ap=vcmp[:, :, :, 64:65], constant=1.0, w=[vcmp])
            P.do("gpsimd", "memset", ap=ovf[:], constant=1.0, w=[ovf])
            for ch in range(2):
                P.do("gpsimd", "affine_select", out=ovf[:, ch, :], in_=ovf[:, ch, :], pattern=[[-4, 64]],
                     compare_op=ALU.is_ge, fill=0.0, base=ch * 128 + 1, channel_multiplier=1)
                P.do("gpsimd", "affine_select", out=ovf[:, ch, :], in_=ovf[:, ch, :], pattern=[[4, 64]],
                     compare_op=ALU.is_ge, fill=0.0, base=3 - ch * 128, channel_multiplier=-1)
                for g in range(4):
                    P.do("vector", "tensor_copy", out=vcmp[:, ch, g, 65:129], in_=ovf[:, ch, :])

            for t in range(ntiles):
                _front(P, t, x_in, xt, sq, ss, rstd, hb, hT, pTb, ident)
                for c in range(8):
                    P.do("tensor", "matmul", out=B[1][:], lhsT=hT[:, c * 128:(c + 1) * 128], rhs=WinA[:, c, :],
                         start=(c == 0), stop=(c == 7))
                _rope(P, B[1][:, 0:256], 4, costab[:, t, :], sintab[:, t, :], rtmp, kcr[:])
                P.do("scalar", "copy", out=vcb[:], in_=B[1][:, 256:512])
                for g in range(4):
                    P.do("tensor", "transpose", out=pTb[0:64, g * 128:(g + 1) * 128], in_=kcr[:, g * 64:(g + 1) * 64],
                         identity=ident[:])
                for g in range(4):
                    P.do("tensor", "transpose", out=pTb[0:64, 512 + g * 128:512 + (g + 1) * 128],
                         in_=vcb[:, g * 64:(g + 1) * 64], identity=ident[:])
                P.do("vector", "tensor_copy", out=kcT[:, :, t * 128:(t + 1) * 128],
                     in_=pTb[0:64, 0:512].rearrange("p (g t) -> p g t", g=4))
                P.do("scalar", "copy", out=vcT[:, :, t * 128:(t + 1) * 128],
                     in_=pTb[0:64, 512:1024].rearrange("p (g t) -> p g t", g=4))

            for kv, srcT in enumerate((kcT, vcT)):
                for g in range(4):
                    for hc in range(2):
                        bk = B[2 + hc]
                        for l in range(32):
                            P.do("tensor", "matmul", out=bk[:, 0:nblk], lhsT=w1[kv][:, l, hc * 128:(hc + 1) * 128],
                                 rhs=srcT[:, g, l:l + 16 * (nblk - 1) + 1:16], start=(l == 0), stop=(l == 31))
                        P.do("scalar", "activation", out=hidT[:, hc, 0:nblk], in_=bk[:, 0:nblk], func=AF.Silu,
                             bias=bias[:, kv * 2 + hc:kv * 2 + hc + 1], scale=1.0)
                    if kv == 0:
                        for hc in range(2):
                            P.do("tensor", "matmul", out=B[4][0:64, 0:nblk], lhsT=w2[0][:, hc, :],
                                 rhs=hidT[:, hc, 0:nblk], start=(hc == 0), stop=(hc == 1))
                        P.do("vector", "tensor_copy", out=kcmpT[:, g, 0:nblk], in_=B[4][0:64, 0:nblk])
                    else:
                        for ch in range(2):
                            rows = min(128, nblk - ch * 128)
                            if rows <= 0:
                                continue
                            for hc in range(2):
                                P.do("tensor", "matmul", out=B[4][0:rows, ch * 64:(ch + 1) * 64],
                                     lhsT=hidT[:, hc, ch * 128:ch * 128 + rows], rhs=w2[1][:, hc, :],
                                     start=(hc == 0), stop=(hc == 1))
                            P.do("vector", "tensor_copy", out=vcmp[0:rows, ch, g, 0:64],
                                 in_=B[4][0:rows, ch * 64:(ch + 1) * 64])
            P.flush()

        with ExitStack() as st1:
            Win = P.sb(st1, "n1_Win", [128, 8, 3632], BF16)
            Wout = P.sb(st1, "n1_Wout", [128, 8, 1024], BF16)
            gpost = P.sb(st1, "n1_gpost", [128, 1024], F32)
            bgate = P.sb(st1, "n1_bgate", [128, 48], F32)
            with ExitStack() as stl:
                stage = [P.sb(stl, f"n1_stage{i}", [128, 3632], F32) for i in range(2)]
                for c in range(8):
                    sg = stage[c % 2]
                    P.dma("sync" if c % 2 == 0 else "gpsimd", sg[:], w_in[c * 128:(c + 1) * 128, :])
                    P.do("vector" if c % 2 == 0 else "gpsimd", "tensor_scalar", out=Win[:, c, :], in0=sg[:],
                         scalar1=gpre[:, c:c + 1], scalar2=None, op0=ALU.mult)
                for c in range(8):
                    sg = stage[c % 2]
                    P.dma("sync" if c % 2 == 0 else "gpsimd", sg[:, 0:1024], w_out[c * 128:(c + 1) * 128, :])
                    P.do("vector" if c % 2 == 0 else "gpsimd", "tensor_copy", out=Wout[:, c, :], in_=sg[:, 0:1024])
                P.dma("sync", gpost[:], post_g.rearrange("(o n) -> o n", o=1).to_broadcast([128, 1024]))
                P.dma("sync", bgate[:], b_gate.rearrange("(o n) -> o n", o=1).to_broadcast([128, 48]))
                P.flush()

            ksT = P.sb(st1, "n1_ksT", [128, 4, S], BF16)
            vsa = P.sb(st1, "n1_vsa", [128, NT, 4, 65], BF16)
            kwT = P.sb(st1, "n1_kwT", [64, 4, 5 * 128], BF16)
            vwa = P.sb(st1, "n1_vwa", [128, 5, 4, 65], BF16)
            qaug = P.sb(st1, "n1_qaug", [128, 16, 128], BF16)
            qmask = P.track(qaug.h[64:128, :, :], "n1_qmask")
            qr = P.sb(st1, "n1_qr", [128, 1024], BF16)
            ksr = P.sb(st1, "n1_ksr", [128, 256], BF16)
            kwr = P.sb(st1, "n1_kwr", [128, 256], BF16)
            zs = P.sb(st1, "n1_zs", [128, 1024], BF16)
            glx = P.sb(st1, "n1_glx", [128, 48], F32)
            gt = P.sb(st1, "n1_gt", [128, 48], F32)
            PT = [P.sb(st1, f"n1_PT{i}", [128, 512], BF16) for i in range(3)]
            lrec = P.sb(st1, "n1_lrec", [128, 4], F32)
            wsc = P.sb(st1, "n1_wsc", [128, 4], F32)
            imp = P.sb(st1, "n1_imp", [128, 64], F32)
            imp2 = P.sb(st1, "n1_imp2", [128, 64], F32)
            imp3 = P.sb(st1, "n1_imp3", [128, 64], F32)
            m8 = P.sb(st1, "n1_m8", [128, 16], F32)
            M1 = P.sb(st1, "n1_M1", [128, 64], F32)
            Cm = P.sb(st1, "n1_Cm", [128, 64], F32)
            selm = P.sb(st1, "n1_selm", [128, 128], BF16)
            oacc = P.sb(st1, "n1_oacc", [128, 1024], F32)
            og = P.sb(st1, "n1_og", [128, 1024], BF16)
            ogT = P.sb(st1, "n1_ogT", [128, 1024], BF16)
            t1 = P.sb(st1, "n1_t1", [128, 1024], F32)
            xo = [P.sb(st1, f"n1_xo{i}", [128, 1024], F32) for i in range(2)]

            P.do("gpsimd", "memset", ap=ksT[64:128, :, :], constant=1.0, w=[ksT])
            for g in range(4):
                P.do("gpsimd", "affine_select", out=ksT[64:128, g, :], in_=ksT[64:128, g, :], pattern=[[1, S]],
                     compare_op=ALU.is_ge, fill=0.0, base=0, channel_multiplier=-64)
                P.do("gpsimd", "affine_select", out=ksT[64:128, g, :], in_=ksT[64:128, g, :], pattern=[[-1, S]],
                     compare_op=ALU.is_ge, fill=0.0, base=63, channel_multiplier=64)
            P.do("gpsimd", "memset", ap=vsa[:, :, :, 64:65], constant=1.0, w=[vsa])
            P.do("gpsimd", "memset", ap=vwa[:, :, :, 64:65], constant=1.0, w=[vwa])
            P.do("gpsimd", "memset", ap=selm[:], constant=0.0, w=[selm])

            npt = 0
            nsb = 0
            for T in range(ntiles):
                x_t = _front(P, T, x_in, xt, sq, ss, rstd, hb, hT, pTb, ident)

                def hTc(c):
                    return hT[:, c * 128:(c + 1) * 128]

                for bk, lo, wd in ((B[1], 0, 512), (B[2], 512, 512), (B[3], 1536, 512), (B[4], 2048, 512), (B[5], 2560, 48)):
                    for c in range(8):
                        P.do("tensor", "matmul", out=bk[:, 0:wd], lhsT=hTc(c), rhs=Win[:, c, lo:lo + wd],
                             start=(c == 0), stop=(c == 7))
                cb, sb_ = costab[:, T, :], sintab[:, T, :]
                _rope(P, B[1][:], 8, cb, sb_, rtmp, qr[:, 0:512])
                _rope(P, B[2][:], 8, cb, sb_, rtmp, qr[:, 512:1024])
                _rope(P, B[3][:, 0:256], 4, cb, sb_, rtmp, ksr[:])
                _rope(P, B[4][:, 0:256], 4, cb, sb_, rtmp, kwr[:])
                P.do("scalar", "copy", out=vsa[:, T, :, 0:64], in_=B[3][:, 256:512].rearrange("p (g d) -> p g d", d=64))
                P.do("scalar", "copy", out=vwa[:, T % 5, :, 0:64], in_=B[4][:, 256:512].rearrange("p (g d) -> p g d", d=64))
                P.do("vector", "tensor_tensor", out=glx[:], in0=B[5][:, 0:48], in1=bgate[:], op=ALU.add)
                P.do("scalar", "activation", out=glx[:], in_=glx[:], func=AF.Exp, scale=-1.0)
                P.do("vector", "tensor_scalar", out=glx[:], in0=glx[:], scalar1=1.0, scalar2=None, op0=ALU.add)
                P.do("vector", "reciprocal", out=gt[:], in_=glx[:])
                for i, bk in enumerate((B[1], B[2])):
                    for c in range(8):
                        P.do("tensor", "matmul", out=bk[:], lhsT=hTc(c), rhs=Win[:, c, 2608 + i * 512:2608 + (i + 1) * 512],
                             start=(c == 0), stop=(c == 7))
                for half in range(2):
                    for hh in range(8):
                        h = half * 8 + hh
                        P.do("tensor", "transpose", out=pTb[0:64, hh * 128:(hh + 1) * 128],
                             in_=qr[:, h * 64:(h + 1) * 64], identity=ident[:])
                    P.do("scalar", "mul", out=qaug[0:64, half * 8:(half + 1) * 8, :],
                         in_=pTb[0:64, :].rearrange("p (h t) -> p h t", h=8), mul=0.125)
                for g in range(4):
                    P.do("tensor", "transpose", out=pTb[0:64, g * 128:(g + 1) * 128], in_=ksr[:, g * 64:(g + 1) * 64],
                         identity=ident[:])
                for g in range(4):
                    P.do("tensor", "transpose", out=pTb[0:64, 512 + g * 128:512 + (g + 1) * 128],
                         in_=kwr[:, g * 64:(g + 1) * 64], identity=ident[:])
                P.do("vector", "tensor_copy", out=ksT[0:64, :, T * 128:(T + 1) * 128],
                     in_=pTb[0:64, 0:512].rearrange("p (g t) -> p g t", g=4))
                P.do("vector", "tensor_copy", out=kwT[:, :, (T % 5) * 128:(T % 5 + 1) * 128],
                     in_=pTb[0:64, 512:1024].rearrange("p (g t) -> p g t", g=4))
                P.do("scalar", "activation", out=zs[:, 0:512], in_=B[1][:], func=AF.Silu)
                P.do("scalar", "activation", out=zs[:, 512:1024], in_=B[2][:], func=AF.Silu)

                P.do("gpsimd", "memset", ap=M1[:], constant=0.0, w=[M1])
                P.do("gpsimd", "memset", ap=Cm[:], constant=0.0, w=[Cm])
                for half in range(2):
                    cur = 2 * T + half
                    rs_ = slice(half * 64, (half + 1) * 64)
                    if cur - 2 >= 1:
                        P.do("gpsimd", "memset", ap=M1[rs_, 1:cur - 1], constant=1.0, w=[M1])
                    if cur + 1 < 64:
                        P.do("gpsimd", "memset", ap=Cm[rs_, cur + 1:64], constant=-1e30, w=[Cm])
                    P.do("gpsimd", "memset", ap=Cm[rs_, max(cur - 1, 0):cur + 1], constant=1e9, w=[Cm])
                P.do("gpsimd", "memset", ap=Cm[:, 0:1], constant=1e9, w=[Cm])

                nvalid = min(8 * T + 7, nblk)
                for g in range(4):
                    qg = qaug[0:64, 4 * g:4 * g + 4, :]
                    qga = qaug[:, 4 * g:4 * g + 4, :]
                    nch = (nvalid + 127) // 128
                    for ch in range(nch):
                        rows = min(128, nvalid - ch * 128)
                        sbk = B[3 + nsb % 2]; nsb += 1
                        pt = PT[npt % 3]; npt += 1
                        P.do("tensor", "matmul", out=sbk[0:rows, :], lhsT=kcmpT[:, g, ch * 128:ch * 128 + rows], rhs=qg,
                             start=True, stop=True)
                        P.do("scalar", "activation", out=pt[0:rows, :], in_=sbk[0:rows, :], func=AF.Exp)
                        P.do("gpsimd", "affine_select", out=pt[0:rows, :], in_=pt[0:rows, :], pattern=[[0, 4], [1, 128]],
                             compare_op=ALU.is_ge, fill=0.0, base=128 * T - 2048 * ch - 31, channel_multiplier=-16)
                        for h in range(4):
                            P.do("tensor", "matmul", out=B[1 + h // 2][:, (h % 2) * 129:(h % 2) * 129 + 129],
                                 lhsT=pt[0:rows, h * 128:(h + 1) * 128], rhs=vcmp[0:rows, ch, g, :],
                                 start=(ch == 0 and h % 2 == 0), stop=(ch == nch - 1 and h % 2 == 1))
                    for h in range(4):
                        bo = B[1 + h // 2]
                        off = (h % 2) * 129
                        P.do("vector", "tensor_scalar", out=lrec[:, h:h + 1], in0=bo[:, off + 64:off + 65], scalar1=1e-20,
                             scalar2=None, op0=ALU.max)
                    P.do("vector", "reciprocal", out=lrec[:], in_=lrec[:])
                    P.do("vector", "tensor_tensor", out=wsc[:], in0=lrec[:],
                         in1=gt[:].rearrange("p (h c) -> p h c", c=3)[:, 4 * g:4 * g + 4, 0], op=ALU.mult)
                    for h in range(4):
                        bo = B[1 + h // 2]
                        off = (h % 2) * 129
                        H = 4 * g + h
                        P.do("vector", "tensor_scalar", out=oacc[:, H * 64:(H + 1) * 64], in0=bo[:, off:off + 64],
                             scalar1=wsc[:, h:h + 1], scalar2=None, op0=ALU.mult)
                        if h == 0:
                            P.do("vector", "tensor_scalar", out=imp[:], in0=bo[:, off + 65:off + 129],
                                 scalar1=lrec[:, h:h + 1], scalar2=None, op0=ALU.mult)
                        else:
                            P.do("vector", "scalar_tensor_tensor", out=imp[:], in0=bo[:, off + 65:off + 129],
                                 scalar=lrec[:, h:h + 1], in1=imp[:], op0=ALU.mult, op1=ALU.add)
                    P.do("vector", "tensor_tensor", out=imp2[:], in0=imp[:], in1=M1[:], op=ALU.mult)
                    P.do("vector", "tensor_tensor", out=imp2[:], in0=imp2[:], in1=Cm[:], op=ALU.add)
                    P.do("vector", "max", out=m8[:, 0:8], in_=imp2[:])
                    P.do("vector", "match_replace", out=imp3[:], in_to_replace=m8[:, 0:8], in_values=imp2[:],
                         imm_value=-3e38)
                    P.do("vector", "max", out=m8[:, 8:16], in_=imp3[:])
                    P.do("vector", "tensor_scalar", out=selm[:, 64:128], in0=imp2[:], scalar1=m8[:, 15:16], scalar2=NEGM,
                         op0=ALU.is_lt, op1=ALU.mult)
                    P.do("tensor", "transpose", out=pTb[:, 0:128], in_=selm[:], identity=ident[:])
                    P.do("vector", "tensor_copy", out=qmask[:, 4 * g:4 * g + 4, :],
                         in_=V(B[0], pTb.ap[64:128, 0:128].unsqueeze(1).to_broadcast([64, 4, 128])))
                    kts = list(range(max(0, T - 4), T + 1))
                    for kt in kts:
                        sbk = B[3 + nsb % 2]; nsb += 1
                        pt = PT[npt % 3]; npt += 1
                        sl = kt % 5
                        P.do("tensor", "matmul", out=sbk[:], lhsT=kwT[:, g, sl * 128:(sl + 1) * 128], rhs=qg,
                             start=True, stop=True)
                        P.do("scalar", "activation", out=pt[:], in_=sbk[:], func=AF.Exp)
                        if kt == T:
                            P.do("gpsimd", "affine_select", out=pt[:], in_=pt[:], pattern=[[0, 4], [1, 128]],
                                 compare_op=ALU.is_ge, fill=0.0, base=0, channel_multiplier=-1)
                        if kt == T - 4:
                            P.do("gpsimd", "affine_select", out=pt[:], in_=pt[:], pattern=[[0, 4], [-1, 128]],
                                 compare_op=ALU.is_ge, fill=0.0, base=-1, channel_multiplier=1)
                        for h in range(4):
                            P.do("tensor", "matmul", out=B[6][:, h * 65:h * 65 + 65], lhsT=pt[:, h * 128:(h + 1) * 128],
                                 rhs=vwa[:, sl, g, :], start=(kt == kts[0] and h == 0), stop=(kt == T and h == 3))
                    _combine(P, B[6], g, 2, lrec, wsc, gt, oacc)
                    for kt in range(T + 1):
                        sbk = B[3 + nsb % 2]; nsb += 1
                        pt = PT[npt % 3]; npt += 1
                        P.do("tensor", "matmul", out=sbk[:], lhsT=ksT[:, g, kt * 128:(kt + 1) * 128], rhs=qga,
                             start=True, stop=True, r=[qmask])
                        P.do("scalar", "activation", out=pt[:], in_=sbk[:], func=AF.Exp)
                        if kt == T:
                            P.do("gpsimd", "affine_select", out=pt[:], in_=pt[:], pattern=[[0, 4], [1, 128]],
                                 compare_op=ALU.is_ge, fill=0.0, base=0, channel_multiplier=-1)
                        for h in range(4):
                            P.do("tensor", "matmul", out=B[5][:, h * 65:h * 65 + 65], lhsT=pt[:, h * 128:(h + 1) * 128],
                                 rhs=vsa[:, kt, g, :], start=(kt == 0 and h == 0), stop=(kt == T and h == 3))
                    _combine(P, B[5], g, 1, lrec, wsc, gt, oacc)

                P.do("gpsimd", "tensor_tensor", out=og[:], in0=oacc[:], in1=zs[:], op=ALU.mult)
                for c in range(8):
                    P.do("tensor", "transpose", out=pTb[:, c * 128:(c + 1) * 128], in_=og[:, c * 128:(c + 1) * 128],
                         identity=ident[:])
                P.do("vector", "tensor_copy", out=ogT[:], in_=pTb)
                for i in range(2):
                    for fc in range(8):
                        P.do("tensor", "matmul", out=B[1 + i][:], lhsT=ogT[:, fc * 128:(fc + 1) * 128],
                             rhs=Wout[:, fc, i * 512:(i + 1) * 512], start=(fc == 0), stop=(fc == 7))
                P.do("scalar", "activation", out=sq[:, 0:512], in_=B[1][:], func=AF.Square, accum_out=ss[:, 1:2])
                P.do("scalar", "activation", out=sq[:, 512:1024], in_=B[2][:], func=AF.Square, accum_out=ss[:, 2:3])
                P.do("vector", "tensor_tensor", out=ss[:, 3:4], in0=ss[:, 1:2], in1=ss[:, 2:3], op=ALU.add)
                P.do("vector", "tensor_scalar", out=rstd[:, 1:2], in0=ss[:, 3:4], scalar1=1.0 / D, scalar2=EPS,
                     op0=ALU.mult, op1=ALU.add)
                P.do("scalar", "activation", out=rstd[:, 1:2], in_=rstd[:, 1:2], func=AF.Sqrt)
                P.do("vector", "reciprocal", out=rstd[:, 1:2], in_=rstd[:, 1:2])
                for i in range(2):
                    P.do("vector", "scalar_tensor_tensor", out=t1[:, i * 512:(i + 1) * 512], in0=B[1 + i][:],
                         scalar=rstd[:, 1:2], in1=gpost[:, i * 512:(i + 1) * 512], op0=ALU.mult, op1=ALU.mult)
                x_o = xo[T % 2]
                P.do("gpsimd", "tensor_tensor", out=x_o[:], in0=t1[:], in1=x_t[:], op=ALU.add)
                P.dma("sync", x_out[T * 128:(T + 1) * 128, :], x_o[:])
            P.flush()


def _combine(P, bo, g, c, lrec, wsc, gt, oacc):
    for h in range(4):
        P.do("vector", "tensor_scalar", out=lrec[:, h:h + 1], in0=bo[:, h * 65 + 64:h * 65 + 65], scalar1=1e-20,
             scalar2=None, op0=ALU.max)
    P.do("vector", "reciprocal", out=lrec[:], in_=lrec[:])
    P.do("vector", "tensor_tensor", out=wsc[:], in0=lrec[:],
         in1=gt[:].rearrange("p (h c) -> p h c", c=3)[:, 4 * g:4 * g + 4, c], op=ALU.mult)
    for h in range(4):
        H = 4 * g + h
        P.do("vector", "scalar_tensor_tensor", out=oacc[:, H * 64:(H + 1) * 64], in0=bo[:, h * 65:h * 65 + 64],
             scalar=wsc[:, h:h + 1], in1=oacc[:, H * 64:(H + 1) * 64], op0=ALU.mult, op1=ALU.add)


FUSED = True


def _din(nc, name, shape, dt=F32):
    return nc.dram_tensor(name, list(shape), dt, kind="ExternalInput").ap()


def _gla_inputs(nc):
    return dict(pre=_din(nc, "g_pre", [1024]), post=_din(nc, "g_post", [1024]), w_in=_din(nc, "g_w_in", [1024, 3088]),
                w_up=_din(nc, "g_w_up", [16, 512]), b_gk=_din(nc, "g_b_gk", [512]), hnorm=_din(nc, "g_hnorm", [256]),
                w_out=_din(nc, "g_w_out", [1024, 1024]))


def _nsa_inputs(nc):
    return dict(pos=_din(nc, "n_pos", [4096], I32), pre=_din(nc, "n_pre", [1024]), post=_din(nc, "n_post", [1024]),
                w_in=_din(nc, "n_w_in", [1024, 3632]), b_gate=_din(nc, "n_b_gate", [48]),
                pe_k=_din(nc, "n_pe_k", [32, 64]), pe_v=_din(nc, "n_pe_v", [32, 64]),
                ck_w1=_din(nc, "n_ck_w1", [2048, 256]), ck_w2=_din(nc, "n_ck_w2", [256, 64]),
                cv_w1=_din(nc, "n_cv_w1", [2048, 256]), cv_w2=_din(nc, "n_cv_w2", [256, 64]),
                w_out=_din(nc, "n_w_out", [1024, 1024]))


def _emit_gla(P, nc, x, y, gi):
    gla_layer(P, nc, x, y, gi["pre"], gi["post"], gi["w_in"], gi["w_up"], gi["b_gk"], gi["hnorm"], gi["w_out"])


def _emit_nsa(P, nc, x, y, ni):
    nsa_layer(P, nc, x, y, ni["pos"], ni["pre"], ni["post"], ni["w_in"], ni["b_gate"], ni["pe_k"], ni["pe_v"],
              ni["ck_w1"], ni["ck_w2"], ni["cv_w1"], ni["cv_w2"], ni["w_out"])


def _gla_map(inp, b):
    return {"g_pre": inp["pre_norm"][0], "g_post": inp["post_norm"][0], "g_w_in": inp["gla_w_in"][0],
            "g_w_up": inp["gla_w_gk_up"][0], "g_b_gk": inp["gla_b_gk"][0], "g_hnorm": inp["gla_head_norm"][0],
            "g_w_out": inp["gla_w_out"][0]}


def _nsa_map(inp, b):
    return {"n_pos": np.ascontiguousarray(inp["positions"][b]), "n_pre": inp["pre_norm"][1], "n_post": inp["post_norm"][1],
            "n_w_in": inp["nsa_w_in"][0], "n_b_gate": inp["nsa_b_gate"][0], "n_pe_k": inp["nsa_pe_k"][0],
            "n_pe_v": inp["nsa_pe_v"][0], "n_ck_w1": inp["nsa_ck_w1"][0], "n_ck_w2": inp["nsa_ck_w2"][0],
            "n_cv_w1": inp["nsa_cv_w1"][0], "n_cv_w2": inp["nsa_cv_w2"][0], "n_w_out": inp["nsa_w_out"][0]}


def kernel(**inputs):
    inp = {k: np.ascontiguousarray(np.asarray(v)) for k, v in inputs.items()}
    n = 8
    cores = list(range(n))
    xs = [np.ascontiguousarray(inp["x"][b]) for b in range(n)]
    if FUSED:
        nc = bass.Bass("TRN2", target_bir_lowering=False)
        x = _din(nc, "x", [4096, 1024])
        y = nc.dram_tensor("y", [4096, 1024], F32, kind="ExternalOutput").ap()
        x1 = nc.dram_tensor("x1_stage", [4096, 1024], F32, kind="ExternalOutput").ap()
        gi = _gla_inputs(nc)
        ni = _nsa_inputs(nc)
        P = Prog(nc)
        _emit_gla(P, nc, x, x1, gi)
        _emit_nsa(P, nc, x1, y, ni)
        maps = [dict(x=xs[b], **_gla_map(inp, b), **_nsa_map(inp, b)) for b in range(n)]
        res = run_bass_kernel_spmd(nc, maps, core_ids=cores)
        return np.stack([np.asarray(res.results[b]["y"]) for b in range(n)], axis=0).astype(np.float32)
    nc1 = bass.Bass("TRN2", target_bir_lowering=False)
    x = _din(nc1, "x", [4096, 1024])
    y = nc1.dram_tensor("y", [4096, 1024], F32, kind="ExternalOutput").ap()
    gi = _gla_inputs(nc1)
    _emit_gla(Prog(nc1), nc1, x, y, gi)
    res1 = run_bass_kernel_spmd(nc1, [dict(x=xs[b], **_gla_map(inp, b)) for b in range(n)], core_ids=cores)
    x1s = [np.ascontiguousarray(np.asarray(res1.results[b]["y"])) for b in range(n)]
    nc2 = bass.Bass("TRN2", target_bir_lowering=False)
    x = _din(nc2, "x", [4096, 1024])
    y = nc2.dram_tensor("y", [4096, 1024], F32, kind="ExternalOutput").ap()
    ni = _nsa_inputs(nc2)
    _emit_nsa(Prog(nc2), nc2, x, y, ni)
    res2 = run_bass_kernel_spmd(nc2, [dict(x=x1s[b], **_nsa_map(inp, b)) for b in range(n)], core_ids=cores)
    return np.stack([np.asarray(res2.results[b]["y"]) for b in range(n)], axis=0).astype(np.float32)
```

```python
import math
from contextlib import ExitStack
import numpy as np
import concourse.bass as bass
import concourse.mybir as mybir
from concourse.bass_utils import run_bass_kernel_spmd

F32 = mybir.dt.float32
BF16 = mybir.dt.bfloat16
I32 = mybir.dt.int32
AF = mybir.ActivationFunctionType
ALU = mybir.AluOpType
AX = mybir.AxisListType

ENGS = ("tensor", "vector", "scalar", "gpsimd", "sync")
OUT_KEYS = ("out", "accum_out")


class Buf:
    def __init__(self, h, name):
        self.h = h
        self.name = name
        self.w = None
        self.r = []
        self.sem = None
        self.semcnt = 0

    def __getitem__(self, idx):
        return V(self, self.h[idx])


class V:
    def __init__(self, buf, ap):
        self.buf = buf
        self.ap = ap

    def __getitem__(self, idx):
        return V(self.buf, self.ap[idx])

    def rearrange(self, *a, **k):
        return V(self.buf, self.ap.rearrange(*a, **k))

    def bitcast(self, dt):
        return V(self.buf, self.ap.bitcast(dt))

    def to_broadcast(self, shape):
        return V(self.buf, self.ap.to_broadcast(shape))


class Op:
    __slots__ = ("eng", "fn", "deps", "signal", "idx", "dma_sem", "dma_cnt", "semval")

    def __init__(self, eng, fn):
        self.eng = eng
        self.fn = fn
        self.deps = []
        self.signal = False
        self.dma_sem = None
        self.dma_cnt = 0
        self.semval = 0


class Prog:
    def __init__(self, nc):
        self.nc = nc
        self.esem = {e: nc.alloc_semaphore(f"s_{e}") for e in ENGS}
        self.ecount = {e: 0 for e in ENGS}
        self.ops = {e: [] for e in ENGS}
        self.nbuf = 0
        self.dma_sems = []
        self.all_bufs = []

    def sb(self, stack, name, shape, dtype):
        h = stack.enter_context(self.nc.sbuf_tensor(name, list(shape), dtype))
        b = Buf(h, name)
        self.all_bufs.append(b)
        return b

    def ps(self, stack, name, shape, dtype=F32):
        h = stack.enter_context(self.nc.psum_tensor(name, list(shape), dtype))
        b = Buf(h, name)
        b.psum = True
        self.all_bufs.append(b)
        return b

    def track(self, h, name):
        b = Buf(h, name)
        self.all_bufs.append(b)
        return b

    def _collect(self, kw):
        reads, writes, real = [], [], {}
        for k, v in kw.items():
            if isinstance(v, V):
                (writes if k in OUT_KEYS else reads).append(v.buf)
                real[k] = v.ap
            else:
                real[k] = v
        return reads, writes, real

    def _add(self, eng, fn, reads, writes, is_dma=False, dma_buf=None, extra_reads=(), extra_writes=()):
        self.nops = getattr(self, "nops", 0) + 1
        if self.nops > getattr(self, "maxops", 1 << 60):
            return None
        op = Op(eng, fn)
        reads = list(reads) + list(extra_reads)
        writes = list(writes) + list(extra_writes)
        deps = []
        for b in reads:
            if b.w is not None:
                deps.append(("raw", b.w))
            if getattr(b, "psum", False):
                for t in b.r:
                    if t[0] == "c" and t[1] != eng:
                        deps.append(("rar", t))
        for b in writes:
            if b.w is not None:
                deps.append(("waw", b.w))
            for t in b.r:
                deps.append(("war", t))
        opidx = len(self.ops[eng])
        keep = []
        for kind, t in deps:
            if t[0] == "c":
                if t[1] == eng and not is_dma:
                    if kind != "raw" or eng == "tensor":
                        continue
            keep.append(t)
        op.deps = keep
        self.ops[eng].append(op)
        if is_dma:
            b = dma_buf
            if b.sem is None:
                b.sem = self.nc.alloc_semaphore(f"d_{b.name}")
                self.dma_sems.append(b)
            b.semcnt += 16
            op.dma_sem = b.sem
            ticket = ("d", b, b.semcnt)
        else:
            ticket = ("c", eng, opidx)
        for b in reads:
            b.r.append(ticket)
        for b in writes:
            b.w = ticket
            b.r = []
        return op

    def do(self, eng, name, r=(), w=(), **kw):
        reads, writes, real = self._collect(kw)
        self.last_desc = (eng, name)
        fn = lambda e, name=name, real=real: getattr(e, name)(**real)
        return self._add(eng, fn, reads, writes, extra_reads=r, extra_writes=w)

    def dma(self, eng, out, in_, **kw):
        reads, writes = [], []
        oa, ia = out, in_
        if isinstance(out, V):
            writes.append(out.buf)
            oa = out.ap
        if isinstance(in_, V):
            reads.append(in_.buf)
            ia = in_.ap
        dbuf = writes[0] if writes else reads[0]
        fn = lambda e, oa=oa, ia=ia, kw=kw: e.dma_start(out=oa, in_=ia, **kw)
        return self._add(eng, fn, reads, writes, is_dma=True, dma_buf=dbuf)

    def flush(self, final_wait_bufs=()):
        nc = self.nc
        for e in ENGS:
            for op in self.ops[e]:
                for t in op.deps:
                    if t[0] == "c":
                        self.ops[t[1]][t[2]].signal = True
        for e in ENGS:
            for op in reversed(self.ops[e]):
                if op.dma_sem is None:
                    op.signal = True
                    break
        for e in ENGS:
            c = self.ecount[e]
            for op in self.ops[e]:
                if op.signal:
                    c += 1
                op.semval = c
            self.ecount[e] = c
        ops = self.ops
        esem = self.esem
        ecount = dict(self.ecount)
        dsems = list(self.dma_sems)

        def emit(e):
            def body(eng):
                waited = {}
                for op in ops[e]:
                    need = {}
                    for t in op.deps:
                        if t[0] == "c":
                            key = ("c", t[1])
                            val = ops[t[1]][t[2]].semval
                            sem = esem[t[1]]
                        else:
                            key = ("d", id(t[1]))
                            val = t[2]
                            sem = t[1].sem
                        if waited.get(key, -1) >= val:
                            continue
                        if key not in need or need[key][1] < val:
                            need[key] = (sem, val)
                    for key, (sem, val) in need.items():
                        eng.wait_ge(sem, val)
                        waited[key] = val
                    ins = op.fn(eng)
                    if op.dma_sem is not None:
                        ins.then_inc(op.dma_sem, 16)
                    elif op.signal:
                        ins.then_inc(esem[e], 1)
                for e2 in ENGS:
                    if e2 != e and ecount[e2] > 0 and waited.get(("c", e2), -1) < ecount[e2]:
                        eng.wait_ge(esem[e2], ecount[e2])
                for b in dsems:
                    if waited.get(("d", id(b)), -1) < b.semcnt:
                        eng.wait_ge(b.sem, b.semcnt)
            return body

        with nc.Block() as blk:
            for e in ENGS:
                getattr(blk, e)(emit(e))
        self.ops = {e: [] for e in ENGS}
        for b in self.all_bufs:
            b.w = None
            b.r = []


S = 4096
D = 1024
NT = S // 128
EPS = 1e-6


def make_ident(P, st, pfx="g"):
    identf = P.sb(st, pfx + "_identf", [128, 128], F32)
    ident = P.sb(st, pfx + "_ident", [128, 128], BF16)
    P.do("gpsimd", "memset", ap=identf[:], constant=1.0, w=[identf])
    P.do("gpsimd", "affine_select", out=identf[:], in_=identf[:], pattern=[[-1, 128]],
         compare_op=ALU.is_equal, fill=0.0, base=0, channel_multiplier=1)
    P.do("vector", "tensor_copy", out=ident[:], in_=identf[:])
    return ident, identf


def gla_layer(P, nc, x_in, x_out, pre_g, post_g, w_in, w_up, b_gk, hnorm, w_out, ntiles=NT):
    with ExitStack() as st:
        ident, identf = make_ident(P, st)
        Win = P.sb(st, "g_Win", [128, 8, 3088], BF16)
        Wout = P.sb(st, "g_Wout", [128, 8, 1024], BF16)
        stage = [P.sb(st, f"g_stage{i}", [128, 3088], F32) for i in range(2)]
        gpre = P.sb(st, "g_gpre", [128, 8], F32)
        gpost = P.sb(st, "g_gpost", [128, 1024], F32)
        wupf = P.sb(st, "g_wupf", [16, 512], F32)
        wup = P.sb(st, "g_wup", [16, 512], BF16)
        negb = P.sb(st, "g_negb", [128, 4], F32)
        hn = P.sb(st, "g_hn", [128, 2], F32)
        onesb = P.sb(st, "g_ones", [128, 128], BF16)
        rmask = P.sb(st, "g_rmask", [128, 512], F32)
        bdmask = P.sb(st, "g_bdmask", [128, 512], F32)
        one1 = P.sb(st, "g_one1", [128, 1], F32)

        P.dma("sync", gpre[:], pre_g.rearrange("(c p o) -> p c o", p=128, o=1), allow_slow_non_contiguous=True)
        P.dma("sync", negb[:], b_gk.rearrange("(c p o) -> p c o", p=128, o=1), allow_slow_non_contiguous=True)
        P.dma("sync", hn[:], hnorm.rearrange("(c p o) -> p c o", p=128, o=1), allow_slow_non_contiguous=True)
        P.dma("sync", wupf[:], w_up)
        P.dma("sync", gpost[:], post_g.rearrange("(o n) -> o n", o=1).to_broadcast([128, 1024]))
        P.do("vector", "tensor_scalar", out=negb[:], in0=negb[:], scalar1=-1.0, scalar2=None, op0=ALU.mult)
        P.do("vector", "tensor_copy", out=wup[:], in_=wupf[:])
        P.do("gpsimd", "memset", ap=onesb[:], constant=1.0, w=[onesb])
        P.do("gpsimd", "memset", ap=one1[:], constant=1.0, w=[one1])
        P.do("gpsimd", "memset", ap=rmask[:], constant=1.0, w=[rmask])
        P.do("gpsimd", "memset", ap=rmask[:].rearrange("p (a b) -> p a b", b=64)[:, :, 0:1], constant=0.0, w=[rmask])
        P.do("gpsimd", "memset", ap=bdmask[:], constant=1.0, w=[bdmask])
        for hh in range(4):
            sl = bdmask[:, hh * 128:(hh + 1) * 128]
            P.do("gpsimd", "affine_select", out=sl, in_=sl, pattern=[[1, 128]], compare_op=ALU.is_ge,
                 fill=0.0, base=0, channel_multiplier=-1)
            sl2 = bdmask[64:128, hh * 128:hh * 128 + 64]
            P.do("gpsimd", "memset", ap=sl2, constant=0.0, w=[bdmask])
            sl3 = bdmask[0:64, hh * 128 + 64:(hh + 1) * 128]
            P.do("gpsimd", "memset", ap=sl3, constant=0.0, w=[bdmask])
        for c in range(8):
            sg = stage[c % 2]
            P.dma("sync" if c % 2 == 0 else "gpsimd", sg[:], w_in[c * 128:(c + 1) * 128, :])
            P.do("vector" if c % 2 == 0 else "gpsimd", "tensor_scalar", out=Win[:, c, :], in0=sg[:],
                 scalar1=gpre[:, c:c + 1], scalar2=None, op0=ALU.mult)
        for c in range(8):
            sg = stage[c % 2]
            P.dma("sync" if c % 2 == 0 else "gpsimd", sg[:, 0:1024], w_out[c * 128:(c + 1) * 128, :])
            P.do("vector" if c % 2 == 0 else "gpsimd", "tensor_copy", out=Wout[:, c, :], in_=sg[:, 0:1024])

        xt = [P.sb(st, f"g_xt{i}", [128, 1024], F32) for i in range(2)]
        xo = [P.sb(st, f"g_xo{i}", [128, 1024], F32) for i in range(2)]
        sq = P.sb(st, "g_sq", [128, 1024], F32)
        ss = P.sb(st, "g_ss", [128, 4], F32)
        rstd = P.sb(st, "g_rstd", [128, 2], F32)
        hb = P.sb(st, "g_hb", [128, 1024], BF16)
        hT = P.sb(st, "g_hT", [128, 1024], BF16)
        glrT = P.sb(st, "g_glrT", [16, 128], BF16)
        e1 = P.sb(st, "g_e1", [128, 512], F32)
        yv = P.sb(st, "g_yv", [128, 512], F32)
        Bc = P.sb(st, "g_Bc", [128, 512], F32)
        eb = P.sb(st, "g_eb", [128, 512], F32)
        enb = P.sb(st, "g_enb", [128, 512], F32)
        qeT = P.sb(st, "g_qeT", [128, 512], BF16)
        keT = P.sb(st, "g_keT", [128, 512], BF16)
        kdT = P.sb(st, "g_kdT", [128, 512], BF16)
        kdz = [P.sb(st, f"g_kd{i}", [128, 512], BF16) for i in range(2)]
        for i in range(2):
            P.do("gpsimd", "memset", ap=kdz[i][:], constant=0.0, w=[kdz[i]])
        zs = P.sb(st, "g_zs", [128, 1024], BF16)
        vb = P.sb(st, "g_vb", [128, 1024], BF16)
        ATm = P.sb(st, "g_ATm", [128, 512], BF16)
        oT = P.sb(st, "g_oT", [128, 1024], F32)
        osq = P.sb(st, "g_osq", [128, 1024], BF16)
        rs = P.sb(st, "g_rs", [128, 512], F32)
        tmpo = P.sb(st, "g_tmpo", [128, 1024], F32)
        ogT = P.sb(st, "g_ogT", [128, 1024], BF16)
        S32 = [P.sb(st, f"g_S32_{h}", [128, 256], F32) for h in range(4)]
        Sbf = [[P.sb(st, f"g_Sbf_{h}_{i}", [128, 256], BF16) for i in range(2)] for h in range(4)]
        t1 = P.sb(st, "g_t1", [128, 1024], F32)

        bank = [P.ps(st, f"g_bank{i}", [128, 512], F32) for i in range(7)]
        bS = P.ps(st, "g_bankS", [128, 512], F32)
        pS = [bS[:, i * 256:(i + 1) * 256] for i in range(2)]
        bA, bQ, bK, bZ0, bZ1, bV0, bV1 = bank
        bG = bA
        pTb = bA[:].bitcast(BF16)

        sidx = [0, 0, 0, 0]
        nupd = 0
        P.dma("sync", xt[0][:], x_in[0:128, :])
        for t in range(ntiles):
            x_t = xt[t % 2]
            if t + 1 < ntiles:
                P.dma("sync", xt[(t + 1) % 2][:], x_in[(t + 1) * 128:(t + 2) * 128, :])
            P.do("scalar", "activation", out=sq[:], in_=x_t[:], func=AF.Square, accum_out=ss[:, 0:1])
            P.do("vector", "tensor_scalar", out=rstd[:, 0:1], in0=ss[:, 0:1], scalar1=1.0 / D, scalar2=EPS,
                 op0=ALU.mult, op1=ALU.add)
            P.do("scalar", "activation", out=rstd[:, 0:1], in_=rstd[:, 0:1], func=AF.Sqrt)
            P.do("vector", "reciprocal", out=rstd[:, 0:1], in_=rstd[:, 0:1])
            P.do("vector", "tensor_scalar", out=hb[:], in0=x_t[:], scalar1=rstd[:, 0:1], scalar2=None, op0=ALU.mult)
            for c in range(8):
                P.do("tensor", "transpose", out=pTb[:, c * 128:(c + 1) * 128], in_=hb[:, c * 128:(c + 1) * 128],
                     identity=ident[:])
            P.do("vector", "tensor_copy", out=hT[:], in_=pTb)

            def hTc(c):
                return hT[:, c * 128:(c + 1) * 128]

            for c in range(8):
                P.do("tensor", "matmul", out=bA[0:16, 0:128], lhsT=Win[:, c, 2048:2064], rhs=hTc(c),
                     start=(c == 0), stop=(c == 7))
            P.do("scalar", "copy", out=glrT[:], in_=bA[0:16, 0:128])
            for hh in range(4):
                for c in range(8):
                    P.do("tensor", "matmul", out=bQ[:, hh * 128:(hh + 1) * 128], lhsT=Win[:, c, hh * 128:(hh + 1) * 128],
                         rhs=hTc(c), start=(c == 0), stop=(c == 7))
            for hh in range(4):
                for c in range(8):
                    P.do("tensor", "matmul", out=bK[:, hh * 128:(hh + 1) * 128],
                         lhsT=Win[:, c, 512 + hh * 128:512 + (hh + 1) * 128], rhs=hTc(c), start=(c == 0), stop=(c == 7))
            for hh in range(4):
                P.do("tensor", "matmul", out=bA[:, hh * 128:(hh + 1) * 128], lhsT=wup[:, hh * 128:(hh + 1) * 128],
                     rhs=glrT[:], start=True, stop=True)
            for zc in range(8):
                bz = bZ0 if zc < 4 else bZ1
                for c in range(8):
                    P.do("tensor", "matmul", out=bz[:, (zc % 4) * 128:(zc % 4 + 1) * 128],
                         lhsT=Win[:, c, 2064 + zc * 128:2064 + (zc + 1) * 128], rhs=hTc(c), start=(c == 0), stop=(c == 7))
            for i, bv in enumerate((bV0, bV1)):
                for c in range(8):
                    P.do("tensor", "matmul", out=bv[:], lhsT=hTc(c), rhs=Win[:, c, 1024 + i * 512:1024 + (i + 1) * 512],
                         start=(c == 0), stop=(c == 7))
            for hh in range(4):
                P.do("scalar", "activation", out=e1[:, hh * 128:(hh + 1) * 128], in_=bA[:, hh * 128:(hh + 1) * 128],
                     func=AF.Exp, scale=-1.0, bias=negb[:, hh:hh + 1])
            P.do("scalar", "activation", out=yv[:], in_=e1[:], func=AF.Ln, bias=one1[:, 0:1], scale=1.0)
            P.do("vector", "tensor_tensor_scan", out=Bc[:], data0=rmask[:], data1=yv[:], initial=0.0,
                 op0=ALU.mult, op1=ALU.add)
            P.do("scalar", "activation", out=eb[:], in_=Bc[:], func=AF.Exp, scale=-1.0 / 16.0)
            P.do("scalar", "activation", out=enb[:], in_=Bc[:], func=AF.Exp, scale=1.0 / 16.0)
            P.do("vector", "scalar_tensor_tensor", out=qeT[:], in0=bQ[:], scalar=128 ** -0.5, in1=eb[:],
                 op0=ALU.mult, op1=ALU.mult)
            P.do("vector", "tensor_tensor", out=keT[:], in0=bK[:], in1=enb[:], op=ALU.mult)
            for hh in range(4):
                for cc in range(2):
                    lo = hh * 128 + cc * 64
                    P.do("vector", "scalar_tensor_tensor", out=kdT[:, lo:lo + 64], in0=bK[:, lo:lo + 64],
                         scalar=eb[:, lo + 63:lo + 64], in1=enb[:, lo:lo + 64], op0=ALU.mult, op1=ALU.mult)
            P.do("scalar", "activation", out=zs[:, 0:512], in_=bZ0[:], func=AF.Silu)
            P.do("scalar", "activation", out=zs[:, 512:1024], in_=bZ1[:], func=AF.Silu)
            P.do("gpsimd" if False else "vector", "tensor_copy", out=vb[:, 0:512], in_=bV0[:])
            P.do("scalar", "copy", out=vb[:, 512:1024], in_=bV1[:])
            for hh in range(4):
                P.do("tensor", "transpose", out=pTb[:, hh * 128:(hh + 1) * 128], in_=kdT[:, hh * 128:(hh + 1) * 128],
                     identity=ident[:])
            P.do("vector", "tensor_copy", out=kdz[0][0:64, :], in_=pTb[0:64, 0:512])
            P.do("vector", "tensor_copy", out=kdz[1][64:128, :], in_=pTb[64:128, 0:512])
            for hh in range(4):
                P.do("tensor", "matmul", out=bQ[:, hh * 128:(hh + 1) * 128], lhsT=keT[:, hh * 128:(hh + 1) * 128],
                     rhs=qeT[:, hh * 128:(hh + 1) * 128], start=True, stop=True)
            P.do("vector", "tensor_tensor", out=ATm[:], in0=bQ[:], in1=bdmask[:], op=ALU.mult)
            bO = (bV0, bV1)
            for hh in range(4):
                for cc in range(2):
                    first = (t == 0 and cc == 0)
                    scur = Sbf[hh][sidx[hh]]
                    for vc in range(2):
                        idx = hh * 2 + vc
                        out = bO[idx // 4][:, (idx % 4) * 128 + cc * 64:(idx % 4) * 128 + cc * 64 + 64]
                        P.do("tensor", "matmul", out=out,
                             lhsT=vb[:, hh * 256 + vc * 128:hh * 256 + (vc + 1) * 128],
                             rhs=ATm[:, hh * 128 + cc * 64:hh * 128 + cc * 64 + 64],
                             start=True, stop=first)
                        if not first:
                            P.do("tensor", "matmul", out=out, lhsT=scur[:, vc * 128:(vc + 1) * 128],
                                 rhs=qeT[:, hh * 128 + cc * 64:hh * 128 + cc * 64 + 64], start=False, stop=True)
                    ps = pS[nupd % 2]
                    nupd += 1
                    P.do("tensor", "matmul", out=ps, lhsT=kdz[cc][:, hh * 128:(hh + 1) * 128],
                         rhs=vb[:, hh * 256:(hh + 1) * 256], start=True, stop=True)
                    if first:
                        P.do("vector", "tensor_copy", out=S32[hh][:], in_=ps)
                    else:
                        lo = hh * 128 + cc * 64
                        P.do("vector", "scalar_tensor_tensor", out=S32[hh][:], in0=S32[hh][:],
                             scalar=eb[:, lo + 63:lo + 64], in1=ps, op0=ALU.mult, op1=ALU.add)
                    sidx[hh] ^= 1
                    P.do("gpsimd", "tensor_copy", out=Sbf[hh][sidx[hh]][:], in_=S32[hh][:])
            P.do("scalar", "copy", out=oT[:, 0:512], in_=bV0[:])
            P.do("scalar", "copy", out=oT[:, 512:1024], in_=bV1[:])
            P.do("scalar", "activation", out=osq[:, 0:512], in_=bV0[:], func=AF.Square)
            P.do("scalar", "activation", out=osq[:, 512:1024], in_=bV1[:], func=AF.Square)
            for hh in range(4):
                for vc in range(2):
                    idx = hh * 2 + vc
                    P.do("tensor", "matmul", out=bK[:, hh * 128:(hh + 1) * 128], lhsT=onesb[:],
                         rhs=osq[:, idx * 128:(idx + 1) * 128], start=(vc == 0), stop=(vc == 1))
            P.do("vector", "tensor_scalar", out=rs[:], in0=bK[:], scalar1=1.0 / 256, scalar2=EPS, op0=ALU.mult, op1=ALU.add)
            P.do("scalar", "activation", out=rs[:], in_=rs[:], func=AF.Sqrt)
            P.do("vector", "reciprocal", out=rs[:], in_=rs[:])
            for hh in range(4):
                for vc in range(2):
                    idx = hh * 2 + vc
                    sl = slice(idx * 128, (idx + 1) * 128)
                    P.do("vector", "scalar_tensor_tensor", out=tmpo[:, sl], in0=oT[:, sl], scalar=hn[:, vc:vc + 1],
                         in1=rs[:, hh * 128:(hh + 1) * 128], op0=ALU.mult, op1=ALU.mult)
            P.do("gpsimd", "tensor_tensor", out=ogT[:], in0=tmpo[:], in1=zs[:], op=ALU.mult)
            bY = (bZ0, bZ1)
            for i in range(2):
                for fc in range(8):
                    P.do("tensor", "matmul", out=bY[i][:], lhsT=ogT[:, fc * 128:(fc + 1) * 128],
                         rhs=Wout[:, fc, i * 512:(i + 1) * 512], start=(fc == 0), stop=(fc == 7))
            P.do("scalar", "activation", out=sq[:, 0:512], in_=bZ0[:], func=AF.Square, accum_out=ss[:, 1:2])
            P.do("scalar", "activation", out=sq[:, 512:1024], in_=bZ1[:], func=AF.Square, accum_out=ss[:, 2:3])
            P.do("vector", "tensor_tensor", out=ss[:, 3:4], in0=ss[:, 1:2], in1=ss[:, 2:3], op=ALU.add)
            P.do("vector", "tensor_scalar", out=rstd[:, 1:2], in0=ss[:, 3:4], scalar1=1.0 / D, scalar2=EPS,
                 op0=ALU.mult, op1=ALU.add)
            P.do("scalar", "activation", out=rstd[:, 1:2], in_=rstd[:, 1:2], func=AF.Sqrt)
            P.do("vector", "reciprocal", out=rstd[:, 1:2], in_=rstd[:, 1:2])
            for i in range(2):
                P.do("vector", "scalar_tensor_tensor", out=t1[:, i * 512:(i + 1) * 512], in0=bY[i][:],
                     scalar=rstd[:, 1:2], in1=gpost[:, i * 512:(i + 1) * 512], op0=ALU.mult, op1=ALU.mult)
            x_o = xo[t % 2]
            P.do("gpsimd", "tensor_tensor", out=x_o[:], in0=t1[:], in1=x_t[:], op=ALU.add)
            P.dma("sync", x_out[t * 128:(t + 1) * 128, :], x_o[:])
        P.flush()


S = 4096
D = 1024
NT = S // 128
EPS = 1e-6
NEGM = -30000.0
TWO_PI = 2.0 * math.pi


def _front(P, t, x_in, xt, sq, ss, rstd, hb, hT, pTb, ident, ntl):
    x_t = xt[t % 2]
    if t == 0:
        P.dma("sync", x_t[:], x_in[0:128, :])
    if t + 1 < ntl:
        P.dma("sync", xt[(t + 1) % 2][:], x_in[(t + 1) * 128:(t + 2) * 128, :])
    P.do("scalar", "activation", out=sq[:], in_=x_t[:], func=AF.Square, accum_out=ss[:, 0:1])
    P.do("vector", "tensor_scalar", out=rstd[:, 0:1], in0=ss[:, 0:1], scalar1=1.0 / D, scalar2=EPS,
         op0=ALU.mult, op1=ALU.add)
    P.do("scalar", "activation", out=rstd[:, 0:1], in_=rstd[:, 0:1], func=AF.Sqrt)
    P.do("vector", "reciprocal", out=rstd[:, 0:1], in_=rstd[:, 0:1])
    P.do("vector", "tensor_scalar", out=hb[:], in0=x_t[:], scalar1=rstd[:, 0:1], scalar2=None, op0=ALU.mult)
    for c in range(8):
        P.do("tensor", "transpose", out=pTb[:, c * 128:(c + 1) * 128], in_=hb[:, c * 128:(c + 1) * 128],
             identity=ident[:])
    P.do("vector", "tensor_copy", out=hT[:], in_=pTb)
    return x_t


def _rope(P, src, nh, cosb, sinb, tmp, dst):
    s3 = src.rearrange("p (h d) -> p h d", d=64)
    d3 = dst.rearrange("p (h d) -> p h d", d=64)
    x1, x2 = s3[:, :, 0:32], s3[:, :, 32:64]
    cb = V(cosb.buf, cosb.ap.unsqueeze(1).to_broadcast([128, nh, 32]))
    sb_ = V(sinb.buf, sinb.ap.unsqueeze(1).to_broadcast([128, nh, 32]))
    ta = tmp[0][:, 0:nh * 32].rearrange("p (h d) -> p h d", d=32)
    tb = tmp[1][:, 0:nh * 32].rearrange("p (h d) -> p h d", d=32)
    P.do("vector", "tensor_tensor", out=ta, in0=x1, in1=cb, op=ALU.mult)
    P.do("vector", "tensor_tensor", out=tb, in0=x2, in1=sb_, op=ALU.mult)
    P.do("gpsimd", "tensor_tensor", out=d3[:, :, 0:32], in0=ta, in1=tb, op=ALU.subtract)
    tc_ = tmp[2][:, 0:nh * 32].rearrange("p (h d) -> p h d", d=32)
    td = tmp[3][:, 0:nh * 32].rearrange("p (h d) -> p h d", d=32)
    P.do("vector", "tensor_tensor", out=tc_, in0=x2, in1=cb, op=ALU.mult)
    P.do("vector", "tensor_tensor", out=td, in0=x1, in1=sb_, op=ALU.mult)
    P.do("gpsimd", "tensor_tensor", out=d3[:, :, 32:64], in0=tc_, in1=td, op=ALU.add)


def nsa_layer(P, nc, x_in, x_out, pos, pre_g, post_g, w_in, b_gate, pe_k, pe_v, ck_w1, ck_w2, cv_w1, cv_w2,
              w_out, ntiles=NT):
    nblk = 8 * ntiles - 1
    with ExitStack() as sta:
        ident, identf = make_ident(P, sta, "n")
        kcmpT = P.sb(sta, "n_kcmpT", [64, 4, 256], BF16)
        vcmp = P.sb(sta, "n_vcmp", [128, 2, 4, 129], BF16)
        costab = P.sb(sta, "n_cos", [128, NT, 32], F32)
        sintab = P.sb(sta, "n_sin", [128, NT, 32], F32)
        gpre = P.sb(sta, "n_gpre", [128, 8], F32)
        xt = [P.sb(sta, f"n_xt{i}", [128, 1024], F32) for i in range(2)]
        sq = P.sb(sta, "n_sq", [128, 1024], F32)
        ss = P.sb(sta, "n_ss", [128, 4], F32)
        rstd = P.sb(sta, "n_rstd", [128, 2], F32)
        hb = P.sb(sta, "n_hb", [128, 1024], BF16)
        hT = P.sb(sta, "n_hT", [128, 1024], BF16)
        rtmp = [P.sb(sta, f"n_rtmp{i}", [128, 512], F32) for i in range(4)]
        B = [P.ps(sta, f"n_bank{i}", [128, 512], F32) for i in range(8)]
        pTb = B[0][:].bitcast(BF16)

        P.dma("sync", gpre[:], pre_g.rearrange("(c p o) -> p c o", p=128, o=1), allow_slow_non_contiguous=True)

        with ExitStack() as stc:
            posi = P.sb(stc, "n_posi", [128, NT], I32)
            posf = P.sb(stc, "n_posf", [128, NT], F32)
            invf = P.sb(stc, "n_invf", [128, 32], F32)
            ang = P.sb(stc, "n_ang", [128, NT, 32], F32)
            kf = P.sb(stc, "n_kf", [128, NT, 32], F32)
            ki = P.sb(stc, "n_ki", [128, NT, 32], I32)
            mk = P.sb(stc, "n_mk", [128, NT, 32], F32)
            P.dma("sync", posi[:], pos.rearrange("(t p o) -> p t o", p=128, o=1), allow_slow_non_contiguous=True)
            P.do("vector", "tensor_copy", out=posf[:], in_=posi[:])
            P.do("gpsimd", "iota", out=invf[:], pattern=[[1, 32]], base=0, channel_multiplier=0,
                 allow_small_or_imprecise_dtypes=True)
            P.do("scalar", "activation", out=invf[:], in_=invf[:], func=AF.Exp, scale=-math.log(10000.0) / 32.0)
            for t in range(NT):
                P.do("vector", "tensor_scalar", out=ang[:, t, :], in0=invf[:], scalar1=posf[:, t:t + 1], scalar2=None,
                     op0=ALU.mult)

            def reduce_to_pi(dst, shift):
                P.do("vector", "tensor_scalar", out=kf[:], in0=ang[:], scalar1=shift, scalar2=1.0 / TWO_PI,
                     op0=ALU.add, op1=ALU.mult)
                P.do("vector", "tensor_copy", out=ki[:], in_=kf[:])
                P.do("vector", "tensor_copy", out=kf[:], in_=ki[:])
                P.do("vector", "scalar_tensor_tensor", out=kf[:], in0=kf[:], scalar=-TWO_PI, in1=ang[:],
                     op0=ALU.mult, op1=ALU.add)
                P.do("vector", "tensor_scalar", out=kf[:], in0=kf[:], scalar1=shift, scalar2=None, op0=ALU.add)
                P.do("vector", "tensor_scalar", out=mk[:], in0=kf[:], scalar1=math.pi, scalar2=-TWO_PI,
                     op0=ALU.is_gt, op1=ALU.mult)
                P.do("vector", "tensor_tensor", out=kf[:], in0=kf[:], in1=mk[:], op=ALU.add)
                P.do("vector", "tensor_scalar", out=mk[:], in0=kf[:], scalar1=-math.pi, scalar2=TWO_PI,
                     op0=ALU.is_lt, op1=ALU.mult)
                P.do("vector", "tensor_tensor", out=kf[:], in0=kf[:], in1=mk[:], op=ALU.add)
                P.do("vector", "tensor_scalar", out=kf[:], in0=kf[:], scalar1=math.pi, scalar2=-math.pi,
                     op0=ALU.min, op1=ALU.max)
                P.do("scalar", "activation", out=dst[:], in_=kf[:], func=AF.Sin)

            reduce_to_pi(sintab, 0.0)
            reduce_to_pi(costab, math.pi / 2.0)
            P.flush()

        with ExitStack() as st0:
            WinA = P.sb(st0, "n0_WinA", [128, 8, 512], BF16)
            stg = [P.sb(st0, f"n0_stg{i}", [128, 2048], F32) for i in range(2)]
            w1 = [P.sb(st0, f"n0_w1_{i}", [64, 32, 256], BF16) for i in range(2)]
            w2 = [P.sb(st0, f"n0_w2_{i}", [128, 2, 64], BF16) for i in range(2)]
            w2f = P.sb(st0, "n0_w2f", [128, 2, 64], F32)
            peTf = P.sb(st0, "n0_peTf", [64, 32], F32)
            peT = [P.sb(st0, f"n0_peT{i}", [64, 32], BF16) for i in range(2)]
            bias = P.sb(st0, "n0_bias", [128, 4], F32)
            kcT = P.sb(st0, "n0_kcT", [64, 4, S], BF16)
            vcT = P.sb(st0, "n0_vcT", [64, 4, S], BF16)
            kcr = P.sb(st0, "n0_kcr", [128, 256], BF16)
            vcb = P.sb(st0, "n0_vcb", [128, 256], BF16)
            hidT = P.sb(st0, "n0_hidT", [128, 2, 256], BF16)
            ovf = P.sb(st0, "n0_ovf", [128, 2, 64], F32)

            for c in range(8):
                sg = stg[c % 2]
                P.dma("sync" if c % 2 == 0 else "gpsimd", sg[:, 0:512], w_in[c * 128:(c + 1) * 128, 1024:1536])
                P.do("vector" if c % 2 == 0 else "gpsimd", "tensor_scalar", out=WinA[:, c, :], in0=sg[:, 0:512],
                     scalar1=gpre[:, c:c + 1], scalar2=None, op0=ALU.mult)
            n = 0
            for kv, (wsrc, w2src, pesrc) in enumerate(((ck_w1, ck_w2, pe_k), (cv_w1, cv_w2, pe_v))):
                w1v = wsrc.rearrange("(l d) n -> d l n", d=64)
                for q4 in range(4):
                    sg = stg[n % 2]
                    P.dma("sync" if n % 2 == 0 else "gpsimd", sg[0:64, :].rearrange("p (l n) -> p l n", n=256),
                          w1v[:, q4 * 8:(q4 + 1) * 8, :])
                    P.do("vector" if n % 2 == 0 else "gpsimd", "tensor_copy",
                         out=w1[kv][:, q4 * 8:(q4 + 1) * 8, :], in_=sg[0:64, :].rearrange("p (l n) -> p l n", n=256))
                    n += 1
                P.dma("sync", w2f[:], w2src.rearrange("(c p) n -> p c n", p=128))
                P.do("vector", "tensor_copy", out=w2[kv][:], in_=w2f[:])
                P.dma("sync", peTf[:], pesrc.rearrange("l d -> d l"), allow_slow_non_contiguous=True)
                P.do("vector", "tensor_copy", out=peT[kv][:], in_=peTf[:])
                for hc in range(2):
                    for l in range(32):
                        P.do("tensor", "matmul", out=B[1][:, kv * 2 + hc:kv * 2 + hc + 1],
                             lhsT=w1[kv][:, l, hc * 128:(hc + 1) * 128], rhs=peT[kv][:, l:l + 1],
                             start=(l == 0), stop=(l == 31))
            P.do("vector", "tensor_copy", out=bias[:], in_=B[1][:, 0:4])

            P.do("gpsimd", "memset", ap=vcmp[:], constant=0.0, w=[vcmp])
            P.do("gpsimd", "memset", ap=vcmp[:, :, :, 64:65], constant=1.0, w=[vcmp])
            P.do("gpsimd", "memset", ap=ovf[:], constant=1.0, w=[ovf])
            for ch in range(2):
                P.do("gpsimd", "affine_select", out=ovf[:, ch, :], in_=ovf[:, ch, :], pattern=[[-4, 64]],
                     compare_op=ALU.is_ge, fill=0.0, base=ch * 128 + 1, channel_multiplier=1)
                P.do("gpsimd", "affine_select", out=ovf[:, ch, :], in_=ovf[:, ch, :], pattern=[[4, 64]],
                     compare_op=ALU.is_ge, fill=0.0, base=3 - ch * 128, channel_multiplier=-1)
                for g in range(4):
                    P.do("vector", "tensor_copy", out=vcmp[:, ch, g, 65:129], in_=ovf[:, ch, :])

            for t in range(ntiles):
                _front(P, t, x_in, xt, sq, ss, rstd, hb, hT, pTb, ident, ntiles)
                for c in range(8):
                    P.do("tensor", "matmul", out=B[1][:], lhsT=hT[:, c * 128:(c + 1) * 128], rhs=WinA[:, c, :],
                         start=(c == 0), stop=(c == 7))
                _rope(P, B[1][:, 0:256], 4, costab[:, t, :], sintab[:, t, :], rtmp, kcr[:])
                P.do("scalar", "copy", out=vcb[:], in_=B[1][:, 256:512])
                for g in range(4):
                    P.do("tensor", "transpose", out=pTb[0:64, g * 128:(g + 1) * 128], in_=kcr[:, g * 64:(g + 1) * 64],
                         identity=ident[:])
                for g in range(4):
                    P.do("tensor", "transpose", out=pTb[0:64, 512 + g * 128:512 + (g + 1) * 128],
                         in_=vcb[:, g * 64:(g + 1) * 64], identity=ident[:])
                P.do("vector", "tensor_copy", out=kcT[:, :, t * 128:(t + 1) * 128],
                     in_=pTb[0:64, 0:512].rearrange("p (g t) -> p g t", g=4))
                P.do("scalar", "copy", out=vcT[:, :, t * 128:(t + 1) * 128],
                     in_=pTb[0:64, 512:1024].rearrange("p (g t) -> p g t", g=4))

            for kv, srcT in enumerate((kcT, vcT)):
                for g in range(4):
                    for hc in range(2):
                        bk = B[2 + hc]
                        for l in range(32):
                            P.do("tensor", "matmul", out=bk[:, 0:nblk], lhsT=w1[kv][:, l, hc * 128:(hc + 1) * 128],
                                 rhs=srcT[:, g, l:l + 16 * (nblk - 1) + 1:16], start=(l == 0), stop=(l == 31))
                        P.do("scalar", "activation", out=hidT[:, hc, 0:nblk], in_=bk[:, 0:nblk], func=AF.Silu,
                             bias=bias[:, kv * 2 + hc:kv * 2 + hc + 1], scale=1.0)
                    if kv == 0:
                        for hc in range(2):
                            P.do("tensor", "matmul", out=B[4][0:64, 0:nblk], lhsT=w2[0][:, hc, :],
                                 rhs=hidT[:, hc, 0:nblk], start=(hc == 0), stop=(hc == 1))
                        P.do("vector", "tensor_copy", out=kcmpT[:, g, 0:nblk], in_=B[4][0:64, 0:nblk])
                    else:
                        for ch in range(2):
                            rows = min(128, nblk - ch * 128)
                            if rows <= 0:
                                continue
                            for hc in range(2):
                                P.do("tensor", "matmul", out=B[4][0:rows, ch * 64:(ch + 1) * 64],
                                     lhsT=hidT[:, hc, ch * 128:ch * 128 + rows], rhs=w2[1][:, hc, :],
                                     start=(hc == 0), stop=(hc == 1))
                            P.do("vector", "tensor_copy", out=vcmp[0:rows, ch, g, 0:64],
                                 in_=B[4][0:rows, ch * 64:(ch + 1) * 64])
            P.flush()

        with ExitStack() as st1:
            Win = P.sb(st1, "n1_Win", [128, 8, 3632], BF16)
            Wout = P.sb(st1, "n1_Wout", [128, 8, 1024], BF16)
            gpost = P.sb(st1, "n1_gpost", [128, 1024], F32)
            bgate = P.sb(st1, "n1_bgate", [128, 48], F32)
            with ExitStack() as stl:
                stage = [P.sb(stl, f"n1_stage{i}", [128, 3632], F32) for i in range(2)]
                for c in range(8):
                    sg = stage[c % 2]
                    P.dma("sync" if c % 2 == 0 else "gpsimd", sg[:], w_in[c * 128:(c + 1) * 128, :])
                    P.do("vector" if c % 2 == 0 else "gpsimd", "tensor_scalar", out=Win[:, c, :], in0=sg[:],
                         scalar1=gpre[:, c:c + 1], scalar2=None, op0=ALU.mult)
                for c in range(8):
                    sg = stage[c % 2]
                    P.dma("sync" if c % 2 == 0 else "gpsimd", sg[:, 0:1024], w_out[c * 128:(c + 1) * 128, :])
                    P.do("vector" if c % 2 == 0 else "gpsimd", "tensor_copy", out=Wout[:, c, :], in_=sg[:, 0:1024])
                P.dma("sync", gpost[:], post_g.rearrange("(o n) -> o n", o=1).to_broadcast([128, 1024]))
                P.dma("sync", bgate[:], b_gate.rearrange("(o n) -> o n", o=1).to_broadcast([128, 48]))
                P.flush()

            ksT = P.sb(st1, "n1_ksT", [128, 4, S], BF16)
            vsa = P.sb(st1, "n1_vsa", [128, NT, 4, 65], BF16)
            kwT = P.sb(st1, "n1_kwT", [64, 4, 5 * 128], BF16)
            vwa = P.sb(st1, "n1_vwa", [128, 5, 4, 65], BF16)
            qaug = P.sb(st1, "n1_qaug", [128, 16, 128], BF16)
            qmask = P.track(qaug.h[64:128, :, :], "n1_qmask")
            qr = P.sb(st1, "n1_qr", [128, 1024], BF16)
            ksr = P.sb(st1, "n1_ksr", [128, 256], BF16)
            kwr = P.sb(st1, "n1_kwr", [128, 256], BF16)
            zs = P.sb(st1, "n1_zs", [128, 1024], BF16)
            glx = P.sb(st1, "n1_glx", [128, 48], F32)
            gt = P.sb(st1, "n1_gt", [128, 48], F32)
            PT = [P.sb(st1, f"n1_PT{i}", [128, 512], BF16) for i in range(3)]
            lrec = P.sb(st1, "n1_lrec", [128, 4], F32)
            wsc = P.sb(st1, "n1_wsc", [128, 4], F32)
            imp = P.sb(st1, "n1_imp", [128, 64], F32)
            imp2 = P.sb(st1, "n1_imp2", [128, 64], F32)
            imp3 = P.sb(st1, "n1_imp3", [128, 64], F32)
            m8 = P.sb(st1, "n1_m8", [128, 16], F32)
            M1 = P.sb(st1, "n1_M1", [128, 64], F32)
            Cm = P.sb(st1, "n1_Cm", [128, 64], F32)
            selm = P.sb(st1, "n1_selm", [128, 128], BF16)
            oacc = P.sb(st1, "n1_oacc", [128, 1024], F32)
            otmp = P.sb(st1, "n1_otmp", [128, 256], F32)
            og = P.sb(st1, "n1_og", [128, 1024], BF16)
            ogT = P.sb(st1, "n1_ogT", [128, 1024], BF16)
            t1 = P.sb(st1, "n1_t1", [128, 1024], F32)
            xo = [P.sb(st1, f"n1_xo{i}", [128, 1024], F32) for i in range(2)]

            P.do("gpsimd", "memset", ap=ksT[64:128, :, :], constant=1.0, w=[ksT])
            for g in range(4):
                P.do("gpsimd", "affine_select", out=ksT[64:128, g, :], in_=ksT[64:128, g, :], pattern=[[1, S]],
                     compare_op=ALU.is_ge, fill=0.0, base=0, channel_multiplier=-64)
                P.do("gpsimd", "affine_select", out=ksT[64:128, g, :], in_=ksT[64:128, g, :], pattern=[[-1, S]],
                     compare_op=ALU.is_ge, fill=0.0, base=63, channel_multiplier=64)
            P.do("gpsimd", "memset", ap=vsa[:, :, :, 64:65], constant=1.0, w=[vsa])
            P.do("gpsimd", "memset", ap=vwa[:, :, :, 64:65], constant=1.0, w=[vwa])
            P.do("gpsimd", "memset", ap=selm[:], constant=0.0, w=[selm])

            npt = 0
            nsb = 0
            for T in range(ntiles):
                x_t = _front(P, T, x_in, xt, sq, ss, rstd, hb, hT, pTb, ident, ntiles)

                def hTc(c):
                    return hT[:, c * 128:(c + 1) * 128]

                for bk, lo, wd in ((B[1], 0, 512), (B[2], 512, 512), (B[3], 1536, 512), (B[4], 2048, 512), (B[5], 2560, 48)):
                    for c in range(8):
                        P.do("tensor", "matmul", out=bk[:, 0:wd], lhsT=hTc(c), rhs=Win[:, c, lo:lo + wd],
                             start=(c == 0), stop=(c == 7))
                cb, sb_ = costab[:, T, :], sintab[:, T, :]
                _rope(P, B[1][:], 8, cb, sb_, rtmp, qr[:, 0:512])
                _rope(P, B[2][:], 8, cb, sb_, rtmp, qr[:, 512:1024])
                _rope(P, B[3][:, 0:256], 4, cb, sb_, rtmp, ksr[:])
                _rope(P, B[4][:, 0:256], 4, cb, sb_, rtmp, kwr[:])
                P.do("scalar", "copy", out=vsa[:, T, :, 0:64], in_=B[3][:, 256:512].rearrange("p (g d) -> p g d", d=64))
                P.do("scalar", "copy", out=vwa[:, T % 5, :, 0:64], in_=B[4][:, 256:512].rearrange("p (g d) -> p g d", d=64))
                P.do("vector", "tensor_tensor", out=glx[:], in0=B[5][:, 0:48], in1=bgate[:], op=ALU.add)
                P.do("scalar", "activation", out=glx[:], in_=glx[:], func=AF.Exp, scale=-1.0)
                P.do("vector", "tensor_scalar", out=glx[:], in0=glx[:], scalar1=1.0, scalar2=None, op0=ALU.add)
                P.do("vector", "reciprocal", out=gt[:], in_=glx[:])
                for i, bk in enumerate((B[1], B[2])):
                    for c in range(8):
                        P.do("tensor", "matmul", out=bk[:], lhsT=hTc(c), rhs=Win[:, c, 2608 + i * 512:2608 + (i + 1) * 512],
                             start=(c == 0), stop=(c == 7))
                for half in range(2):
                    for hh in range(8):
                        h = half * 8 + hh
                        P.do("tensor", "transpose", out=pTb[0:64, hh * 128:(hh + 1) * 128],
                             in_=qr[:, h * 64:(h + 1) * 64], identity=ident[:])
                    P.do("scalar", "mul", out=qaug[0:64, half * 8:(half + 1) * 8, :],
                         in_=pTb[0:64, :].rearrange("p (h t) -> p h t", h=8), mul=0.125)
                for g in range(4):
                    P.do("tensor", "transpose", out=pTb[0:64, g * 128:(g + 1) * 128], in_=ksr[:, g * 64:(g + 1) * 64],
                         identity=ident[:])
                for g in range(4):
                    P.do("tensor", "transpose", out=pTb[0:64, 512 + g * 128:512 + (g + 1) * 128],
                         in_=kwr[:, g * 64:(g + 1) * 64], identity=ident[:])
                P.do("vector", "tensor_copy", out=ksT[0:64, :, T * 128:(T + 1) * 128],
                     in_=pTb[0:64, 0:512].rearrange("p (g t) -> p g t", g=4))
                P.do("vector", "tensor_copy", out=kwT[:, :, (T % 5) * 128:(T % 5 + 1) * 128],
                     in_=pTb[0:64, 512:1024].rearrange("p (g t) -> p g t", g=4))
                P.do("scalar", "activation", out=zs[:, 0:512], in_=B[1][:], func=AF.Silu)
                P.do("scalar", "activation", out=zs[:, 512:1024], in_=B[2][:], func=AF.Silu)

                P.do("gpsimd", "memset", ap=M1[:], constant=0.0, w=[M1])
                P.do("gpsimd", "memset", ap=Cm[:], constant=0.0, w=[Cm])
                for half in range(2):
                    cur = 2 * T + half
                    rs_ = slice(half * 64, (half + 1) * 64)
                    if cur - 2 >= 1:
                        P.do("gpsimd", "memset", ap=M1[rs_, 1:cur - 1], constant=1.0, w=[M1])
                    if cur + 1 < 64:
                        P.do("gpsimd", "memset", ap=Cm[rs_, cur + 1:64], constant=-1e30, w=[Cm])
                    P.do("gpsimd", "memset", ap=Cm[rs_, max(cur - 1, 0):cur + 1], constant=1e9, w=[Cm])
                P.do("gpsimd", "memset", ap=Cm[:, 0:1], constant=1e9, w=[Cm])

                nvalid = min(8 * T + 7, nblk)
                for g in range(4):
                    qg = qaug[0:64, 4 * g:4 * g + 4, :]
                    qga = qaug[:, 4 * g:4 * g + 4, :]
                    nch = (nvalid + 127) // 128
                    for ch in range(nch):
                        rows = min(128, nvalid - ch * 128)
                        sbk = B[3 + nsb % 2]; nsb += 1
                        pt = PT[npt % 3]; npt += 1
                        P.do("tensor", "matmul", out=sbk[0:rows, :], lhsT=kcmpT[:, g, ch * 128:ch * 128 + rows], rhs=qg,
                             start=True, stop=True)
                        P.do("scalar", "activation", out=pt[0:rows, :], in_=sbk[0:rows, :], func=AF.Exp)
                        P.do("gpsimd", "affine_select", out=pt[0:rows, :], in_=pt[0:rows, :], pattern=[[0, 4], [1, 128]],
                             compare_op=ALU.is_ge, fill=0.0, base=128 * T - 2048 * ch - 31, channel_multiplier=-16)
                        for h in range(4):
                            P.do("tensor", "matmul", out=B[1 + h // 2][:, (h % 2) * 129:(h % 2) * 129 + 129],
                                 lhsT=pt[0:rows, h * 128:(h + 1) * 128], rhs=vcmp[0:rows, ch, g, :],
                                 start=(ch == 0 and h % 2 == 0), stop=(ch == nch - 1 and h % 2 == 1))
                    for b2 in range(2):
                        P.do("vector", "tensor_scalar", out=lrec[:, 2 * b2:2 * b2 + 2], in0=B[1 + b2][:, 64:258:129],
                             scalar1=1e-20, scalar2=None, op0=ALU.max)
                    P.do("vector", "reciprocal", out=lrec[:], in_=lrec[:])
                    P.do("vector", "tensor_tensor", out=wsc[:], in0=lrec[:],
                         in1=gt[:].rearrange("p (h c) -> p h c", c=3)[:, 4 * g:4 * g + 4, 0], op=ALU.mult)
                    for h in range(4):
                        bo = B[1 + h // 2]
                        off = (h % 2) * 129
                        H = 4 * g + h
                        if h % 2 == 0:
                            P.do("vector", "tensor_tensor",
                                 out=oacc[:, H * 64:(H + 2) * 64].rearrange("p (h d) -> p h d", d=64),
                                 in0=bo[:, 0:258].rearrange("p (h d) -> p h d", d=129)[:, :, 0:64],
                                 in1=V(wsc, wsc[:, h:h + 2].ap.unsqueeze(2).to_broadcast([128, 2, 64])), op=ALU.mult)
                        if h == 0:
                            P.do("vector", "tensor_scalar", out=imp[:], in0=bo[:, off + 65:off + 129],
                                 scalar1=lrec[:, h:h + 1], scalar2=None, op0=ALU.mult)
                        else:
                            P.do("vector", "scalar_tensor_tensor", out=imp[:], in0=bo[:, off + 65:off + 129],
                                 scalar=lrec[:, h:h + 1], in1=imp[:], op0=ALU.mult, op1=ALU.add)
                    P.do("vector", "tensor_tensor", out=imp2[:], in0=imp[:], in1=M1[:], op=ALU.mult)
                    P.do("vector", "tensor_tensor", out=imp2[:], in0=imp2[:], in1=Cm[:], op=ALU.add)
                    P.do("vector", "max", out=m8[:, 0:8], in_=imp2[:])
                    P.do("vector", "match_replace", out=imp3[:], in_to_replace=m8[:, 0:8], in_values=imp2[:],
                         imm_value=-3e38)
                    P.do("vector", "max", out=m8[:, 8:16], in_=imp3[:])
                    P.do("vector", "tensor_scalar", out=selm[:, 64:128], in0=imp2[:], scalar1=m8[:, 15:16], scalar2=NEGM,
                         op0=ALU.is_lt, op1=ALU.mult)
                    P.do("tensor", "transpose", out=pTb[:, 0:128], in_=selm[:], identity=ident[:])
                    P.do("vector", "tensor_copy", out=qmask[:, 4 * g:4 * g + 4, :],
                         in_=V(B[0], pTb.ap[64:128, 0:128].unsqueeze(1).to_broadcast([64, 4, 128])))
                    kts = list(range(max(0, T - 4), T + 1))
                    for kt in kts:
                        sbk = B[3 + nsb % 2]; nsb += 1
                        pt = PT[npt % 3]; npt += 1
                        sl = kt % 5
                        P.do("tensor", "matmul", out=sbk[:], lhsT=kwT[:, g, sl * 128:(sl + 1) * 128], rhs=qg,
                             start=True, stop=True)
                        P.do("scalar", "activation", out=pt[:], in_=sbk[:], func=AF.Exp)
                        if kt == T:
                            P.do("gpsimd", "affine_select", out=pt[:], in_=pt[:], pattern=[[0, 4], [1, 128]],
                                 compare_op=ALU.is_ge, fill=0.0, base=0, channel_multiplier=-1)
                        if kt == T - 4:
                            P.do("gpsimd", "affine_select", out=pt[:], in_=pt[:], pattern=[[0, 4], [-1, 128]],
                                 compare_op=ALU.is_ge, fill=0.0, base=-1, channel_multiplier=1)
                        for h in range(4):
                            P.do("tensor", "matmul", out=B[6][:, h * 65:h * 65 + 65], lhsT=pt[:, h * 128:(h + 1) * 128],
                                 rhs=vwa[:, sl, g, :], start=(kt == kts[0] and h == 0), stop=(kt == T and h == 3))
                    _combine(P, B[6], g, 2, lrec, wsc, gt, oacc, otmp)
                    for kt in range(T + 1):
                        sbk = B[3 + nsb % 2]; nsb += 1
                        pt = PT[npt % 3]; npt += 1
                        P.do("tensor", "matmul", out=sbk[:], lhsT=ksT[:, g, kt * 128:(kt + 1) * 128], rhs=qga,
                             start=True, stop=True, r=[qmask])
                        P.do("scalar", "activation", out=pt[:], in_=sbk[:], func=AF.Exp)
                        if kt == T:
                            P.do("gpsimd", "affine_select", out=pt[:], in_=pt[:], pattern=[[0, 4], [1, 128]],
                                 compare_op=ALU.is_ge, fill=0.0, base=0, channel_multiplier=-1)
                        for h in range(4):
                            P.do("tensor", "matmul", out=B[5][:, h * 65:h * 65 + 65], lhsT=pt[:, h * 128:(h + 1) * 128],
                                 rhs=vsa[:, kt, g, :], start=(kt == 0 and h == 0), stop=(kt == T and h == 3))
                    _combine(P, B[5], g, 1, lrec, wsc, gt, oacc, otmp)

                P.do("gpsimd", "tensor_tensor", out=og[:], in0=oacc[:], in1=zs[:], op=ALU.mult)
                for c in range(8):
                    P.do("tensor", "transpose", out=pTb[:, c * 128:(c + 1) * 128], in_=og[:, c * 128:(c + 1) * 128],
                         identity=ident[:])
                P.do("vector", "tensor_copy", out=ogT[:], in_=pTb)
                for i in range(2):
                    for fc in range(8):
                        P.do("tensor", "matmul", out=B[1 + i][:], lhsT=ogT[:, fc * 128:(fc + 1) * 128],
                             rhs=Wout[:, fc, i * 512:(i + 1) * 512], start=(fc == 0), stop=(fc == 7))
                P.do("scalar", "activation", out=sq[:, 0:512], in_=B[1][:], func=AF.Square, accum_out=ss[:, 1:2])
                P.do("scalar", "activation", out=sq[:, 512:1024], in_=B[2][:], func=AF.Square, accum_out=ss[:, 2:3])
                P.do("vector", "tensor_tensor", out=ss[:, 3:4], in0=ss[:, 1:2], in1=ss[:, 2:3], op=ALU.add)
                P.do("vector", "tensor_scalar", out=rstd[:, 1:2], in0=ss[:, 3:4], scalar1=1.0 / D, scalar2=EPS,
                     op0=ALU.mult, op1=ALU.add)
                P.do("scalar", "activation", out=rstd[:, 1:2], in_=rstd[:, 1:2], func=AF.Sqrt)
                P.do("vector", "reciprocal", out=rstd[:, 1:2], in_=rstd[:, 1:2])
                for i in range(2):
                    P.do("vector", "scalar_tensor_tensor", out=t1[:, i * 512:(i + 1) * 512], in0=B[1 + i][:],
                         scalar=rstd[:, 1:2], in1=gpost[:, i * 512:(i + 1) * 512], op0=ALU.mult, op1=ALU.mult)
                x_o = xo[T % 2]
                P.do("gpsimd", "tensor_tensor", out=x_o[:], in0=t1[:], in1=x_t[:], op=ALU.add)
                P.dma("sync", x_out[T * 128:(T + 1) * 128, :], x_o[:])
            P.flush()


def _combine(P, bo, g, c, lrec, wsc, gt, oacc, otmp):
    P.do("vector", "tensor_scalar", out=lrec[:], in0=bo[:, 64:260:65], scalar1=1e-20, scalar2=None, op0=ALU.max)
    P.do("vector", "reciprocal", out=lrec[:], in_=lrec[:])
    P.do("vector", "tensor_tensor", out=wsc[:], in0=lrec[:],
         in1=gt[:].rearrange("p (h c) -> p h c", c=3)[:, 4 * g:4 * g + 4, c], op=ALU.mult)
    bo3 = bo[:, 0:260].rearrange("p (h d) -> p h d", d=65)[:, :, 0:64]
    wbc = V(wsc, wsc[:].ap.unsqueeze(2).to_broadcast([128, 4, 64]))
    P.do("vector", "tensor_tensor", out=otmp[:].rearrange("p (h d) -> p h d", d=64), in0=bo3, in1=wbc, op=ALU.mult)
    P.do("gpsimd", "tensor_tensor", out=oacc[:, g * 256:(g + 1) * 256], in0=oacc[:, g * 256:(g + 1) * 256],
         in1=otmp[:], op=ALU.add)


FUSED = True


def _din(nc, name, shape, dt=F32):
    return nc.dram_tensor(name, list(shape), dt, kind="ExternalInput").ap()


def _gla_inputs(nc):
    return dict(pre=_din(nc, "g_pre", [1024]), post=_din(nc, "g_post", [1024]), w_in=_din(nc, "g_w_in", [1024, 3088]),
                w_up=_din(nc, "g_w_up", [16, 512]), b_gk=_din(nc, "g_b_gk", [512]), hnorm=_din(nc, "g_hnorm", [256]),
                w_out=_din(nc, "g_w_out", [1024, 1024]))


def _nsa_inputs(nc):
    return dict(pos=_din(nc, "n_pos", [4096], I32), pre=_din(nc, "n_pre", [1024]), post=_din(nc, "n_post", [1024]),
                w_in=_din(nc, "n_w_in", [1024, 3632]), b_gate=_din(nc, "n_b_gate", [48]),
                pe_k=_din(nc, "n_pe_k", [32, 64]), pe_v=_din(nc, "n_pe_v", [32, 64]),
                ck_w1=_din(nc, "n_ck_w1", [2048, 256]), ck_w2=_din(nc, "n_ck_w2", [256, 64]),
                cv_w1=_din(nc, "n_cv_w1", [2048, 256]), cv_w2=_din(nc, "n_cv_w2", [256, 64]),
                w_out=_din(nc, "n_w_out", [1024, 1024]))


def _emit_gla(P, nc, x, y, gi):
    gla_layer(P, nc, x, y, gi["pre"], gi["post"], gi["w_in"], gi["w_up"], gi["b_gk"], gi["hnorm"], gi["w_out"])


def _emit_nsa(P, nc, x, y, ni):
    nsa_layer(P, nc, x, y, ni["pos"], ni["pre"], ni["post"], ni["w_in"], ni["b_gate"], ni["pe_k"], ni["pe_v"],
              ni["ck_w1"], ni["ck_w2"], ni["cv_w1"], ni["cv_w2"], ni["w_out"])


def _gla_map(inp, b):
    return {"g_pre": inp["pre_norm"][0], "g_post": inp["post_norm"][0], "g_w_in": inp["gla_w_in"][0],
            "g_w_up": inp["gla_w_gk_up"][0], "g_b_gk": inp["gla_b_gk"][0], "g_hnorm": inp["gla_head_norm"][0],
            "g_w_out": inp["gla_w_out"][0]}


def _nsa_map(inp, b):
    return {"n_pos": np.ascontiguousarray(inp["positions"][b]), "n_pre": inp["pre_norm"][1], "n_post": inp["post_norm"][1],
            "n_w_in": inp["nsa_w_in"][0], "n_b_gate": inp["nsa_b_gate"][0], "n_pe_k": inp["nsa_pe_k"][0],
            "n_pe_v": inp["nsa_pe_v"][0], "n_ck_w1": inp["nsa_ck_w1"][0], "n_ck_w2": inp["nsa_ck_w2"][0],
            "n_cv_w1": inp["nsa_cv_w1"][0], "n_cv_w2": inp["nsa_cv_w2"][0], "n_w_out": inp["nsa_w_out"][0]}


def kernel(**inputs):
    inp = {k: np.ascontiguousarray(np.asarray(v)) for k, v in inputs.items()}
    n = 8
    cores = list(range(n))
    xs = [np.ascontiguousarray(inp["x"][b]) for b in range(n)]
    if FUSED:
        nc = bass.Bass("TRN2", target_bir_lowering=False)
        x = _din(nc, "x", [4096, 1024])
        y = nc.dram_tensor("y", [4096, 1024], F32, kind="ExternalOutput").ap()
        x1 = nc.dram_tensor("x1_stage", [4096, 1024], F32, kind="ExternalOutput").ap()
        gi = _gla_inputs(nc)
        ni = _nsa_inputs(nc)
        P = Prog(nc)
        _emit_gla(P, nc, x, x1, gi)
        _emit_nsa(P, nc, x1, y, ni)
        maps = [dict(x=xs[b], **_gla_map(inp, b), **_nsa_map(inp, b)) for b in range(n)]
        res = run_bass_kernel_spmd(nc, maps, core_ids=cores)
        return np.stack([np.asarray(res.results[b]["y"]) for b in range(n)], axis=0).astype(np.float32)
    nc1 = bass.Bass("TRN2", target_bir_lowering=False)
    x = _din(nc1, "x", [4096, 1024])
    y = nc1.dram_tensor("y", [4096, 1024], F32, kind="ExternalOutput").ap()
    gi = _gla_inputs(nc1)
    _emit_gla(Prog(nc1), nc1, x, y, gi)
    res1 = run_bass_kernel_spmd(nc1, [dict(x=xs[b], **_gla_map(inp, b)) for b in range(n)], core_ids=cores)
    x1s = [np.ascontiguousarray(np.asarray(res1.results[b]["y"])) for b in range(n)]
    nc2 = bass.Bass("TRN2", target_bir_lowering=False)
    x = _din(nc2, "x", [4096, 1024])
    y = nc2.dram_tensor("y", [4096, 1024], F32, kind="ExternalOutput").ap()
    ni = _nsa_inputs(nc2)
    _emit_nsa(Prog(nc2), nc2, x, y, ni)
    res2 = run_bass_kernel_spmd(nc2, [dict(x=x1s[b], **_nsa_map(inp, b)) for b in range(n)], core_ids=cores)
    return np.stack([np.asarray(res2.results[b]["y"]) for b in range(n)], axis=0).astype(np.float32)
```

```python
import math
from contextlib import ExitStack
import numpy as np
import concourse.bass as bass
import concourse.mybir as mybir
from concourse.bass_utils import run_bass_kernel_spmd

F32 = mybir.dt.float32
BF16 = mybir.dt.bfloat16
I32 = mybir.dt.int32
AF = mybir.ActivationFunctionType
ALU = mybir.AluOpType
AX = mybir.AxisListType

ENGS = ("tensor", "vector", "scalar", "gpsimd", "sync")
OUT_KEYS = ("out", "accum_out")


class Buf:
    def __init__(self, h, name):
        self.h = h
        self.name = name
        self.w = None
        self.r = []
        self.sem = None
        self.semcnt = 0

    def __getitem__(self, idx):
        return V(self, self.h[idx])


class V:
    def __init__(self, buf, ap):
        self.buf = buf
        self.ap = ap

    def __getitem__(self, idx):
        return V(self.buf, self.ap[idx])

    def rearrange(self, *a, **k):
        return V(self.buf, self.ap.rearrange(*a, **k))

    def bitcast(self, dt):
        return V(self.buf, self.ap.bitcast(dt))

    def to_broadcast(self, shape):
        return V(self.buf, self.ap.to_broadcast(shape))


class Op:
    __slots__ = ("eng", "fn", "deps", "signal", "idx", "dma_sem", "dma_cnt", "semval")

    def __init__(self, eng, fn):
        self.eng = eng
        self.fn = fn
        self.deps = []
        self.signal = False
        self.dma_sem = None
        self.dma_cnt = 0
        self.semval = 0


class Prog:
    def __init__(self, nc):
        self.nc = nc
        self.esem = {e: nc.alloc_semaphore(f"s_{e}") for e in ENGS}
        self.ecount = {e: 0 for e in ENGS}
        self.ops = {e: [] for e in ENGS}
        self.nbuf = 0
        self.dma_sems = []
        self.all_bufs = []

    def sb(self, stack, name, shape, dtype):
        h = stack.enter_context(self.nc.sbuf_tensor(name, list(shape), dtype))
        b = Buf(h, name)
        self.all_bufs.append(b)
        return b

    def ps(self, stack, name, shape, dtype=F32):
        h = stack.enter_context(self.nc.psum_tensor(name, list(shape), dtype))
        b = Buf(h, name)
        b.psum = True
        self.all_bufs.append(b)
        return b

    def track(self, h, name):
        b = Buf(h, name)
        self.all_bufs.append(b)
        return b

    def _collect(self, kw):
        reads, writes, real = [], [], {}
        for k, v in kw.items():
            if isinstance(v, V):
                (writes if k in OUT_KEYS else reads).append(v.buf)
                real[k] = v.ap
            else:
                real[k] = v
        return reads, writes, real

    def _add(self, eng, fn, reads, writes, is_dma=False, dma_buf=None, extra_reads=(), extra_writes=()):
        self.nops = getattr(self, "nops", 0) + 1
        if self.nops > getattr(self, "maxops", 1 << 60):
            return None
        op = Op(eng, fn)
        reads = list(reads) + list(extra_reads)
        writes = list(writes) + list(extra_writes)
        deps = []
        for b in reads:
            if b.w is not None:
                deps.append(("raw", b.w))
            if getattr(b, "psum", False):
                for t in b.r:
                    if t[0] == "c" and t[1] != eng:
                        deps.append(("rar", t))
        for b in writes:
            if b.w is not None:
                deps.append(("waw", b.w))
            for t in b.r:
                deps.append(("war", t))
        opidx = len(self.ops[eng])
        keep = []
        for kind, t in deps:
            if t[0] == "c":
                if t[1] == eng and not is_dma:
                    if kind != "raw" or eng == "tensor":
                        continue
            keep.append(t)
        op.deps = keep
        self.ops[eng].append(op)
        if is_dma:
            b = dma_buf
            if b.sem is None:
                b.sem = self.nc.alloc_semaphore(f"d_{b.name}")
                self.dma_sems.append(b)
            b.semcnt += 16
            op.dma_sem = b.sem
            ticket = ("d", b, b.semcnt)
        else:
            ticket = ("c", eng, opidx)
        for b in reads:
            b.r.append(ticket)
        for b in writes:
            b.w = ticket
            b.r = []
        return op

    def do(self, eng, name, r=(), w=(), **kw):
        reads, writes, real = self._collect(kw)
        self.last_desc = (eng, name)
        fn = lambda e, name=name, real=real: getattr(e, name)(**real)
        return self._add(eng, fn, reads, writes, extra_reads=r, extra_writes=w)

    def dma(self, eng, out, in_, **kw):
        reads, writes = [], []
        oa, ia = out, in_
        if isinstance(out, V):
            writes.append(out.buf)
            oa = out.ap
        if isinstance(in_, V):
            reads.append(in_.buf)
            ia = in_.ap
        dbuf = writes[0] if writes else reads[0]
        fn = lambda e, oa=oa, ia=ia, kw=kw: e.dma_start(out=oa, in_=ia, **kw)
        return self._add(eng, fn, reads, writes, is_dma=True, dma_buf=dbuf)

    def flush(self, final_wait_bufs=()):
        nc = self.nc
        for e in ENGS:
            for op in self.ops[e]:
                for t in op.deps:
                    if t[0] == "c":
                        self.ops[t[1]][t[2]].signal = True
        for e in ENGS:
            for op in reversed(self.ops[e]):
                if op.dma_sem is None:
                    op.signal = True
                    break
        for e in ENGS:
            c = self.ecount[e]
            for op in self.ops[e]:
                if op.signal:
                    c += 1
                op.semval = c
            self.ecount[e] = c
        ops = self.ops
        esem = self.esem
        ecount = dict(self.ecount)
        dsems = list(self.dma_sems)

        def emit(e):
            def body(eng):
                waited = {}
                for op in ops[e]:
                    need = {}
                    for t in op.deps:
                        if t[0] == "c":
                            key = ("c", t[1])
                            val = ops[t[1]][t[2]].semval
                            sem = esem[t[1]]
                        else:
                            key = ("d", id(t[1]))
                            val = t[2]
                            sem = t[1].sem
                        if waited.get(key, -1) >= val:
                            continue
                        if key not in need or need[key][1] < val:
                            need[key] = (sem, val)
                    for key, (sem, val) in need.items():
                        eng.wait_ge(sem, val)
                        waited[key] = val
                    ins = op.fn(eng)
                    if op.dma_sem is not None:
                        ins.then_inc(op.dma_sem, 16)
                    elif op.signal:
                        ins.then_inc(esem[e], 1)
                for e2 in ENGS:
                    if e2 != e and ecount[e2] > 0 and waited.get(("c", e2), -1) < ecount[e2]:
                        eng.wait_ge(esem[e2], ecount[e2])
                for b in dsems:
                    if waited.get(("d", id(b)), -1) < b.semcnt:
                        eng.wait_ge(b.sem, b.semcnt)
            return body

        with nc.Block() as blk:
            for e in ENGS:
                getattr(blk, e)(emit(e))
        self.ops = {e: [] for e in ENGS}
        for b in self.all_bufs:
            b.w = None
            b.r = []


S = 4096
D = 1024
NT = S // 128
EPS = 1e-6


def make_ident(P, st, pfx="g"):
    identf = P.sb(st, pfx + "_identf", [128, 128], F32)
    ident = P.sb(st, pfx + "_ident", [128, 128], BF16)
    P.do("gpsimd", "memset", ap=identf[:], constant=1.0, w=[identf])
    P.do("gpsimd", "affine_select", out=identf[:], in_=identf[:], pattern=[[-1, 128]],
         compare_op=ALU.is_equal, fill=0.0, base=0, channel_multiplier=1)
    P.do("vector", "tensor_copy", out=ident[:], in_=identf[:])
    return ident, identf


def gla_layer(P, nc, x_in, x_out, pre_g, post_g, w_in, w_up, b_gk, hnorm, w_out, ntiles=NT):
    with ExitStack() as st:
        ident, identf = make_ident(P, st)
        Win = P.sb(st, "g_Win", [128, 8, 3088], BF16)
        Wout = P.sb(st, "g_Wout", [128, 8, 1024], BF16)
        stage = [P.sb(st, f"g_stage{i}", [128, 3088], F32) for i in range(2)]
        gpre = P.sb(st, "g_gpre", [128, 8], F32)
        gpost = P.sb(st, "g_gpost", [128, 1024], F32)
        wupf = P.sb(st, "g_wupf", [16, 512], F32)
        wup = P.sb(st, "g_wup", [16, 512], BF16)
        negb = P.sb(st, "g_negb", [128, 4], F32)
        hn = P.sb(st, "g_hn", [128, 2], F32)
        onesb = P.sb(st, "g_ones", [128, 128], BF16)
        rmask = P.sb(st, "g_rmask", [128, 512], F32)
        bdmask = P.sb(st, "g_bdmask", [128, 512], F32)
        one1 = P.sb(st, "g_one1", [128, 1], F32)

        P.dma("sync", gpre[:], pre_g.rearrange("(c p o) -> p c o", p=128, o=1), allow_slow_non_contiguous=True)
        P.dma("sync", negb[:], b_gk.rearrange("(c p o) -> p c o", p=128, o=1), allow_slow_non_contiguous=True)
        P.dma("sync", hn[:], hnorm.rearrange("(c p o) -> p c o", p=128, o=1), allow_slow_non_contiguous=True)
        P.dma("sync", wupf[:], w_up)
        P.dma("sync", gpost[:], post_g.rearrange("(o n) -> o n", o=1).to_broadcast([128, 1024]))
        P.do("vector", "tensor_scalar", out=negb[:], in0=negb[:], scalar1=-1.0, scalar2=None, op0=ALU.mult)
        P.do("vector", "tensor_copy", out=wup[:], in_=wupf[:])
        P.do("gpsimd", "memset", ap=onesb[:], constant=1.0, w=[onesb])
        P.do("gpsimd", "memset", ap=one1[:], constant=1.0, w=[one1])
        P.do("gpsimd", "memset", ap=rmask[:], constant=1.0, w=[rmask])
        P.do("gpsimd", "memset", ap=rmask[:].rearrange("p (a b) -> p a b", b=64)[:, :, 0:1], constant=0.0, w=[rmask])
        P.do("gpsimd", "memset", ap=bdmask[:], constant=1.0, w=[bdmask])
        for hh in range(4):
            sl = bdmask[:, hh * 128:(hh + 1) * 128]
            P.do("gpsimd", "affine_select", out=sl, in_=sl, pattern=[[1, 128]], compare_op=ALU.is_ge,
                 fill=0.0, base=0, channel_multiplier=-1)
            sl2 = bdmask[64:128, hh * 128:hh * 128 + 64]
            P.do("gpsimd", "memset", ap=sl2, constant=0.0, w=[bdmask])
            sl3 = bdmask[0:64, hh * 128 + 64:(hh + 1) * 128]
            P.do("gpsimd", "memset", ap=sl3, constant=0.0, w=[bdmask])
        for c in range(8):
            sg = stage[c % 2]
            P.dma("sync" if c % 2 == 0 else "gpsimd", sg[:], w_in[c * 128:(c + 1) * 128, :])
            P.do("vector" if c % 2 == 0 else "gpsimd", "tensor_scalar", out=Win[:, c, :], in0=sg[:],
                 scalar1=gpre[:, c:c + 1], scalar2=None, op0=ALU.mult)
        for c in range(8):
            sg = stage[c % 2]
            P.dma("sync" if c % 2 == 0 else "gpsimd", sg[:, 0:1024], w_out[c * 128:(c + 1) * 128, :])
            P.do("vector" if c % 2 == 0 else "gpsimd", "tensor_copy", out=Wout[:, c, :], in_=sg[:, 0:1024])

        xt = [P.sb(st, f"g_xt{i}", [128, 1024], F32) for i in range(2)]
        xo = [P.sb(st, f"g_xo{i}", [128, 1024], F32) for i in range(2)]
        sq = P.sb(st, "g_sq", [128, 1024], F32)
        ss = P.sb(st, "g_ss", [128, 4], F32)
        rstd = P.sb(st, "g_rstd", [128, 2], F32)
        hb = P.sb(st, "g_hb", [128, 1024], BF16)
        hT = P.sb(st, "g_hT", [128, 1024], BF16)
        glrT = P.sb(st, "g_glrT", [16, 128], BF16)
        e1 = P.sb(st, "g_e1", [128, 512], F32)
        yv = P.sb(st, "g_yv", [128, 512], F32)
        Bc = P.sb(st, "g_Bc", [128, 512], F32)
        eb = P.sb(st, "g_eb", [128, 512], F32)
        enb = P.sb(st, "g_enb", [128, 512], F32)
        qeT = P.sb(st, "g_qeT", [128, 512], BF16)
        keT = P.sb(st, "g_keT", [128, 512], BF16)
        kdT = P.sb(st, "g_kdT", [128, 512], BF16)
        kdz = [P.sb(st, f"g_kd{i}", [128, 512], BF16) for i in range(2)]
        for i in range(2):
            P.do("gpsimd", "memset", ap=kdz[i][:], constant=0.0, w=[kdz[i]])
        zs = P.sb(st, "g_zs", [128, 1024], BF16)
        vb = P.sb(st, "g_vb", [128, 1024], BF16)
        ATm = P.sb(st, "g_ATm", [128, 512], BF16)
        oT = P.sb(st, "g_oT", [128, 1024], F32)
        osq = P.sb(st, "g_osq", [128, 1024], BF16)
        rs = P.sb(st, "g_rs", [128, 512], F32)
        tmpo = P.sb(st, "g_tmpo", [128, 1024], F32)
        ogT = P.sb(st, "g_ogT", [128, 1024], BF16)
        S32 = [P.sb(st, f"g_S32_{h}", [128, 256], F32) for h in range(4)]
        Sbf = [[P.sb(st, f"g_Sbf_{h}_{i}", [128, 256], BF16) for i in range(2)] for h in range(4)]
        t1 = P.sb(st, "g_t1", [128, 1024], F32)

        bank = [P.ps(st, f"g_bank{i}", [128, 512], F32) for i in range(7)]
        bS = P.ps(st, "g_bankS", [128, 512], F32)
        pS = [bS[:, i * 256:(i + 1) * 256] for i in range(2)]
        bA, bQ, bK, bZ0, bZ1, bV0, bV1 = bank
        bG = bA
        pTb = bA[:].bitcast(BF16)

        sidx = [0, 0, 0, 0]
        nupd = 0
        P.dma("sync", xt[0][:], x_in[0:128, :])
        for t in range(ntiles):
            x_t = xt[t % 2]
            if t + 1 < ntiles:
                P.dma("sync", xt[(t + 1) % 2][:], x_in[(t + 1) * 128:(t + 2) * 128, :])
            P.do("scalar", "activation", out=sq[:], in_=x_t[:], func=AF.Square, accum_out=ss[:, 0:1])
            P.do("vector", "tensor_scalar", out=rstd[:, 0:1], in0=ss[:, 0:1], scalar1=1.0 / D, scalar2=EPS,
                 op0=ALU.mult, op1=ALU.add)
            P.do("scalar", "activation", out=rstd[:, 0:1], in_=rstd[:, 0:1], func=AF.Sqrt)
            P.do("vector", "reciprocal", out=rstd[:, 0:1], in_=rstd[:, 0:1])
            P.do("vector", "tensor_scalar", out=hb[:], in0=x_t[:], scalar1=rstd[:, 0:1], scalar2=None, op0=ALU.mult)
            for c in range(8):
                P.do("tensor", "transpose", out=pTb[:, c * 128:(c + 1) * 128], in_=hb[:, c * 128:(c + 1) * 128],
                     identity=ident[:])
            P.do("vector", "tensor_copy", out=hT[:], in_=pTb)

            def hTc(c):
                return hT[:, c * 128:(c + 1) * 128]

            for c in range(8):
                P.do("tensor", "matmul", out=bA[0:16, 0:128], lhsT=Win[:, c, 2048:2064], rhs=hTc(c),
                     start=(c == 0), stop=(c == 7))
            P.do("scalar", "copy", out=glrT[:], in_=bA[0:16, 0:128])
            for hh in range(4):
                for c in range(8):
                    P.do("tensor", "matmul", out=bQ[:, hh * 128:(hh + 1) * 128], lhsT=Win[:, c, hh * 128:(hh + 1) * 128],
                         rhs=hTc(c), start=(c == 0), stop=(c == 7))
            for hh in range(4):
                for c in range(8):
                    P.do("tensor", "matmul", out=bK[:, hh * 128:(hh + 1) * 128],
                         lhsT=Win[:, c, 512 + hh * 128:512 + (hh + 1) * 128], rhs=hTc(c), start=(c == 0), stop=(c == 7))
            for hh in range(4):
                P.do("tensor", "matmul", out=bA[:, hh * 128:(hh + 1) * 128], lhsT=wup[:, hh * 128:(hh + 1) * 128],
                     rhs=glrT[:], start=True, stop=True)
            for zc in range(8):
                bz = bZ0 if zc < 4 else bZ1
                for c in range(8):
                    P.do("tensor", "matmul", out=bz[:, (zc % 4) * 128:(zc % 4 + 1) * 128],
                         lhsT=Win[:, c, 2064 + zc * 128:2064 + (zc + 1) * 128], rhs=hTc(c), start=(c == 0), stop=(c == 7))
            for i, bv in enumerate((bV0, bV1)):
                for c in range(8):
                    P.do("tensor", "matmul", out=bv[:], lhsT=hTc(c), rhs=Win[:, c, 1024 + i * 512:1024 + (i + 1) * 512],
                         start=(c == 0), stop=(c == 7))
            for hh in range(4):
                P.do("scalar", "activation", out=e1[:, hh * 128:(hh + 1) * 128], in_=bA[:, hh * 128:(hh + 1) * 128],
                     func=AF.Exp, scale=-1.0, bias=negb[:, hh:hh + 1])
            P.do("scalar", "activation", out=yv[:], in_=e1[:], func=AF.Ln, bias=one1[:, 0:1], scale=1.0)
            P.do("vector", "tensor_tensor_scan", out=Bc[:], data0=rmask[:], data1=yv[:], initial=0.0,
                 op0=ALU.mult, op1=ALU.add)
            P.do("scalar", "activation", out=eb[:], in_=Bc[:], func=AF.Exp, scale=-1.0 / 16.0)
            P.do("scalar", "activation", out=enb[:], in_=Bc[:], func=AF.Exp, scale=1.0 / 16.0)
            P.do("vector", "scalar_tensor_tensor", out=qeT[:], in0=bQ[:], scalar=128 ** -0.5, in1=eb[:],
                 op0=ALU.mult, op1=ALU.mult)
            P.do("vector", "tensor_tensor", out=keT[:], in0=bK[:], in1=enb[:], op=ALU.mult)
            for hh in range(4):
                for cc in range(2):
                    lo = hh * 128 + cc * 64
                    P.do("vector", "scalar_tensor_tensor", out=kdT[:, lo:lo + 64], in0=bK[:, lo:lo + 64],
                         scalar=eb[:, lo + 63:lo + 64], in1=enb[:, lo:lo + 64], op0=ALU.mult, op1=ALU.mult)
            P.do("scalar", "activation", out=zs[:, 0:512], in_=bZ0[:], func=AF.Silu)
            P.do("scalar", "activation", out=zs[:, 512:1024], in_=bZ1[:], func=AF.Silu)
            P.do("gpsimd" if False else "vector", "tensor_copy", out=vb[:, 0:512], in_=bV0[:])
            P.do("scalar", "copy", out=vb[:, 512:1024], in_=bV1[:])
            for hh in range(4):
                P.do("tensor", "transpose", out=pTb[:, hh * 128:(hh + 1) * 128], in_=kdT[:, hh * 128:(hh + 1) * 128],
                     identity=ident[:])
            P.do("vector", "tensor_copy", out=kdz[0][0:64, :], in_=pTb[0:64, 0:512])
            P.do("vector", "tensor_copy", out=kdz[1][64:128, :], in_=pTb[64:128, 0:512])
            for hh in range(4):
                P.do("tensor", "matmul", out=bQ[:, hh * 128:(hh + 1) * 128], lhsT=keT[:, hh * 128:(hh + 1) * 128],
                     rhs=qeT[:, hh * 128:(hh + 1) * 128], start=True, stop=True)
            P.do("vector", "tensor_tensor", out=ATm[:], in0=bQ[:], in1=bdmask[:], op=ALU.mult)
            bO = (bV0, bV1)
            for hh in range(4):
                for cc in range(2):
                    first = (t == 0 and cc == 0)
                    scur = Sbf[hh][sidx[hh]]
                    for vc in range(2):
                        idx = hh * 2 + vc
                        out = bO[idx // 4][:, (idx % 4) * 128 + cc * 64:(idx % 4) * 128 + cc * 64 + 64]
                        P.do("tensor", "matmul", out=out,
                             lhsT=vb[:, hh * 256 + vc * 128:hh * 256 + (vc + 1) * 128],
                             rhs=ATm[:, hh * 128 + cc * 64:hh * 128 + cc * 64 + 64],
                             start=True, stop=first)
                        if not first:
                            P.do("tensor", "matmul", out=out, lhsT=scur[:, vc * 128:(vc + 1) * 128],
                                 rhs=qeT[:, hh * 128 + cc * 64:hh * 128 + cc * 64 + 64], start=False, stop=True)
                    ps = pS[nupd % 2]
                    nupd += 1
                    P.do("tensor", "matmul", out=ps, lhsT=kdz[cc][:, hh * 128:(hh + 1) * 128],
                         rhs=vb[:, hh * 256:(hh + 1) * 256], start=True, stop=True)
                    if first:
                        P.do("vector", "tensor_copy", out=S32[hh][:], in_=ps)
                    else:
                        lo = hh * 128 + cc * 64
                        P.do("vector", "scalar_tensor_tensor", out=S32[hh][:], in0=S32[hh][:],
                             scalar=eb[:, lo + 63:lo + 64], in1=ps, op0=ALU.mult, op1=ALU.add)
                    sidx[hh] ^= 1
                    P.do("gpsimd", "tensor_copy", out=Sbf[hh][sidx[hh]][:], in_=S32[hh][:])
            P.do("scalar", "copy", out=oT[:, 0:512], in_=bV0[:])
            P.do("scalar", "copy", out=oT[:, 512:1024], in_=bV1[:])
            P.do("scalar", "activation", out=osq[:, 0:512], in_=bV0[:], func=AF.Square)
            P.do("scalar", "activation", out=osq[:, 512:1024], in_=bV1[:], func=AF.Square)
            for hh in range(4):
                for vc in range(2):
                    idx = hh * 2 + vc
                    P.do("tensor", "matmul", out=bK[:, hh * 128:(hh + 1) * 128], lhsT=onesb[:],
                         rhs=osq[:, idx * 128:(idx + 1) * 128], start=(vc == 0), stop=(vc == 1))
            P.do("vector", "tensor_scalar", out=rs[:], in0=bK[:], scalar1=1.0 / 256, scalar2=EPS, op0=ALU.mult, op1=ALU.add)
            P.do("scalar", "activation", out=rs[:], in_=rs[:], func=AF.Sqrt)
            P.do("vector", "reciprocal", out=rs[:], in_=rs[:])
            for hh in range(4):
                for vc in range(2):
                    idx = hh * 2 + vc
                    sl = slice(idx * 128, (idx + 1) * 128)
                    P.do("vector", "scalar_tensor_tensor", out=tmpo[:, sl], in0=oT[:, sl], scalar=hn[:, vc:vc + 1],
                         in1=rs[:, hh * 128:(hh + 1) * 128], op0=ALU.mult, op1=ALU.mult)
            P.do("gpsimd", "tensor_tensor", out=ogT[:], in0=tmpo[:], in1=zs[:], op=ALU.mult)
            bY = (bZ0, bZ1)
            for i in range(2):
                for fc in range(8):
                    P.do("tensor", "matmul", out=bY[i][:], lhsT=ogT[:, fc * 128:(fc + 1) * 128],
                         rhs=Wout[:, fc, i * 512:(i + 1) * 512], start=(fc == 0), stop=(fc == 7))
            P.do("scalar", "activation", out=sq[:, 0:512], in_=bZ0[:], func=AF.Square, accum_out=ss[:, 1:2])
            P.do("scalar", "activation", out=sq[:, 512:1024], in_=bZ1[:], func=AF.Square, accum_out=ss[:, 2:3])
            P.do("vector", "tensor_tensor", out=ss[:, 3:4], in0=ss[:, 1:2], in1=ss[:, 2:3], op=ALU.add)
            P.do("vector", "tensor_scalar", out=rstd[:, 1:2], in0=ss[:, 3:4], scalar1=1.0 / D, scalar2=EPS,
                 op0=ALU.mult, op1=ALU.add)
            P.do("scalar", "activation", out=rstd[:, 1:2], in_=rstd[:, 1:2], func=AF.Sqrt)
            P.do("vector", "reciprocal", out=rstd[:, 1:2], in_=rstd[:, 1:2])
            for i in range(2):
                P.do("vector", "scalar_tensor_tensor", out=t1[:, i * 512:(i + 1) * 512], in0=bY[i][:],
                     scalar=rstd[:, 1:2], in1=gpost[:, i * 512:(i + 1) * 512], op0=ALU.mult, op1=ALU.mult)
            x_o = xo[t % 2]
            P.do("gpsimd", "tensor_tensor", out=x_o[:], in0=t1[:], in1=x_t[:], op=ALU.add)
            P.dma("sync", x_out[t * 128:(t + 1) * 128, :], x_o[:])
        P.flush()


S = 4096
D = 1024
NT = S // 128
EPS = 1e-6
NEGM = -30000.0
TWO_PI = 2.0 * math.pi


def _front(P, t, x_in, xt, sq, ss, rstd, hb, hT, pTb, ident, ntl):
    x_t = xt[t % 2]
    if t == 0:
        P.dma("sync", x_t[:], x_in[0:128, :])
    if t + 1 < ntl:
        P.dma("sync", xt[(t + 1) % 2][:], x_in[(t + 1) * 128:(t + 2) * 128, :])
    P.do("scalar", "activation", out=sq[:], in_=x_t[:], func=AF.Square, accum_out=ss[:, 0:1])
    P.do("vector", "tensor_scalar", out=rstd[:, 0:1], in0=ss[:, 0:1], scalar1=1.0 / D, scalar2=EPS,
         op0=ALU.mult, op1=ALU.add)
    P.do("scalar", "activation", out=rstd[:, 0:1], in_=rstd[:, 0:1], func=AF.Sqrt)
    P.do("vector", "reciprocal", out=rstd[:, 0:1], in_=rstd[:, 0:1])
    P.do("vector", "tensor_scalar", out=hb[:], in0=x_t[:], scalar1=rstd[:, 0:1], scalar2=None, op0=ALU.mult)
    for c in range(8):
        P.do("tensor", "transpose", out=pTb[:, c * 128:(c + 1) * 128], in_=hb[:, c * 128:(c + 1) * 128],
             identity=ident[:])
    P.do("vector", "tensor_copy", out=hT[:], in_=pTb)
    return x_t


def _rope(P, src, nh, cosb, sinb, tmp, dst):
    s3 = src.rearrange("p (h d) -> p h d", d=64)
    d3 = dst.rearrange("p (h d) -> p h d", d=64)
    x1, x2 = s3[:, :, 0:32], s3[:, :, 32:64]
    cb = V(cosb.buf, cosb.ap.unsqueeze(1).to_broadcast([128, nh, 32]))
    sb_ = V(sinb.buf, sinb.ap.unsqueeze(1).to_broadcast([128, nh, 32]))
    ta = tmp[0][:, 0:nh * 32].rearrange("p (h d) -> p h d", d=32)
    tb = tmp[1][:, 0:nh * 32].rearrange("p (h d) -> p h d", d=32)
    P.do("vector", "tensor_tensor", out=ta, in0=x1, in1=cb, op=ALU.mult)
    P.do("vector", "tensor_tensor", out=tb, in0=x2, in1=sb_, op=ALU.mult)
    P.do("gpsimd", "tensor_tensor", out=d3[:, :, 0:32], in0=ta, in1=tb, op=ALU.subtract)
    tc_ = tmp[2][:, 0:nh * 32].rearrange("p (h d) -> p h d", d=32)
    td = tmp[3][:, 0:nh * 32].rearrange("p (h d) -> p h d", d=32)
    P.do("vector", "tensor_tensor", out=tc_, in0=x2, in1=cb, op=ALU.mult)
    P.do("vector", "tensor_tensor", out=td, in0=x1, in1=sb_, op=ALU.mult)
    P.do("gpsimd", "tensor_tensor", out=d3[:, :, 32:64], in0=tc_, in1=td, op=ALU.add)


def nsa_layer(P, nc, x_in, x_out, pos, pre_g, post_g, w_in, b_gate, pe_k, pe_v, ck_w1, ck_w2, cv_w1, cv_w2,
              w_out, ntiles=NT):
    nblk = 8 * ntiles - 1
    with ExitStack() as sta:
        ident, identf = make_ident(P, sta, "n")
        kcmpT = P.sb(sta, "n_kcmpT", [64, 4, 256], BF16)
        vcmp = P.sb(sta, "n_vcmp", [128, 2, 4, 129], BF16)
        costab = P.sb(sta, "n_cos", [128, NT, 32], F32)
        sintab = P.sb(sta, "n_sin", [128, NT, 32], F32)
        gpre = P.sb(sta, "n_gpre", [128, 8], F32)
        xt = [P.sb(sta, f"n_xt{i}", [128, 1024], F32) for i in range(2)]
        sq = P.sb(sta, "n_sq", [128, 1024], F32)
        ss = P.sb(sta, "n_ss", [128, 4], F32)
        rstd = P.sb(sta, "n_rstd", [128, 2], F32)
        hb = P.sb(sta, "n_hb", [128, 1024], BF16)
        hT = P.sb(sta, "n_hT", [128, 1024], BF16)
        rtmp = [P.sb(sta, f"n_rtmp{i}", [128, 512], F32) for i in range(4)]
        B = [P.ps(sta, f"n_bank{i}", [128, 512], F32) for i in range(8)]
        pTb = B[0][:].bitcast(BF16)

        P.dma("sync", gpre[:], pre_g.rearrange("(c p o) -> p c o", p=128, o=1), allow_slow_non_contiguous=True)

        with ExitStack() as stc:
            posi = P.sb(stc, "n_posi", [128, NT], I32)
            posf = P.sb(stc, "n_posf", [128, NT], F32)
            invf = P.sb(stc, "n_invf", [128, 32], F32)
            ang = P.sb(stc, "n_ang", [128, NT, 32], F32)
            kf = P.sb(stc, "n_kf", [128, NT, 32], F32)
            ki = P.sb(stc, "n_ki", [128, NT, 32], I32)
            mk = P.sb(stc, "n_mk", [128, NT, 32], F32)
            P.dma("sync", posi[:], pos.rearrange("(t p o) -> p t o", p=128, o=1), allow_slow_non_contiguous=True)
            P.do("vector", "tensor_copy", out=posf[:], in_=posi[:])
            P.do("gpsimd", "iota", out=invf[:], pattern=[[1, 32]], base=0, channel_multiplier=0,
                 allow_small_or_imprecise_dtypes=True)
            P.do("scalar", "activation", out=invf[:], in_=invf[:], func=AF.Exp, scale=-math.log(10000.0) / 32.0)
            for t in range(NT):
                P.do("vector", "tensor_scalar", out=ang[:, t, :], in0=invf[:], scalar1=posf[:, t:t + 1], scalar2=None,
                     op0=ALU.mult)

            def reduce_to_pi(dst, shift):
                P.do("vector", "tensor_scalar", out=kf[:], in0=ang[:], scalar1=shift, scalar2=1.0 / TWO_PI,
                     op0=ALU.add, op1=ALU.mult)
                P.do("vector", "tensor_copy", out=ki[:], in_=kf[:])
                P.do("vector", "tensor_copy", out=kf[:], in_=ki[:])
                P.do("vector", "scalar_tensor_tensor", out=kf[:], in0=kf[:], scalar=-TWO_PI, in1=ang[:],
                     op0=ALU.mult, op1=ALU.add)
                P.do("vector", "tensor_scalar", out=kf[:], in0=kf[:], scalar1=shift, scalar2=None, op0=ALU.add)
                P.do("vector", "tensor_scalar", out=mk[:], in0=kf[:], scalar1=math.pi, scalar2=-TWO_PI,
                     op0=ALU.is_gt, op1=ALU.mult)
                P.do("vector", "tensor_tensor", out=kf[:], in0=kf[:], in1=mk[:], op=ALU.add)
                P.do("vector", "tensor_scalar", out=mk[:], in0=kf[:], scalar1=-math.pi, scalar2=TWO_PI,
                     op0=ALU.is_lt, op1=ALU.mult)
                P.do("vector", "tensor_tensor", out=kf[:], in0=kf[:], in1=mk[:], op=ALU.add)
                P.do("vector", "tensor_scalar", out=kf[:], in0=kf[:], scalar1=math.pi, scalar2=-math.pi,
                     op0=ALU.min, op1=ALU.max)
                P.do("scalar", "activation", out=dst[:], in_=kf[:], func=AF.Sin)

            reduce_to_pi(sintab, 0.0)
            reduce_to_pi(costab, math.pi / 2.0)
            P.flush()

        with ExitStack() as st0:
            WinA = P.sb(st0, "n0_WinA", [128, 8, 512], BF16)
            stg = [P.sb(st0, f"n0_stg{i}", [128, 2048], F32) for i in range(2)]
            w1 = [P.sb(st0, f"n0_w1_{i}", [64, 32, 256], BF16) for i in range(2)]
            w2 = [P.sb(st0, f"n0_w2_{i}", [128, 2, 64], BF16) for i in range(2)]
            w2f = P.sb(st0, "n0_w2f", [128, 2, 64], F32)
            peTf = P.sb(st0, "n0_peTf", [64, 32], F32)
            peT = [P.sb(st0, f"n0_peT{i}", [64, 32], BF16) for i in range(2)]
            bias = P.sb(st0, "n0_bias", [128, 4], F32)
            kcT = P.sb(st0, "n0_kcT", [64, 4, S], BF16)
            vcT = P.sb(st0, "n0_vcT", [64, 4, S], BF16)
            kcr = P.sb(st0, "n0_kcr", [128, 256], BF16)
            vcb = P.sb(st0, "n0_vcb", [128, 256], BF16)
            hidT = P.sb(st0, "n0_hidT", [128, 2, 256], BF16)
            ovf = P.sb(st0, "n0_ovf", [128, 2, 64], F32)

            for c in range(8):
                sg = stg[c % 2]
                P.dma("sync" if c % 2 == 0 else "gpsimd", sg[:, 0:512], w_in[c * 128:(c + 1) * 128, 1024:1536])
                P.do("vector" if c % 2 == 0 else "gpsimd", "tensor_scalar", out=WinA[:, c, :], in0=sg[:, 0:512],
                     scalar1=gpre[:, c:c + 1], scalar2=None, op0=ALU.mult)
            n = 0
            for kv, (wsrc, w2src, pesrc) in enumerate(((ck_w1, ck_w2, pe_k), (cv_w1, cv_w2, pe_v))):
                w1v = wsrc.rearrange("(l d) n -> d l n", d=64)
                for q4 in range(4):
                    sg = stg[n % 2]
                    P.dma("sync" if n % 2 == 0 else "gpsimd", sg[0:64, :].rearrange("p (l n) -> p l n", n=256),
                          w1v[:, q4 * 8:(q4 + 1) * 8, :])
                    P.do("vector" if n % 2 == 0 else "gpsimd", "tensor_copy",
                         out=w1[kv][:, q4 * 8:(q4 + 1) * 8, :], in_=sg[0:64, :].rearrange("p (l n) -> p l n", n=256))
                    n += 1
                P.dma("sync", w2f[:], w2src.rearrange("(c p) n -> p c n", p=128))
                P.do("vector", "tensor_copy", out=w2[kv][:], in_=w2f[:])
                P.dma("sync", peTf[:], pesrc.rearrange("l d -> d l"), allow_slow_non_contiguous=True)
                P.do("vector", "tensor_copy", out=peT[kv][:], in_=peTf[:])
                for hc in range(2):
                    for l in range(32):
                        P.do("tensor", "matmul", out=B[1][:, kv * 2 + hc:kv * 2 + hc + 1],
                             lhsT=w1[kv][:, l, hc * 128:(hc + 1) * 128], rhs=peT[kv][:, l:l + 1],
                             start=(l == 0), stop=(l == 31))
            P.do("vector", "tensor_copy", out=bias[:], in_=B[1][:, 0:4])

            P.do("gpsimd", "memset", ap=vcmp[:], constant=0.0, w=[vcmp])
            P.do("gpsimd", "memset", ap=vcmp[:, :, :, 64:65], constant=1.0, w=[vcmp])
            P.do("gpsimd", "memset", ap=ovf[:], constant=1.0, w=[ovf])
            for ch in range(2):
                P.do("gpsimd", "affine_select", out=ovf[:, ch, :], in_=ovf[:, ch, :], pattern=[[-4, 64]],
                     compare_op=ALU.is_ge, fill=0.0, base=ch * 128 + 1, channel_multiplier=1)
                P.do("gpsimd", "affine_select", out=ovf[:, ch, :], in_=ovf[:, ch, :], pattern=[[4, 64]],
                     compare_op=ALU.is_ge, fill=0.0, base=3 - ch * 128, channel_multiplier=-1)
                for g in range(4):
                    P.do("vector", "tensor_copy", out=vcmp[:, ch, g, 65:129], in_=ovf[:, ch, :])

            for t in range(ntiles):
                _front(P, t, x_in, xt, sq, ss, rstd, hb, hT, pTb, ident, ntiles)
                for c in range(8):
                    P.do("tensor", "matmul", out=B[1][:], lhsT=hT[:, c * 128:(c + 1) * 128], rhs=WinA[:, c, :],
                         start=(c == 0), stop=(c == 7))
                _rope(P, B[1][:, 0:256], 4, costab[:, t, :], sintab[:, t, :], rtmp, kcr[:])
                P.do("scalar", "copy", out=vcb[:], in_=B[1][:, 256:512])
                for g in range(4):
                    P.do("tensor", "transpose", out=pTb[0:64, g * 128:(g + 1) * 128], in_=kcr[:, g * 64:(g + 1) * 64],
                         identity=ident[:])
                for g in range(4):
                    P.do("tensor", "transpose", out=pTb[0:64, 512 + g * 128:512 + (g + 1) * 128],
                         in_=vcb[:, g * 64:(g + 1) * 64], identity=ident[:])
                P.do("vector", "tensor_copy", out=kcT[:, :, t * 128:(t + 1) * 128],
                     in_=pTb[0:64, 0:512].rearrange("p (g t) -> p g t", g=4))
                P.do("scalar", "copy", out=vcT[:, :, t * 128:(t + 1) * 128],
                     in_=pTb[0:64, 512:1024].rearrange("p (g t) -> p g t", g=4))

            for kv, srcT in enumerate((kcT, vcT)):
                for g in range(4):
                    for hc in range(2):
                        bk = B[2 + hc]
                        for l in range(32):
                            P.do("tensor", "matmul", out=bk[:, 0:nblk], lhsT=w1[kv][:, l, hc * 128:(hc + 1) * 128],
                                 rhs=srcT[:, g, l:l + 16 * (nblk - 1) + 1:16], start=(l == 0), stop=(l == 31))
                        P.do("scalar", "activation", out=hidT[:, hc, 0:nblk], in_=bk[:, 0:nblk], func=AF.Silu,
                             bias=bias[:, kv * 2 + hc:kv * 2 + hc + 1], scale=1.0)
                    if kv == 0:
                        for hc in range(2):
                            P.do("tensor", "matmul", out=B[4][0:64, 0:nblk], lhsT=w2[0][:, hc, :],
                                 rhs=hidT[:, hc, 0:nblk], start=(hc == 0), stop=(hc == 1))
                        P.do("vector", "tensor_copy", out=kcmpT[:, g, 0:nblk], in_=B[4][0:64, 0:nblk])
                    else:
                        for ch in range(2):
                            rows = min(128, nblk - ch * 128)
                            if rows <= 0:
                                continue
                            for hc in range(2):
                                P.do("tensor", "matmul", out=B[4][0:rows, ch * 64:(ch + 1) * 64],
                                     lhsT=hidT[:, hc, ch * 128:ch * 128 + rows], rhs=w2[1][:, hc, :],
                                     start=(hc == 0), stop=(hc == 1))
                            P.do("vector", "tensor_copy", out=vcmp[0:rows, ch, g, 0:64],
                                 in_=B[4][0:rows, ch * 64:(ch + 1) * 64])
            P.flush()

        with ExitStack() as st1:
            Win = P.sb(st1, "n1_Win", [128, 8, 3632], BF16)
            Wout = P.sb(st1, "n1_Wout", [128, 8, 1024], BF16)
            gpost = P.sb(st1, "n1_gpost", [128, 1024], F32)
            bgate = P.sb(st1, "n1_bgate", [128, 48], F32)
            with ExitStack() as stl:
                stage = [P.sb(stl, f"n1_stage{i}", [128, 3632], F32) for i in range(2)]
                for c in range(8):
                    sg = stage[c % 2]
                    P.dma("sync" if c % 2 == 0 else "gpsimd", sg[:], w_in[c * 128:(c + 1) * 128, :])
                    P.do("vector" if c % 2 == 0 else "gpsimd", "tensor_scalar", out=Win[:, c, :], in0=sg[:],
                         scalar1=gpre[:, c:c + 1], scalar2=None, op0=ALU.mult)
                for c in range(8):
                    sg = stage[c % 2]
                    P.dma("sync" if c % 2 == 0 else "gpsimd", sg[:, 0:1024], w_out[c * 128:(c + 1) * 128, :])
                    P.do("vector" if c % 2 == 0 else "gpsimd", "tensor_copy", out=Wout[:, c, :], in_=sg[:, 0:1024])
                P.dma("sync", gpost[:], post_g.rearrange("(o n) -> o n", o=1).to_broadcast([128, 1024]))
                P.dma("sync", bgate[:], b_gate.rearrange("(o n) -> o n", o=1).to_broadcast([128, 48]))
                P.flush()

            ksT = P.sb(st1, "n1_ksT", [128, 4, S], BF16)
            vsa = P.sb(st1, "n1_vsa", [128, NT, 4, 65], BF16)
            kwT = P.sb(st1, "n1_kwT", [64, 4, 5 * 128], BF16)
            vwa = P.sb(st1, "n1_vwa", [128, 5, 4, 65], BF16)
            qaug = P.sb(st1, "n1_qaug", [128, 16, 128], BF16)
            qmask = P.track(qaug.h[64:128, :, :], "n1_qmask")
            qr = P.sb(st1, "n1_qr", [128, 1024], BF16)
            ksr = P.sb(st1, "n1_ksr", [128, 256], BF16)
            kwr = P.sb(st1, "n1_kwr", [128, 256], BF16)
            zs = P.sb(st1, "n1_zs", [128, 1024], BF16)
            glx = P.sb(st1, "n1_glx", [128, 48], F32)
            gt = P.sb(st1, "n1_gt", [128, 48], F32)
            PT = [P.sb(st1, f"n1_PT{i}", [128, 512], BF16) for i in range(3)]
            lrec = P.sb(st1, "n1_lrec", [128, 4], F32)
            wsc = P.sb(st1, "n1_wsc", [128, 4], F32)
            imp = P.sb(st1, "n1_imp", [128, 64], F32)
            imp2 = P.sb(st1, "n1_imp2", [128, 64], F32)
            imp3 = P.sb(st1, "n1_imp3", [128, 64], F32)
            m8 = P.sb(st1, "n1_m8", [128, 16], F32)
            M1 = P.sb(st1, "n1_M1", [128, 64], F32)
            Cm = P.sb(st1, "n1_Cm", [128, 64], F32)
            selm = P.sb(st1, "n1_selm", [128, 128], BF16)
            oacc = P.sb(st1, "n1_oacc", [128, 1024], F32)
            otmp = P.sb(st1, "n1_otmp", [128, 256], F32)
            og = P.sb(st1, "n1_og", [128, 1024], BF16)
            ogT = P.sb(st1, "n1_ogT", [128, 1024], BF16)
            t1 = P.sb(st1, "n1_t1", [128, 1024], F32)
            xo = [P.sb(st1, f"n1_xo{i}", [128, 1024], F32) for i in range(2)]

            P.do("gpsimd", "memset", ap=ksT[64:128, :, :], constant=1.0, w=[ksT])
            for g in range(4):
                P.do("gpsimd", "affine_select", out=ksT[64:128, g, :], in_=ksT[64:128, g, :], pattern=[[1, S]],
                     compare_op=ALU.is_ge, fill=0.0, base=0, channel_multiplier=-64)
                P.do("gpsimd", "affine_select", out=ksT[64:128, g, :], in_=ksT[64:128, g, :], pattern=[[-1, S]],
                     compare_op=ALU.is_ge, fill=0.0, base=63, channel_multiplier=64)
            P.do("gpsimd", "memset", ap=vsa[:, :, :, 64:65], constant=1.0, w=[vsa])
            P.do("gpsimd", "memset", ap=vwa[:, :, :, 64:65], constant=1.0, w=[vwa])
            P.do("gpsimd", "memset", ap=selm[:], constant=0.0, w=[selm])

            npt = 0
            nsb = 0
            for T in range(ntiles):
                x_t = _front(P, T, x_in, xt, sq, ss, rstd, hb, hT, pTb, ident, ntiles)

                def hTc(c):
                    return hT[:, c * 128:(c + 1) * 128]

                for bk, lo, wd in ((B[1], 0, 512), (B[2], 512, 512), (B[3], 1536, 512), (B[4], 2048, 512), (B[5], 2560, 48)):
                    for c in range(8):
                        P.do("tensor", "matmul", out=bk[:, 0:wd], lhsT=hTc(c), rhs=Win[:, c, lo:lo + wd],
                             start=(c == 0), stop=(c == 7))
                cb, sb_ = costab[:, T, :], sintab[:, T, :]
                _rope(P, B[1][:], 8, cb, sb_, rtmp, qr[:, 0:512])
                _rope(P, B[2][:], 8, cb, sb_, rtmp, qr[:, 512:1024])
                _rope(P, B[3][:, 0:256], 4, cb, sb_, rtmp, ksr[:])
                _rope(P, B[4][:, 0:256], 4, cb, sb_, rtmp, kwr[:])
                P.do("scalar", "copy", out=vsa[:, T, :, 0:64], in_=B[3][:, 256:512].rearrange("p (g d) -> p g d", d=64))
                P.do("scalar", "copy", out=vwa[:, T % 5, :, 0:64], in_=B[4][:, 256:512].rearrange("p (g d) -> p g d", d=64))
                P.do("vector", "tensor_tensor", out=glx[:], in0=B[5][:, 0:48], in1=bgate[:], op=ALU.add)
                P.do("scalar", "activation", out=glx[:], in_=glx[:], func=AF.Exp, scale=-1.0)
                P.do("vector", "tensor_scalar", out=glx[:], in0=glx[:], scalar1=1.0, scalar2=None, op0=ALU.add)
                P.do("vector", "reciprocal", out=gt[:], in_=glx[:])
                for i, bk in enumerate((B[1], B[2])):
                    for c in range(8):
                        P.do("tensor", "matmul", out=bk[:], lhsT=hTc(c), rhs=Win[:, c, 2608 + i * 512:2608 + (i + 1) * 512],
                             start=(c == 0), stop=(c == 7))
                for half in range(2):
                    for hh in range(8):
                        h = half * 8 + hh
                        P.do("tensor", "transpose", out=pTb[0:64, hh * 128:(hh + 1) * 128],
                             in_=qr[:, h * 64:(h + 1) * 64], identity=ident[:])
                    P.do("scalar", "mul", out=qaug[0:64, half * 8:(half + 1) * 8, :],
                         in_=pTb[0:64, :].rearrange("p (h t) -> p h t", h=8), mul=0.125)
                for g in range(4):
                    P.do("tensor", "transpose", out=pTb[0:64, g * 128:(g + 1) * 128], in_=ksr[:, g * 64:(g + 1) * 64],
                         identity=ident[:])
                for g in range(4):
                    P.do("tensor", "transpose", out=pTb[0:64, 512 + g * 128:512 + (g + 1) * 128],
                         in_=kwr[:, g * 64:(g + 1) * 64], identity=ident[:])
                P.do("vector", "tensor_copy", out=ksT[0:64, :, T * 128:(T + 1) * 128],
                     in_=pTb[0:64, 0:512].rearrange("p (g t) -> p g t", g=4))
                P.do("vector", "tensor_copy", out=kwT[:, :, (T % 5) * 128:(T % 5 + 1) * 128],
                     in_=pTb[0:64, 512:1024].rearrange("p (g t) -> p g t", g=4))
                P.do("scalar", "activation", out=zs[:, 0:512], in_=B[1][:], func=AF.Silu)
                P.do("scalar", "activation", out=zs[:, 512:1024], in_=B[2][:], func=AF.Silu)

                P.do("gpsimd", "memset", ap=M1[:], constant=0.0, w=[M1])
                P.do("gpsimd", "memset", ap=Cm[:], constant=0.0, w=[Cm])
                for half in range(2):
                    cur = 2 * T + half
                    rs_ = slice(half * 64, (half + 1) * 64)
                    if cur - 2 >= 1:
                        P.do("gpsimd", "memset", ap=M1[rs_, 1:cur - 1], constant=1.0, w=[M1])
                    if cur + 1 < 64:
                        P.do("gpsimd", "memset", ap=Cm[rs_, cur + 1:64], constant=-1e30, w=[Cm])
                    P.do("gpsimd", "memset", ap=Cm[rs_, max(cur - 1, 0):cur + 1], constant=1e9, w=[Cm])
                P.do("gpsimd", "memset", ap=Cm[:, 0:1], constant=1e9, w=[Cm])

                nvalid = min(8 * T + 7, nblk)
                for g in range(4):
                    qg = qaug[0:64, 4 * g:4 * g + 4, :]
                    qga = qaug[:, 4 * g:4 * g + 4, :]
                    nch = (nvalid + 127) // 128
                    for ch in range(nch):
                        rows = min(128, nvalid - ch * 128)
                        sbk = B[3 + nsb % 2]; nsb += 1
                        pt = PT[npt % 3]; npt += 1
                        P.do("tensor", "matmul", out=sbk[0:rows, :], lhsT=kcmpT[:, g, ch * 128:ch * 128 + rows], rhs=qg,
                             start=True, stop=True)
                        P.do("scalar", "activation", out=pt[0:rows, :], in_=sbk[0:rows, :], func=AF.Exp)
                        P.do("gpsimd", "affine_select", out=pt[0:rows, :], in_=pt[0:rows, :], pattern=[[0, 4], [1, 128]],
                             compare_op=ALU.is_ge, fill=0.0, base=128 * T - 2048 * ch - 31, channel_multiplier=-16)
                        for h in range(4):
                            P.do("tensor", "matmul", out=B[1 + h // 2][:, (h % 2) * 129:(h % 2) * 129 + 129],
                                 lhsT=pt[0:rows, h * 128:(h + 1) * 128], rhs=vcmp[0:rows, ch, g, :],
                                 start=(ch == 0 and h % 2 == 0), stop=(ch == nch - 1 and h % 2 == 1))
                    for b2 in range(2):
                        P.do("vector", "tensor_scalar", out=lrec[:, 2 * b2:2 * b2 + 2], in0=B[1 + b2][:, 64:258:129],
                             scalar1=1e-20, scalar2=None, op0=ALU.max)
                    P.do("vector", "reciprocal", out=lrec[:], in_=lrec[:])
                    P.do("vector", "tensor_tensor", out=wsc[:], in0=lrec[:],
                         in1=gt[:].rearrange("p (h c) -> p h c", c=3)[:, 4 * g:4 * g + 4, 0], op=ALU.mult)
                    for h in range(4):
                        bo = B[1 + h // 2]
                        off = (h % 2) * 129
                        H = 4 * g + h
                        if h % 2 == 0:
                            P.do("vector", "tensor_tensor",
                                 out=oacc[:, H * 64:(H + 2) * 64].rearrange("p (h d) -> p h d", d=64),
                                 in0=bo[:, 0:258].rearrange("p (h d) -> p h d", d=129)[:, :, 0:64],
                                 in1=V(wsc, wsc[:, h:h + 2].ap.unsqueeze(2).to_broadcast([128, 2, 64])), op=ALU.mult)
                        if h == 0:
                            P.do("vector", "tensor_scalar", out=imp[:], in0=bo[:, off + 65:off + 129],
                                 scalar1=lrec[:, h:h + 1], scalar2=None, op0=ALU.mult)
                        else:
                            P.do("vector", "scalar_tensor_tensor", out=imp[:], in0=bo[:, off + 65:off + 129],
                                 scalar=lrec[:, h:h + 1], in1=imp[:], op0=ALU.mult, op1=ALU.add)
                    P.do("vector", "tensor_tensor", out=imp2[:], in0=imp[:], in1=M1[:], op=ALU.mult)
                    P.do("vector", "tensor_tensor", out=imp2[:], in0=imp2[:], in1=Cm[:], op=ALU.add)
                    P.do("vector", "max", out=m8[:, 0:8], in_=imp2[:])
                    P.do("vector", "match_replace", out=imp3[:], in_to_replace=m8[:, 0:8], in_values=imp2[:],
                         imm_value=-3e38)
                    P.do("vector", "max", out=m8[:, 8:16], in_=imp3[:])
                    P.do("vector", "tensor_scalar", out=selm[:, 64:128], in0=imp2[:], scalar1=m8[:, 15:16], scalar2=NEGM,
                         op0=ALU.is_lt, op1=ALU.mult)
                    P.do("tensor", "transpose", out=pTb[:, 0:128], in_=selm[:], identity=ident[:])
                    P.do("vector", "tensor_copy", out=qmask[:, 4 * g:4 * g + 4, :],
                         in_=V(B[0], pTb.ap[64:128, 0:128].unsqueeze(1).to_broadcast([64, 4, 128])))
                    kts = list(range(max(0, T - 4), T + 1))
                    for kt in kts:
                        sbk = B[3 + nsb % 2]; nsb += 1
                        pt = PT[npt % 3]; npt += 1
                        sl = kt % 5
                        P.do("tensor", "matmul", out=sbk[:], lhsT=kwT[:, g, sl * 128:(sl + 1) * 128], rhs=qg,
                             start=True, stop=True)
                        P.do("scalar", "activation", out=pt[:], in_=sbk[:], func=AF.Exp)
                        if kt == T:
                            P.do("gpsimd", "affine_select", out=pt[:], in_=pt[:], pattern=[[0, 4], [1, 128]],
                                 compare_op=ALU.is_ge, fill=0.0, base=0, channel_multiplier=-1)
                        if kt == T - 4:
                            P.do("gpsimd", "affine_select", out=pt[:], in_=pt[:], pattern=[[0, 4], [-1, 128]],
                                 compare_op=ALU.is_ge, fill=0.0, base=-1, channel_multiplier=1)
                        for h in range(4):
                            P.do("tensor", "matmul", out=B[6][:, h * 65:h * 65 + 65], lhsT=pt[:, h * 128:(h + 1) * 128],
                                 rhs=vwa[:, sl, g, :], start=(kt == kts[0] and h == 0), stop=(kt == T and h == 3))
                    _combine(P, B[6], g, 2, lrec, wsc, gt, oacc, otmp)
                for g in range(4):
                    qga = qaug[:, 4 * g:4 * g + 4, :]
                    bsel = B[5] if g % 2 == 0 else B[7]
                    for kt in range(T + 1):
                        sbk = B[3 + nsb % 2]; nsb += 1
                        pt = PT[npt % 3]; npt += 1
                        P.do("tensor", "matmul", out=sbk[:], lhsT=ksT[:, g, kt * 128:(kt + 1) * 128], rhs=qga,
                             start=True, stop=True, r=[qmask])
                        P.do("scalar", "activation", out=pt[:], in_=sbk[:], func=AF.Exp)
                        if kt == T:
                            P.do("gpsimd", "affine_select", out=pt[:], in_=pt[:], pattern=[[0, 4], [1, 128]],
                                 compare_op=ALU.is_ge, fill=0.0, base=0, channel_multiplier=-1)
                        for h in range(4):
                            P.do("tensor", "matmul", out=bsel[:, h * 65:h * 65 + 65], lhsT=pt[:, h * 128:(h + 1) * 128],
                                 rhs=vsa[:, kt, g, :], start=(kt == 0 and h == 0), stop=(kt == T and h == 3))
                    _combine(P, bsel, g, 1, lrec, wsc, gt, oacc, otmp)

                P.do("gpsimd", "tensor_tensor", out=og[:], in0=oacc[:], in1=zs[:], op=ALU.mult)
                for c in range(8):
                    P.do("tensor", "transpose", out=pTb[:, c * 128:(c + 1) * 128], in_=og[:, c * 128:(c + 1) * 128],
                         identity=ident[:])
                P.do("vector", "tensor_copy", out=ogT[:], in_=pTb)
                for i in range(2):
                    for fc in range(8):
                        P.do("tensor", "matmul", out=B[1 + i][:], lhsT=ogT[:, fc * 128:(fc + 1) * 128],
                             rhs=Wout[:, fc, i * 512:(i + 1) * 512], start=(fc == 0), stop=(fc == 7))
                P.do("scalar", "activation", out=sq[:, 0:512], in_=B[1][:], func=AF.Square, accum_out=ss[:, 1:2])
                P.do("scalar", "activation", out=sq[:, 512:1024], in_=B[2][:], func=AF.Square, accum_out=ss[:, 2:3])
                P.do("vector", "tensor_tensor", out=ss[:, 3:4], in0=ss[:, 1:2], in1=ss[:, 2:3], op=ALU.add)
                P.do("vector", "tensor_scalar", out=rstd[:, 1:2], in0=ss[:, 3:4], scalar1=1.0 / D, scalar2=EPS,
                     op0=ALU.mult, op1=ALU.add)
                P.do("scalar", "activation", out=rstd[:, 1:2], in_=rstd[:, 1:2], func=AF.Sqrt)
                P.do("vector", "reciprocal", out=rstd[:, 1:2], in_=rstd[:, 1:2])
                for i in range(2):
                    P.do("vector", "scalar_tensor_tensor", out=t1[:, i * 512:(i + 1) * 512], in0=B[1 + i][:],
                         scalar=rstd[:, 1:2], in1=gpost[:, i * 512:(i + 1) * 512], op0=ALU.mult, op1=ALU.mult)
                x_o = xo[T % 2]
                P.do("gpsimd", "tensor_tensor", out=x_o[:], in0=t1[:], in1=x_t[:], op=ALU.add)
                P.dma("sync", x_out[T * 128:(T + 1) * 128, :], x_o[:])
            P.flush()


def _combine(P, bo, g, c, lrec, wsc, gt, oacc, otmp):
    P.do("vector", "tensor_scalar", out=lrec[:], in0=bo[:, 64:260:65], scalar1=1e-20, scalar2=None, op0=ALU.max)
    P.do("vector", "reciprocal", out=lrec[:], in_=lrec[:])
    P.do("vector", "tensor_tensor", out=wsc[:], in0=lrec[:],
         in1=gt[:].rearrange("p (h c) -> p h c", c=3)[:, 4 * g:4 * g + 4, c], op=ALU.mult)
    bo3 = bo[:, 0:260].rearrange("p (h d) -> p h d", d=65)[:, :, 0:64]
    wbc = V(wsc, wsc[:].ap.unsqueeze(2).to_broadcast([128, 4, 64]))
    P.do("vector", "tensor_tensor", out=otmp[:].rearrange("p (h d) -> p h d", d=64), in0=bo3, in1=wbc, op=ALU.mult)
    P.do("gpsimd", "tensor_tensor", out=oacc[:, g * 256:(g + 1) * 256], in0=oacc[:, g * 256:(g + 1) * 256],
         in1=otmp[:], op=ALU.add)


FUSED = True


def _din(nc, name, shape, dt=F32):
    return nc.dram_tensor(name, list(shape), dt, kind="ExternalInput").ap()


def _gla_inputs(nc):
    return dict(pre=_din(nc, "g_pre", [1024]), post=_din(nc, "g_post", [1024]), w_in=_din(nc, "g_w_in", [1024, 3088]),
                w_up=_din(nc, "g_w_up", [16, 512]), b_gk=_din(nc, "g_b_gk", [512]), hnorm=_din(nc, "g_hnorm", [256]),
                w_out=_din(nc, "g_w_out", [1024, 1024]))


def _nsa_inputs(nc):
    return dict(pos=_din(nc, "n_pos", [4096], I32), pre=_din(nc, "n_pre", [1024]), post=_din(nc, "n_post", [1024]),
                w_in=_din(nc, "n_w_in", [1024, 3632]), b_gate=_din(nc, "n_b_gate", [48]),
                pe_k=_din(nc, "n_pe_k", [32, 64]), pe_v=_din(nc, "n_pe_v", [32, 64]),
                ck_w1=_din(nc, "n_ck_w1", [2048, 256]), ck_w2=_din(nc, "n_ck_w2", [256, 64]),
                cv_w1=_din(nc, "n_cv_w1", [2048, 256]), cv_w2=_din(nc, "n_cv_w2", [256, 64]),
                w_out=_din(nc, "n_w_out", [1024, 1024]))


def _emit_gla(P, nc, x, y, gi):
    gla_layer(P, nc, x, y, gi["pre"], gi["post"], gi["w_in"], gi["w_up"], gi["b_gk"], gi["hnorm"], gi["w_out"])


def _emit_nsa(P, nc, x, y, ni):
    nsa_layer(P, nc, x, y, ni["pos"], ni["pre"], ni["post"], ni["w_in"], ni["b_gate"], ni["pe_k"], ni["pe_v"],
              ni["ck_w1"], ni["ck_w2"], ni["cv_w1"], ni["cv_w2"], ni["w_out"])


def _gla_map(inp, b):
    return {"g_pre": inp["pre_norm"][0], "g_post": inp["post_norm"][0], "g_w_in": inp["gla_w_in"][0],
            "g_w_up": inp["gla_w_gk_up"][0], "g_b_gk": inp["gla_b_gk"][0], "g_hnorm": inp["gla_head_norm"][0],
            "g_w_out": inp["gla_w_out"][0]}


def _nsa_map(inp, b):
    return {"n_pos": np.ascontiguousarray(inp["positions"][b]), "n_pre": inp["pre_norm"][1], "n_post": inp["post_norm"][1],
            "n_w_in": inp["nsa_w_in"][0], "n_b_gate": inp["nsa_b_gate"][0], "n_pe_k": inp["nsa_pe_k"][0],
            "n_pe_v": inp["nsa_pe_v"][0], "n_ck_w1": inp["nsa_ck_w1"][0], "n_ck_w2": inp["nsa_ck_w2"][0],
            "n_cv_w1": inp["nsa_cv_w1"][0], "n_cv_w2": inp["nsa_cv_w2"][0], "n_w_out": inp["nsa_w_out"][0]}


def kernel(**inputs):
    inp = {k: np.ascontiguousarray(np.asarray(v)) for k, v in inputs.items()}
    n = 8
    cores = list(range(n))
    xs = [np.ascontiguousarray(inp["x"][b]) for b in range(n)]
    if FUSED:
        nc = bass.Bass("TRN2", target_bir_lowering=False)
        x = _din(nc, "x", [4096, 1024])
        y = nc.dram_tensor("y", [4096, 1024], F32, kind="ExternalOutput").ap()
        x1 = nc.dram_tensor("x1_stage", [4096, 1024], F32, kind="ExternalOutput").ap()
        gi = _gla_inputs(nc)
        ni = _nsa_inputs(nc)
        P = Prog(nc)
        _emit_gla(P, nc, x, x1, gi)
        _emit_nsa(P, nc, x1, y, ni)
        maps = [dict(x=xs[b], **_gla_map(inp, b), **_nsa_map(inp, b)) for b in range(n)]
        res = run_bass_kernel_spmd(nc, maps, core_ids=cores)
        return np.stack([np.asarray(res.results[b]["y"]) for b in range(n)], axis=0).astype(np.float32)
    nc1 = bass.Bass("TRN2", target_bir_lowering=False)
    x = _din(nc1, "x", [4096, 1024])
    y = nc1.dram_tensor("y", [4096, 1024], F32, kind="ExternalOutput").ap()
    gi = _gla_inputs(nc1)
    _emit_gla(Prog(nc1), nc1, x, y, gi)
    res1 = run_bass_kernel_spmd(nc1, [dict(x=xs[b], **_gla_map(inp, b)) for b in range(n)], core_ids=cores)
    x1s = [np.ascontiguousarray(np.asarray(res1.results[b]["y"])) for b in range(n)]
    nc2 = bass.Bass("TRN2", target_bir_lowering=False)
    x = _din(nc2, "x", [4096, 1024])
    y = nc2.dram_tensor("y", [4096, 1024], F32, kind="ExternalOutput").ap()
    ni = _nsa_inputs(nc2)
    _emit_nsa(Prog(nc2), nc2, x, y, ni)
    res2 = run_bass_kernel_spmd(nc2, [dict(x=x1s[b], **_nsa_map(inp, b)) for b in range(n)], core_ids=cores)
    return np.stack([np.asarray(res2.results[b]["y"]) for b in range(n)], axis=0).astype(np.float32)
```

```python
import math
from contextlib import ExitStack
import numpy as np
import concourse.bass as bass
import concourse.mybir as mybir
from concourse.bass_utils import run_bass_kernel_spmd

F32 = mybir.dt.float32
BF16 = mybir.dt.bfloat16
I32 = mybir.dt.int32
AF = mybir.ActivationFunctionType
ALU = mybir.AluOpType
AX = mybir.AxisListType

ENGS = ("tensor", "vector", "scalar", "gpsimd", "sync")
OUT_KEYS = ("out", "accum_out")


class Buf:
    def __init__(self, h, name):
        self.h = h
        self.name = name
        self.w = None
        self.r = []
        self.sem = None
        self.semcnt = 0

    def __getitem__(self, idx):
        return V(self, self.h[idx])


class V:
    def __init__(self, buf, ap):
        self.buf = buf
        self.ap = ap

    def __getitem__(self, idx):
        return V(self.buf, self.ap[idx])

    def rearrange(self, *a, **k):
        return V(self.buf, self.ap.rearrange(*a, **k))

    def bitcast(self, dt):
        return V(self.buf, self.ap.bitcast(dt))

    def to_broadcast(self, shape):
        return V(self.buf, self.ap.to_broadcast(shape))


class Op:
    __slots__ = ("eng", "fn", "deps", "signal", "idx", "dma_sem", "dma_cnt", "semval")

    def __init__(self, eng, fn):
        self.eng = eng
        self.fn = fn
        self.deps = []
        self.signal = False
        self.dma_sem = None
        self.dma_cnt = 0
        self.semval = 0


class Prog:
    def __init__(self, nc):
        self.nc = nc
        self.esem = {e: nc.alloc_semaphore(f"s_{e}") for e in ENGS}
        self.ecount = {e: 0 for e in ENGS}
        self.ops = {e: [] for e in ENGS}
        self.nbuf = 0
        self.dma_sems = []
        self.all_bufs = []

    def sb(self, stack, name, shape, dtype):
        h = stack.enter_context(self.nc.sbuf_tensor(name, list(shape), dtype))
        b = Buf(h, name)
        self.all_bufs.append(b)
        return b

    def ps(self, stack, name, shape, dtype=F32):
        h = stack.enter_context(self.nc.psum_tensor(name, list(shape), dtype))
        b = Buf(h, name)
        b.psum = True
        self.all_bufs.append(b)
        return b

    def track(self, h, name):
        b = Buf(h, name)
        self.all_bufs.append(b)
        return b

    def _collect(self, kw):
        reads, writes, real = [], [], {}
        for k, v in kw.items():
            if isinstance(v, V):
                (writes if k in OUT_KEYS else reads).append(v.buf)
                real[k] = v.ap
            else:
                real[k] = v
        return reads, writes, real

    def _add(self, eng, fn, reads, writes, is_dma=False, dma_buf=None, extra_reads=(), extra_writes=()):
        self.nops = getattr(self, "nops", 0) + 1
        if self.nops > getattr(self, "maxops", 1 << 60):
            return None
        op = Op(eng, fn)
        reads = list(reads) + list(extra_reads)
        writes = list(writes) + list(extra_writes)
        deps = []
        for b in reads:
            if b.w is not None:
                deps.append(("raw", b.w))
            if getattr(b, "psum", False):
                for t in b.r:
                    if t[0] == "c" and t[1] != eng:
                        deps.append(("rar", t))
        for b in writes:
            if b.w is not None:
                deps.append(("waw", b.w))
            for t in b.r:
                deps.append(("war", t))
        opidx = len(self.ops[eng])
        keep = []
        for kind, t in deps:
            if t[0] == "c":
                if t[1] == eng and not is_dma:
                    if kind != "raw" or eng == "tensor":
                        continue
            keep.append(t)
        op.deps = keep
        self.ops[eng].append(op)
        if is_dma:
            b = dma_buf
            if b.sem is None:
                b.sem = self.nc.alloc_semaphore(f"d_{b.name}")
                self.dma_sems.append(b)
            b.semcnt += 16
            op.dma_sem = b.sem
            ticket = ("d", b, b.semcnt)
        else:
            ticket = ("c", eng, opidx)
        for b in reads:
            b.r.append(ticket)
        for b in writes:
            b.w = ticket
            b.r = []
        return op

    def do(self, eng, name, r=(), w=(), **kw):
        reads, writes, real = self._collect(kw)
        self.last_desc = (eng, name)
        fn = lambda e, name=name, real=real: getattr(e, name)(**real)
        return self._add(eng, fn, reads, writes, extra_reads=r, extra_writes=w)

    def dma(self, eng, out, in_, **kw):
        reads, writes = [], []
        oa, ia = out, in_
        if isinstance(out, V):
            writes.append(out.buf)
            oa = out.ap
        if isinstance(in_, V):
            reads.append(in_.buf)
            ia = in_.ap
        dbuf = writes[0] if writes else reads[0]
        fn = lambda e, oa=oa, ia=ia, kw=kw: e.dma_start(out=oa, in_=ia, **kw)
        return self._add(eng, fn, reads, writes, is_dma=True, dma_buf=dbuf)

    def flush(self, final_wait_bufs=()):
        nc = self.nc
        for e in ENGS:
            for op in self.ops[e]:
                for t in op.deps:
                    if t[0] == "c":
                        self.ops[t[1]][t[2]].signal = True
        for e in ENGS:
            for op in reversed(self.ops[e]):
                if op.dma_sem is None:
                    op.signal = True
                    break
        for e in ENGS:
            c = self.ecount[e]
            for op in self.ops[e]:
                if op.signal:
                    c += 1
                op.semval = c
            self.ecount[e] = c
        ops = self.ops
        esem = self.esem
        ecount = dict(self.ecount)
        dsems = list(self.dma_sems)

        def emit(e):
            def body(eng):
                waited = {}
                for op in ops[e]:
                    need = {}
                    for t in op.deps:
                        if t[0] == "c":
                            key = ("c", t[1])
                            val = ops[t[1]][t[2]].semval
                            sem = esem[t[1]]
                        else:
                            key = ("d", id(t[1]))
                            val = t[2]
                            sem = t[1].sem
                        if waited.get(key, -1) >= val:
                            continue
                        if key not in need or need[key][1] < val:
                            need[key] = (sem, val)
                    for key, (sem, val) in need.items():
                        eng.wait_ge(sem, val)
                        waited[key] = val
                    ins = op.fn(eng)
                    if op.dma_sem is not None:
                        ins.then_inc(op.dma_sem, 16)
                    elif op.signal:
                        ins.then_inc(esem[e], 1)
                for e2 in ENGS:
                    if e2 != e and ecount[e2] > 0 and waited.get(("c", e2), -1) < ecount[e2]:
                        eng.wait_ge(esem[e2], ecount[e2])
                for b in dsems:
                    if waited.get(("d", id(b)), -1) < b.semcnt:
                        eng.wait_ge(b.sem, b.semcnt)
            return body

        with nc.Block() as blk:
            for e in ENGS:
                getattr(blk, e)(emit(e))
        self.ops = {e: [] for e in ENGS}
        for b in self.all_bufs:
            b.w = None
            b.r = []


S = 4096
D = 1024
NT = S // 128
EPS = 1e-6


def make_ident(P, st, pfx="g"):
    identf = P.sb(st, pfx + "_identf", [128, 128], F32)
    ident = P.sb(st, pfx + "_ident", [128, 128], BF16)
    P.do("gpsimd", "memset", ap=identf[:], constant=1.0, w=[identf])
    P.do("gpsimd", "affine_select", out=identf[:], in_=identf[:], pattern=[[-1, 128]],
         compare_op=ALU.is_equal, fill=0.0, base=0, channel_multiplier=1)
    P.do("vector", "tensor_copy", out=ident[:], in_=identf[:])
    return ident, identf


def gla_layer(P, nc, x_in, x_out, pre_g, post_g, w_in, w_up, b_gk, hnorm, w_out, ntiles=NT):
    with ExitStack() as st:
        ident, identf = make_ident(P, st)
        Win = P.sb(st, "g_Win", [128, 8, 3088], BF16)
        Wout = P.sb(st, "g_Wout", [128, 8, 1024], BF16)
        stage = [P.sb(st, f"g_stage{i}", [128, 3088], F32) for i in range(2)]
        gpre = P.sb(st, "g_gpre", [128, 8], F32)
        gpost = P.sb(st, "g_gpost", [128, 1024], F32)
        wupf = P.sb(st, "g_wupf", [16, 512], F32)
        wup = P.sb(st, "g_wup", [16, 512], BF16)
        negb = P.sb(st, "g_negb", [128, 4], F32)
        hn = P.sb(st, "g_hn", [128, 2], F32)
        onesb = P.sb(st, "g_ones", [128, 128], BF16)
        rmask = P.sb(st, "g_rmask", [128, 512], F32)
        bdmask = P.sb(st, "g_bdmask", [128, 512], F32)
        one1 = P.sb(st, "g_one1", [128, 1], F32)

        P.dma("sync", gpre[:], pre_g.rearrange("(c p o) -> p c o", p=128, o=1), allow_slow_non_contiguous=True)
        P.dma("sync", negb[:], b_gk.rearrange("(c p o) -> p c o", p=128, o=1), allow_slow_non_contiguous=True)
        P.dma("sync", hn[:], hnorm.rearrange("(c p o) -> p c o", p=128, o=1), allow_slow_non_contiguous=True)
        P.dma("sync", wupf[:], w_up)
        P.dma("sync", gpost[:], post_g.rearrange("(o n) -> o n", o=1).to_broadcast([128, 1024]))
        P.do("vector", "tensor_scalar", out=negb[:], in0=negb[:], scalar1=-1.0, scalar2=None, op0=ALU.mult)
        P.do("vector", "tensor_copy", out=wup[:], in_=wupf[:])
        P.do("gpsimd", "memset", ap=onesb[:], constant=1.0, w=[onesb])
        P.do("gpsimd", "memset", ap=one1[:], constant=1.0, w=[one1])
        P.do("gpsimd", "memset", ap=rmask[:], constant=1.0, w=[rmask])
        P.do("gpsimd", "memset", ap=rmask[:].rearrange("p (a b) -> p a b", b=64)[:, :, 0:1], constant=0.0, w=[rmask])
        P.do("gpsimd", "memset", ap=bdmask[:], constant=1.0, w=[bdmask])
        for hh in range(4):
            sl = bdmask[:, hh * 128:(hh + 1) * 128]
            P.do("gpsimd", "affine_select", out=sl, in_=sl, pattern=[[1, 128]], compare_op=ALU.is_ge,
                 fill=0.0, base=0, channel_multiplier=-1)
            sl2 = bdmask[64:128, hh * 128:hh * 128 + 64]
            P.do("gpsimd", "memset", ap=sl2, constant=0.0, w=[bdmask])
            sl3 = bdmask[0:64, hh * 128 + 64:(hh + 1) * 128]
            P.do("gpsimd", "memset", ap=sl3, constant=0.0, w=[bdmask])
        for c in range(8):
            sg = stage[c % 2]
            P.dma("sync" if c % 2 == 0 else "gpsimd", sg[:], w_in[c * 128:(c + 1) * 128, :])
            P.do("vector" if c % 2 == 0 else "gpsimd", "tensor_scalar", out=Win[:, c, :], in0=sg[:],
                 scalar1=gpre[:, c:c + 1], scalar2=None, op0=ALU.mult)
        for c in range(8):
            sg = stage[c % 2]
            P.dma("sync" if c % 2 == 0 else "gpsimd", sg[:, 0:1024], w_out[c * 128:(c + 1) * 128, :])
            P.do("vector" if c % 2 == 0 else "gpsimd", "tensor_copy", out=Wout[:, c, :], in_=sg[:, 0:1024])

        xt = [P.sb(st, f"g_xt{i}", [128, 1024], F32) for i in range(2)]
        xo = [P.sb(st, f"g_xo{i}", [128, 1024], F32) for i in range(2)]
        sq = P.sb(st, "g_sq", [128, 1024], F32)
        ss = P.sb(st, "g_ss", [128, 4], F32)
        rstd = P.sb(st, "g_rstd", [128, 2], F32)
        hb = P.sb(st, "g_hb", [128, 1024], BF16)
        hT = P.sb(st, "g_hT", [128, 1024], BF16)
        glrT = P.sb(st, "g_glrT", [16, 128], BF16)
        e1 = P.sb(st, "g_e1", [128, 512], F32)
        yv = P.sb(st, "g_yv", [128, 512], F32)
        Bc = P.sb(st, "g_Bc", [128, 512], F32)
        eb = P.sb(st, "g_eb", [128, 512], F32)
        enb = P.sb(st, "g_enb", [128, 512], F32)
        qeT = P.sb(st, "g_qeT", [128, 512], BF16)
        keT = P.sb(st, "g_keT", [128, 512], BF16)
        kdT = P.sb(st, "g_kdT", [128, 512], BF16)
        kdz = [P.sb(st, f"g_kd{i}", [128, 512], BF16) for i in range(2)]
        for i in range(2):
            P.do("gpsimd", "memset", ap=kdz[i][:], constant=0.0, w=[kdz[i]])
        zs = P.sb(st, "g_zs", [128, 1024], BF16)
        vb = P.sb(st, "g_vb", [128, 1024], BF16)
        ATm = P.sb(st, "g_ATm", [128, 512], BF16)
        oT = P.sb(st, "g_oT", [128, 1024], F32)
        osq = P.sb(st, "g_osq", [128, 1024], BF16)
        rs = P.sb(st, "g_rs", [128, 512], F32)
        tmpo = P.sb(st, "g_tmpo", [128, 1024], F32)
        ogT = P.sb(st, "g_ogT", [128, 1024], BF16)
        S32 = [P.sb(st, f"g_S32_{h}", [128, 256], F32) for h in range(4)]
        Sbf = [[P.sb(st, f"g_Sbf_{h}_{i}", [128, 256], BF16) for i in range(2)] for h in range(4)]
        t1 = P.sb(st, "g_t1", [128, 1024], F32)

        bank = [P.ps(st, f"g_bank{i}", [128, 512], F32) for i in range(7)]
        bS = P.ps(st, "g_bankS", [128, 512], F32)
        pS = [bS[:, i * 256:(i + 1) * 256] for i in range(2)]
        bA, bQ, bK, bZ0, bZ1, bV0, bV1 = bank
        bG = bA
        pTb = bA[:].bitcast(BF16)

        sidx = [0, 0, 0, 0]
        nupd = 0
        P.dma("sync", xt[0][:], x_in[0:128, :])
        for t in range(ntiles):
            x_t = xt[t % 2]
            if t + 1 < ntiles:
                P.dma("sync", xt[(t + 1) % 2][:], x_in[(t + 1) * 128:(t + 2) * 128, :])
            P.do("scalar", "activation", out=sq[:], in_=x_t[:], func=AF.Square, accum_out=ss[:, 0:1])
            P.do("vector", "tensor_scalar", out=rstd[:, 0:1], in0=ss[:, 0:1], scalar1=1.0 / D, scalar2=EPS,
                 op0=ALU.mult, op1=ALU.add)
            P.do("scalar", "activation", out=rstd[:, 0:1], in_=rstd[:, 0:1], func=AF.Sqrt)
            P.do("vector", "reciprocal", out=rstd[:, 0:1], in_=rstd[:, 0:1])
            P.do("vector", "tensor_scalar", out=hb[:], in0=x_t[:], scalar1=rstd[:, 0:1], scalar2=None, op0=ALU.mult)
            for c in range(8):
                P.do("tensor", "transpose", out=pTb[:, c * 128:(c + 1) * 128], in_=hb[:, c * 128:(c + 1) * 128],
                     identity=ident[:])
            P.do("vector", "tensor_copy", out=hT[:], in_=pTb)

            def hTc(c):
                return hT[:, c * 128:(c + 1) * 128]

            for c in range(8):
                P.do("tensor", "matmul", out=bA[0:16, 0:128], lhsT=Win[:, c, 2048:2064], rhs=hTc(c),
                     start=(c == 0), stop=(c == 7))
            P.do("scalar", "copy", out=glrT[:], in_=bA[0:16, 0:128])
            for hh in range(4):
                for c in range(8):
                    P.do("tensor", "matmul", out=bQ[:, hh * 128:(hh + 1) * 128], lhsT=Win[:, c, hh * 128:(hh + 1) * 128],
                         rhs=hTc(c), start=(c == 0), stop=(c == 7))
            for hh in range(4):
                for c in range(8):
                    P.do("tensor", "matmul", out=bK[:, hh * 128:(hh + 1) * 128],
                         lhsT=Win[:, c, 512 + hh * 128:512 + (hh + 1) * 128], rhs=hTc(c), start=(c == 0), stop=(c == 7))
            for hh in range(4):
                P.do("tensor", "matmul", out=bA[:, hh * 128:(hh + 1) * 128], lhsT=wup[:, hh * 128:(hh + 1) * 128],
                     rhs=glrT[:], start=True, stop=True)
            for zc in range(8):
                bz = bZ0 if zc < 4 else bZ1
                for c in range(8):
                    P.do("tensor", "matmul", out=bz[:, (zc % 4) * 128:(zc % 4 + 1) * 128],
                         lhsT=Win[:, c, 2064 + zc * 128:2064 + (zc + 1) * 128], rhs=hTc(c), start=(c == 0), stop=(c == 7))
            for i, bv in enumerate((bV0, bV1)):
                for c in range(8):
                    P.do("tensor", "matmul", out=bv[:], lhsT=hTc(c), rhs=Win[:, c, 1024 + i * 512:1024 + (i + 1) * 512],
                         start=(c == 0), stop=(c == 7))
            for hh in range(4):
                P.do("scalar", "activation", out=e1[:, hh * 128:(hh + 1) * 128], in_=bA[:, hh * 128:(hh + 1) * 128],
                     func=AF.Exp, scale=-1.0, bias=negb[:, hh:hh + 1])
            P.do("scalar", "activation", out=yv[:], in_=e1[:], func=AF.Ln, bias=one1[:, 0:1], scale=1.0)
            P.do("vector", "tensor_tensor_scan", out=Bc[:], data0=rmask[:], data1=yv[:], initial=0.0,
                 op0=ALU.mult, op1=ALU.add)
            P.do("scalar", "activation", out=eb[:], in_=Bc[:], func=AF.Exp, scale=-1.0 / 16.0)
            P.do("scalar", "activation", out=enb[:], in_=Bc[:], func=AF.Exp, scale=1.0 / 16.0)
            P.do("vector", "scalar_tensor_tensor", out=qeT[:], in0=bQ[:], scalar=128 ** -0.5, in1=eb[:],
                 op0=ALU.mult, op1=ALU.mult)
            P.do("vector", "tensor_tensor", out=keT[:], in0=bK[:], in1=enb[:], op=ALU.mult)
            for hh in range(4):
                for cc in range(2):
                    lo = hh * 128 + cc * 64
                    P.do("vector", "scalar_tensor_tensor", out=kdT[:, lo:lo + 64], in0=bK[:, lo:lo + 64],
                         scalar=eb[:, lo + 63:lo + 64], in1=enb[:, lo:lo + 64], op0=ALU.mult, op1=ALU.mult)
            P.do("scalar", "activation", out=zs[:, 0:512], in_=bZ0[:], func=AF.Silu)
            P.do("scalar", "activation", out=zs[:, 512:1024], in_=bZ1[:], func=AF.Silu)
            P.do("gpsimd" if False else "vector", "tensor_copy", out=vb[:, 0:512], in_=bV0[:])
            P.do("scalar", "copy", out=vb[:, 512:1024], in_=bV1[:])
            for hh in range(4):
                P.do("tensor", "transpose", out=pTb[:, hh * 128:(hh + 1) * 128], in_=kdT[:, hh * 128:(hh + 1) * 128],
                     identity=ident[:])
            P.do("vector", "tensor_copy", out=kdz[0][0:64, :], in_=pTb[0:64, 0:512])
            P.do("vector", "tensor_copy", out=kdz[1][64:128, :], in_=pTb[64:128, 0:512])
            for hh in range(4):
                P.do("tensor", "matmul", out=bQ[:, hh * 128:(hh + 1) * 128], lhsT=keT[:, hh * 128:(hh + 1) * 128],
                     rhs=qeT[:, hh * 128:(hh + 1) * 128], start=True, stop=True)
            P.do("vector", "tensor_tensor", out=ATm[:], in0=bQ[:], in1=bdmask[:], op=ALU.mult)
            bO = (bV0, bV1)
            for cc in range(2):
                for hh in range(4):
                    first = (t == 0 and cc == 0)
                    scur = Sbf[hh][sidx[hh]]
                    for vc in range(2):
                        idx = hh * 2 + vc
                        out = bO[idx // 4][:, (idx % 4) * 128 + cc * 64:(idx % 4) * 128 + cc * 64 + 64]
                        P.do("tensor", "matmul", out=out,
                             lhsT=vb[:, hh * 256 + vc * 128:hh * 256 + (vc + 1) * 128],
                             rhs=ATm[:, hh * 128 + cc * 64:hh * 128 + cc * 64 + 64],
                             start=True, stop=first)
                        if not first:
                            P.do("tensor", "matmul", out=out, lhsT=scur[:, vc * 128:(vc + 1) * 128],
                                 rhs=qeT[:, hh * 128 + cc * 64:hh * 128 + cc * 64 + 64], start=False, stop=True)
                    ps = pS[nupd % 2]
                    nupd += 1
                    P.do("tensor", "matmul", out=ps, lhsT=kdz[cc][:, hh * 128:(hh + 1) * 128],
                         rhs=vb[:, hh * 256:(hh + 1) * 256], start=True, stop=True)
                    if first:
                        P.do("vector", "tensor_copy", out=S32[hh][:], in_=ps)
                    else:
                        lo = hh * 128 + cc * 64
                        P.do("vector", "scalar_tensor_tensor", out=S32[hh][:], in0=S32[hh][:],
                             scalar=eb[:, lo + 63:lo + 64], in1=ps, op0=ALU.mult, op1=ALU.add)
                    sidx[hh] ^= 1
                    P.do("gpsimd", "tensor_copy", out=Sbf[hh][sidx[hh]][:], in_=S32[hh][:])
            P.do("scalar", "copy", out=oT[:, 0:512], in_=bV0[:])
            P.do("scalar", "copy", out=oT[:, 512:1024], in_=bV1[:])
            P.do("scalar", "activation", out=osq[:, 0:512], in_=bV0[:], func=AF.Square)
            P.do("scalar", "activation", out=osq[:, 512:1024], in_=bV1[:], func=AF.Square)
            for hh in range(4):
                for vc in range(2):
                    idx = hh * 2 + vc
                    P.do("tensor", "matmul", out=bK[:, hh * 128:(hh + 1) * 128], lhsT=onesb[:],
                         rhs=osq[:, idx * 128:(idx + 1) * 128], start=(vc == 0), stop=(vc == 1))
            P.do("vector", "tensor_scalar", out=rs[:], in0=bK[:], scalar1=1.0 / 256, scalar2=EPS, op0=ALU.mult, op1=ALU.add)
            P.do("scalar", "activation", out=rs[:], in_=rs[:], func=AF.Sqrt)
            P.do("vector", "reciprocal", out=rs[:], in_=rs[:])
            for hh in range(4):
                for vc in range(2):
                    idx = hh * 2 + vc
                    sl = slice(idx * 128, (idx + 1) * 128)
                    P.do("vector", "scalar_tensor_tensor", out=tmpo[:, sl], in0=oT[:, sl], scalar=hn[:, vc:vc + 1],
                         in1=rs[:, hh * 128:(hh + 1) * 128], op0=ALU.mult, op1=ALU.mult)
            P.do("gpsimd", "tensor_tensor", out=ogT[:], in0=tmpo[:], in1=zs[:], op=ALU.mult)
            bY = (bZ0, bZ1)
            for i in range(2):
                for fc in range(8):
                    P.do("tensor", "matmul", out=bY[i][:], lhsT=ogT[:, fc * 128:(fc + 1) * 128],
                         rhs=Wout[:, fc, i * 512:(i + 1) * 512], start=(fc == 0), stop=(fc == 7))
            P.do("scalar", "activation", out=sq[:, 0:512], in_=bZ0[:], func=AF.Square, accum_out=ss[:, 1:2])
            P.do("scalar", "activation", out=sq[:, 512:1024], in_=bZ1[:], func=AF.Square, accum_out=ss[:, 2:3])
            P.do("vector", "tensor_tensor", out=ss[:, 3:4], in0=ss[:, 1:2], in1=ss[:, 2:3], op=ALU.add)
            P.do("vector", "tensor_scalar", out=rstd[:, 1:2], in0=ss[:, 3:4], scalar1=1.0 / D, scalar2=EPS,
                 op0=ALU.mult, op1=ALU.add)
            P.do("scalar", "activation", out=rstd[:, 1:2], in_=rstd[:, 1:2], func=AF.Sqrt)
            P.do("vector", "reciprocal", out=rstd[:, 1:2], in_=rstd[:, 1:2])
            for i in range(2):
                P.do("vector", "scalar_tensor_tensor", out=t1[:, i * 512:(i + 1) * 512], in0=bY[i][:],
                     scalar=rstd[:, 1:2], in1=gpost[:, i * 512:(i + 1) * 512], op0=ALU.mult, op1=ALU.mult)
            x_o = xo[t % 2]
            P.do("gpsimd", "tensor_tensor", out=x_o[:], in0=t1[:], in1=x_t[:], op=ALU.add)
            P.dma("sync", x_out[t * 128:(t + 1) * 128, :], x_o[:])
        P.flush()


S = 4096
D = 1024
NT = S // 128
EPS = 1e-6
NEGM = -30000.0
TWO_PI = 2.0 * math.pi


def _front(P, t, x_in, xt, sq, ss, rstd, hb, hT, pTb, ident, ntl):
    x_t = xt[t % 2]
    if t == 0:
        P.dma("sync", x_t[:], x_in[0:128, :])
    if t + 1 < ntl:
        P.dma("sync", xt[(t + 1) % 2][:], x_in[(t + 1) * 128:(t + 2) * 128, :])
    P.do("scalar", "activation", out=sq[:], in_=x_t[:], func=AF.Square, accum_out=ss[:, 0:1])
    P.do("vector", "tensor_scalar", out=rstd[:, 0:1], in0=ss[:, 0:1], scalar1=1.0 / D, scalar2=EPS,
         op0=ALU.mult, op1=ALU.add)
    P.do("scalar", "activation", out=rstd[:, 0:1], in_=rstd[:, 0:1], func=AF.Sqrt)
    P.do("vector", "reciprocal", out=rstd[:, 0:1], in_=rstd[:, 0:1])
    P.do("vector", "tensor_scalar", out=hb[:], in0=x_t[:], scalar1=rstd[:, 0:1], scalar2=None, op0=ALU.mult)
    for c in range(8):
        P.do("tensor", "transpose", out=pTb[:, c * 128:(c + 1) * 128], in_=hb[:, c * 128:(c + 1) * 128],
             identity=ident[:])
    P.do("vector", "tensor_copy", out=hT[:], in_=pTb)
    return x_t


def _rope(P, src, nh, cosb, sinb, tmp, dst):
    s3 = src.rearrange("p (h d) -> p h d", d=64)
    d3 = dst.rearrange("p (h d) -> p h d", d=64)
    x1, x2 = s3[:, :, 0:32], s3[:, :, 32:64]
    cb = V(cosb.buf, cosb.ap.unsqueeze(1).to_broadcast([128, nh, 32]))
    sb_ = V(sinb.buf, sinb.ap.unsqueeze(1).to_broadcast([128, nh, 32]))
    ta = tmp[0][:, 0:nh * 32].rearrange("p (h d) -> p h d", d=32)
    tb = tmp[1][:, 0:nh * 32].rearrange("p (h d) -> p h d", d=32)
    P.do("vector", "tensor_tensor", out=ta, in0=x1, in1=cb, op=ALU.mult)
    P.do("vector", "tensor_tensor", out=tb, in0=x2, in1=sb_, op=ALU.mult)
    P.do("gpsimd", "tensor_tensor", out=d3[:, :, 0:32], in0=ta, in1=tb, op=ALU.subtract)
    tc_ = tmp[2][:, 0:nh * 32].rearrange("p (h d) -> p h d", d=32)
    td = tmp[3][:, 0:nh * 32].rearrange("p (h d) -> p h d", d=32)
    P.do("vector", "tensor_tensor", out=tc_, in0=x2, in1=cb, op=ALU.mult)
    P.do("vector", "tensor_tensor", out=td, in0=x1, in1=sb_, op=ALU.mult)
    P.do("gpsimd", "tensor_tensor", out=d3[:, :, 32:64], in0=tc_, in1=td, op=ALU.add)


def nsa_layer(P, nc, x_in, x_out, pos, pre_g, post_g, w_in, b_gate, pe_k, pe_v, ck_w1, ck_w2, cv_w1, cv_w2,
              w_out, ntiles=NT):
    nblk = 8 * ntiles - 1
    with ExitStack() as sta:
        ident, identf = make_ident(P, sta, "n")
        kcmpT = P.sb(sta, "n_kcmpT", [64, 4, 256], BF16)
        vcmp = P.sb(sta, "n_vcmp", [128, 2, 4, 129], BF16)
        costab = P.sb(sta, "n_cos", [128, NT, 32], F32)
        sintab = P.sb(sta, "n_sin", [128, NT, 32], F32)
        gpre = P.sb(sta, "n_gpre", [128, 8], F32)
        xt = [P.sb(sta, f"n_xt{i}", [128, 1024], F32) for i in range(2)]
        sq = P.sb(sta, "n_sq", [128, 1024], F32)
        ss = P.sb(sta, "n_ss", [128, 4], F32)
        rstd = P.sb(sta, "n_rstd", [128, 2], F32)
        hb = P.sb(sta, "n_hb", [128, 1024], BF16)
        hT = P.sb(sta, "n_hT", [128, 1024], BF16)
        rtmp = [P.sb(sta, f"n_rtmp{i}", [128, 512], F32) for i in range(4)]
        B = [P.ps(sta, f"n_bank{i}", [128, 512], F32) for i in range(8)]
        pTb = B[0][:].bitcast(BF16)

        P.dma("sync", gpre[:], pre_g.rearrange("(c p o) -> p c o", p=128, o=1), allow_slow_non_contiguous=True)

        with ExitStack() as stc:
            posi = P.sb(stc, "n_posi", [128, NT], I32)
            posf = P.sb(stc, "n_posf", [128, NT], F32)
            invf = P.sb(stc, "n_invf", [128, 32], F32)
            ang = P.sb(stc, "n_ang", [128, NT, 32], F32)
            kf = P.sb(stc, "n_kf", [128, NT, 32], F32)
            ki = P.sb(stc, "n_ki", [128, NT, 32], I32)
            mk = P.sb(stc, "n_mk", [128, NT, 32], F32)
            P.dma("sync", posi[:], pos.rearrange("(t p o) -> p t o", p=128, o=1), allow_slow_non_contiguous=True)
            P.do("vector", "tensor_copy", out=posf[:], in_=posi[:])
            P.do("gpsimd", "iota", out=invf[:], pattern=[[1, 32]], base=0, channel_multiplier=0,
                 allow_small_or_imprecise_dtypes=True)
            P.do("scalar", "activation", out=invf[:], in_=invf[:], func=AF.Exp, scale=-math.log(10000.0) / 32.0)
            for t in range(NT):
                P.do("vector", "tensor_scalar", out=ang[:, t, :], in0=invf[:], scalar1=posf[:, t:t + 1], scalar2=None,
                     op0=ALU.mult)

            def reduce_to_pi(dst, shift):
                P.do("vector", "tensor_scalar", out=kf[:], in0=ang[:], scalar1=shift, scalar2=1.0 / TWO_PI,
                     op0=ALU.add, op1=ALU.mult)
                P.do("vector", "tensor_copy", out=ki[:], in_=kf[:])
                P.do("vector", "tensor_copy", out=kf[:], in_=ki[:])
                P.do("vector", "scalar_tensor_tensor", out=kf[:], in0=kf[:], scalar=-TWO_PI, in1=ang[:],
                     op0=ALU.mult, op1=ALU.add)
                P.do("vector", "tensor_scalar", out=kf[:], in0=kf[:], scalar1=shift, scalar2=None, op0=ALU.add)
                P.do("vector", "tensor_scalar", out=mk[:], in0=kf[:], scalar1=math.pi, scalar2=-TWO_PI,
                     op0=ALU.is_gt, op1=ALU.mult)
                P.do("vector", "tensor_tensor", out=kf[:], in0=kf[:], in1=mk[:], op=ALU.add)
                P.do("vector", "tensor_scalar", out=mk[:], in0=kf[:], scalar1=-math.pi, scalar2=TWO_PI,
                     op0=ALU.is_lt, op1=ALU.mult)
                P.do("vector", "tensor_tensor", out=kf[:], in0=kf[:], in1=mk[:], op=ALU.add)
                P.do("vector", "tensor_scalar", out=kf[:], in0=kf[:], scalar1=math.pi, scalar2=-math.pi,
                     op0=ALU.min, op1=ALU.max)
                P.do("scalar", "activation", out=dst[:], in_=kf[:], func=AF.Sin)

            reduce_to_pi(sintab, 0.0)
            reduce_to_pi(costab, math.pi / 2.0)
            P.flush()

        with ExitStack() as st0:
            WinA = P.sb(st0, "n0_WinA", [128, 8, 512], BF16)
            stg = [P.sb(st0, f"n0_stg{i}", [128, 2048], F32) for i in range(2)]
            w1 = [P.sb(st0, f"n0_w1_{i}", [64, 32, 256], BF16) for i in range(2)]
            w2 = [P.sb(st0, f"n0_w2_{i}", [128, 2, 64], BF16) for i in range(2)]
            w2f = P.sb(st0, "n0_w2f", [128, 2, 64], F32)
            peTf = P.sb(st0, "n0_peTf", [64, 32], F32)
            peT = [P.sb(st0, f"n0_peT{i}", [64, 32], BF16) for i in range(2)]
            bias = P.sb(st0, "n0_bias", [128, 4], F32)
            kcT = P.sb(st0, "n0_kcT", [64, 4, S], BF16)
            vcT = P.sb(st0, "n0_vcT", [64, 4, S], BF16)
            kcr = P.sb(st0, "n0_kcr", [128, 256], BF16)
            vcb = P.sb(st0, "n0_vcb", [128, 256], BF16)
            hidT = P.sb(st0, "n0_hidT", [128, 2, 256], BF16)
            ovf = P.sb(st0, "n0_ovf", [128, 2, 64], F32)

            for c in range(8):
                sg = stg[c % 2]
                P.dma("sync" if c % 2 == 0 else "gpsimd", sg[:, 0:512], w_in[c * 128:(c + 1) * 128, 1024:1536])
                P.do("vector" if c % 2 == 0 else "gpsimd", "tensor_scalar", out=WinA[:, c, :], in0=sg[:, 0:512],
                     scalar1=gpre[:, c:c + 1], scalar2=None, op0=ALU.mult)
            n = 0
            for kv, (wsrc, w2src, pesrc) in enumerate(((ck_w1, ck_w2, pe_k), (cv_w1, cv_w2, pe_v))):
                w1v = wsrc.rearrange("(l d) n -> d l n", d=64)
                for q4 in range(4):
                    sg = stg[n % 2]
                    P.dma("sync" if n % 2 == 0 else "gpsimd", sg[0:64, :].rearrange("p (l n) -> p l n", n=256),
                          w1v[:, q4 * 8:(q4 + 1) * 8, :])
                    P.do("vector" if n % 2 == 0 else "gpsimd", "tensor_copy",
                         out=w1[kv][:, q4 * 8:(q4 + 1) * 8, :], in_=sg[0:64, :].rearrange("p (l n) -> p l n", n=256))
                    n += 1
                P.dma("sync", w2f[:], w2src.rearrange("(c p) n -> p c n", p=128))
                P.do("vector", "tensor_copy", out=w2[kv][:], in_=w2f[:])
                P.dma("sync", peTf[:], pesrc.rearrange("l d -> d l"), allow_slow_non_contiguous=True)
                P.do("vector", "tensor_copy", out=peT[kv][:], in_=peTf[:])
                for hc in range(2):
                    for l in range(32):
                        P.do("tensor", "matmul", out=B[1][:, kv * 2 + hc:kv * 2 + hc + 1],
                             lhsT=w1[kv][:, l, hc * 128:(hc + 1) * 128], rhs=peT[kv][:, l:l + 1],
                             start=(l == 0), stop=(l == 31))
            P.do("vector", "tensor_copy", out=bias[:], in_=B[1][:, 0:4])

            P.do("gpsimd", "memset", ap=vcmp[:], constant=0.0, w=[vcmp])
            P.do("gpsimd", "memset", ap=vcmp[:, :, :, 64:65], constant=1.0, w=[vcmp])
            P.do("gpsimd", "memset", ap=ovf[:], constant=1.0, w=[ovf])
            for ch in range(2):
                P.do("gpsimd", "affine_select", out=ovf[:, ch, :], in_=ovf[:, ch, :], pattern=[[-4, 64]],
                     compare_op=ALU.is_ge, fill=0.0, base=ch * 128 + 1, channel_multiplier=1)
                P.do("gpsimd", "affine_select", out=ovf[:, ch, :], in_=ovf[:, ch, :], pattern=[[4, 64]],
                     compare_op=ALU.is_ge, fill=0.0, base=3 - ch * 128, channel_multiplier=-1)
                for g in range(4):
                    P.do("vector", "tensor_copy", out=vcmp[:, ch, g, 65:129], in_=ovf[:, ch, :])

            for t in range(ntiles):
                _front(P, t, x_in, xt, sq, ss, rstd, hb, hT, pTb, ident, ntiles)
                for c in range(8):
                    P.do("tensor", "matmul", out=B[1][:], lhsT=hT[:, c * 128:(c + 1) * 128], rhs=WinA[:, c, :],
                         start=(c == 0), stop=(c == 7))
                _rope(P, B[1][:, 0:256], 4, costab[:, t, :], sintab[:, t, :], rtmp, kcr[:])
                P.do("scalar", "copy", out=vcb[:], in_=B[1][:, 256:512])
                for g in range(4):
                    P.do("tensor", "transpose", out=pTb[0:64, g * 128:(g + 1) * 128], in_=kcr[:, g * 64:(g + 1) * 64],
                         identity=ident[:])
                for g in range(4):
                    P.do("tensor", "transpose", out=pTb[0:64, 512 + g * 128:512 + (g + 1) * 128],
                         in_=vcb[:, g * 64:(g + 1) * 64], identity=ident[:])
                P.do("vector", "tensor_copy", out=kcT[:, :, t * 128:(t + 1) * 128],
                     in_=pTb[0:64, 0:512].rearrange("p (g t) -> p g t", g=4))
                P.do("scalar", "copy", out=vcT[:, :, t * 128:(t + 1) * 128],
                     in_=pTb[0:64, 512:1024].rearrange("p (g t) -> p g t", g=4))

            for kv, srcT in enumerate((kcT, vcT)):
                for g in range(4):
                    for hc in range(2):
                        bk = B[2 + hc]
                        for l in range(32):
                            P.do("tensor", "matmul", out=bk[:, 0:nblk], lhsT=w1[kv][:, l, hc * 128:(hc + 1) * 128],
                                 rhs=srcT[:, g, l:l + 16 * (nblk - 1) + 1:16], start=(l == 0), stop=(l == 31))
                        P.do("scalar", "activation", out=hidT[:, hc, 0:nblk], in_=bk[:, 0:nblk], func=AF.Silu,
                             bias=bias[:, kv * 2 + hc:kv * 2 + hc + 1], scale=1.0)
                    if kv == 0:
                        for hc in range(2):
                            P.do("tensor", "matmul", out=B[4][0:64, 0:nblk], lhsT=w2[0][:, hc, :],
                                 rhs=hidT[:, hc, 0:nblk], start=(hc == 0), stop=(hc == 1))
                        P.do("vector", "tensor_copy", out=kcmpT[:, g, 0:nblk], in_=B[4][0:64, 0:nblk])
                    else:
                        for ch in range(2):
                            rows = min(128, nblk - ch * 128)
                            if rows <= 0:
                                continue
                            for hc in range(2):
                                P.do("tensor", "matmul", out=B[4][0:rows, ch * 64:(ch + 1) * 64],
                                     lhsT=hidT[:, hc, ch * 128:ch * 128 + rows], rhs=w2[1][:, hc, :],
                                     start=(hc == 0), stop=(hc == 1))
                            P.do("vector", "tensor_copy", out=vcmp[0:rows, ch, g, 0:64],
                                 in_=B[4][0:rows, ch * 64:(ch + 1) * 64])
            P.flush()

        with ExitStack() as st1:
            Win = P.sb(st1, "n1_Win", [128, 8, 3632], BF16)
            Wout = P.sb(st1, "n1_Wout", [128, 8, 1024], BF16)
            gpost = P.sb(st1, "n1_gpost", [128, 1024], F32)
            bgate = P.sb(st1, "n1_bgate", [128, 48], F32)
            with ExitStack() as stl:
                stage = [P.sb(stl, f"n1_stage{i}", [128, 3632], F32) for i in range(2)]
                for c in range(8):
                    sg = stage[c % 2]
                    P.dma("sync" if c % 2 == 0 else "gpsimd", sg[:], w_in[c * 128:(c + 1) * 128, :])
                    P.do("vector" if c % 2 == 0 else "gpsimd", "tensor_scalar", out=Win[:, c, :], in0=sg[:],
                         scalar1=gpre[:, c:c + 1], scalar2=None, op0=ALU.mult)
                for c in range(8):
                    sg = stage[c % 2]
                    P.dma("sync" if c % 2 == 0 else "gpsimd", sg[:, 0:1024], w_out[c * 128:(c + 1) * 128, :])
                    P.do("vector" if c % 2 == 0 else "gpsimd", "tensor_copy", out=Wout[:, c, :], in_=sg[:, 0:1024])
                P.dma("sync", gpost[:], post_g.rearrange("(o n) -> o n", o=1).to_broadcast([128, 1024]))
                P.dma("sync", bgate[:], b_gate.rearrange("(o n) -> o n", o=1).to_broadcast([128, 48]))
                P.flush()

            ksT = P.sb(st1, "n1_ksT", [128, 4, S], BF16)
            vsa = P.sb(st1, "n1_vsa", [128, NT, 4, 65], BF16)
            kwT = P.sb(st1, "n1_kwT", [64, 4, 5 * 128], BF16)
            vwa = P.sb(st1, "n1_vwa", [128, 5, 4, 65], BF16)
            qaug = P.sb(st1, "n1_qaug", [128, 16, 128], BF16)
            qmask = P.track(qaug.h[64:128, :, :], "n1_qmask")
            qr = P.sb(st1, "n1_qr", [128, 1024], BF16)
            ksr = P.sb(st1, "n1_ksr", [128, 256], BF16)
            kwr = P.sb(st1, "n1_kwr", [128, 256], BF16)
            zs = P.sb(st1, "n1_zs", [128, 1024], BF16)
            glx = P.sb(st1, "n1_glx", [128, 48], F32)
            gt = P.sb(st1, "n1_gt", [128, 48], F32)
            PT = [P.sb(st1, f"n1_PT{i}", [128, 512], BF16) for i in range(3)]
            lrec = P.sb(st1, "n1_lrec", [128, 4], F32)
            wsc = P.sb(st1, "n1_wsc", [128, 4], F32)
            imp = P.sb(st1, "n1_imp", [128, 64], F32)
            imp2 = P.sb(st1, "n1_imp2", [128, 64], F32)
            imp3 = P.sb(st1, "n1_imp3", [128, 64], F32)
            m8 = P.sb(st1, "n1_m8", [128, 16], F32)
            M1 = P.sb(st1, "n1_M1", [128, 64], F32)
            Cm = P.sb(st1, "n1_Cm", [128, 64], F32)
            selm = P.sb(st1, "n1_selm", [128, 128], BF16)
            oacc = P.sb(st1, "n1_oacc", [128, 1024], F32)
            otmp = P.sb(st1, "n1_otmp", [128, 256], F32)
            og = P.sb(st1, "n1_og", [128, 1024], BF16)
            ogT = P.sb(st1, "n1_ogT", [128, 1024], BF16)
            t1 = P.sb(st1, "n1_t1", [128, 1024], F32)
            xo = [P.sb(st1, f"n1_xo{i}", [128, 1024], F32) for i in range(2)]

            P.do("gpsimd", "memset", ap=ksT[64:128, :, :], constant=1.0, w=[ksT])
            for g in range(4):
                P.do("gpsimd", "affine_select", out=ksT[64:128, g, :], in_=ksT[64:128, g, :], pattern=[[1, S]],
                     compare_op=ALU.is_ge, fill=0.0, base=0, channel_multiplier=-64)
                P.do("gpsimd", "affine_select", out=ksT[64:128, g, :], in_=ksT[64:128, g, :], pattern=[[-1, S]],
                     compare_op=ALU.is_ge, fill=0.0, base=63, channel_multiplier=64)
            P.do("gpsimd", "memset", ap=vsa[:, :, :, 64:65], constant=1.0, w=[vsa])
            P.do("gpsimd", "memset", ap=vwa[:, :, :, 64:65], constant=1.0, w=[vwa])
            P.do("gpsimd", "memset", ap=selm[:], constant=0.0, w=[selm])

            npt = 0
            nsb = 0
            for T in range(ntiles):
                x_t = _front(P, T, x_in, xt, sq, ss, rstd, hb, hT, pTb, ident, ntiles)

                def hTc(c):
                    return hT[:, c * 128:(c + 1) * 128]

                for bk, lo, wd in ((B[1], 0, 512), (B[2], 512, 512), (B[3], 1536, 512), (B[4], 2048, 512), (B[5], 2560, 48)):
                    for c in range(8):
                        P.do("tensor", "matmul", out=bk[:, 0:wd], lhsT=hTc(c), rhs=Win[:, c, lo:lo + wd],
                             start=(c == 0), stop=(c == 7))
                cb, sb_ = costab[:, T, :], sintab[:, T, :]
                _rope(P, B[1][:], 8, cb, sb_, rtmp, qr[:, 0:512])
                _rope(P, B[2][:], 8, cb, sb_, rtmp, qr[:, 512:1024])
                _rope(P, B[3][:, 0:256], 4, cb, sb_, rtmp, ksr[:])
                _rope(P, B[4][:, 0:256], 4, cb, sb_, rtmp, kwr[:])
                P.do("scalar", "copy", out=vsa[:, T, :, 0:64], in_=B[3][:, 256:512].rearrange("p (g d) -> p g d", d=64))
                P.do("scalar", "copy", out=vwa[:, T % 5, :, 0:64], in_=B[4][:, 256:512].rearrange("p (g d) -> p g d", d=64))
                P.do("vector", "tensor_tensor", out=glx[:], in0=B[5][:, 0:48], in1=bgate[:], op=ALU.add)
                P.do("scalar", "activation", out=glx[:], in_=glx[:], func=AF.Exp, scale=-1.0)
                P.do("vector", "tensor_scalar", out=glx[:], in0=glx[:], scalar1=1.0, scalar2=None, op0=ALU.add)
                P.do("vector", "reciprocal", out=gt[:], in_=glx[:])
                for i, bk in enumerate((B[1], B[2])):
                    for c in range(8):
                        P.do("tensor", "matmul", out=bk[:], lhsT=hTc(c), rhs=Win[:, c, 2608 + i * 512:2608 + (i + 1) * 512],
                             start=(c == 0), stop=(c == 7))
                for half in range(2):
                    for hh in range(8):
                        h = half * 8 + hh
                        P.do("tensor", "transpose", out=pTb[0:64, hh * 128:(hh + 1) * 128],
                             in_=qr[:, h * 64:(h + 1) * 64], identity=ident[:])
                    P.do("scalar", "mul", out=qaug[0:64, half * 8:(half + 1) * 8, :],
                         in_=pTb[0:64, :].rearrange("p (h t) -> p h t", h=8), mul=0.125)
                for g in range(4):
                    P.do("tensor", "transpose", out=pTb[0:64, g * 128:(g + 1) * 128], in_=ksr[:, g * 64:(g + 1) * 64],
                         identity=ident[:])
                for g in range(4):
                    P.do("tensor", "transpose", out=pTb[0:64, 512 + g * 128:512 + (g + 1) * 128],
                         in_=kwr[:, g * 64:(g + 1) * 64], identity=ident[:])
                P.do("vector", "tensor_copy", out=ksT[0:64, :, T * 128:(T + 1) * 128],
                     in_=pTb[0:64, 0:512].rearrange("p (g t) -> p g t", g=4))
                P.do("vector", "tensor_copy", out=kwT[:, :, (T % 5) * 128:(T % 5 + 1) * 128],
                     in_=pTb[0:64, 512:1024].rearrange("p (g t) -> p g t", g=4))
                P.do("scalar", "activation", out=zs[:, 0:512], in_=B[1][:], func=AF.Silu)
                P.do("scalar", "activation", out=zs[:, 512:1024], in_=B[2][:], func=AF.Silu)

                P.do("gpsimd", "memset", ap=M1[:], constant=0.0, w=[M1])
                P.do("gpsimd", "memset", ap=Cm[:], constant=0.0, w=[Cm])
                for half in range(2):
                    cur = 2 * T + half
                    rs_ = slice(half * 64, (half + 1) * 64)
                    if cur - 2 >= 1:
                        P.do("gpsimd", "memset", ap=M1[rs_, 1:cur - 1], constant=1.0, w=[M1])
                    if cur + 1 < 64:
                        P.do("gpsimd", "memset", ap=Cm[rs_, cur + 1:64], constant=-1e30, w=[Cm])
                    P.do("gpsimd", "memset", ap=Cm[rs_, max(cur - 1, 0):cur + 1], constant=1e9, w=[Cm])
                P.do("gpsimd", "memset", ap=Cm[:, 0:1], constant=1e9, w=[Cm])

                nvalid = min(8 * T + 7, nblk)
                for g in range(4):
                    qg = qaug[0:64, 4 * g:4 * g + 4, :]
                    qga = qaug[:, 4 * g:4 * g + 4, :]
                    nch = (nvalid + 127) // 128
                    for ch in range(nch):
                        rows = min(128, nvalid - ch * 128)
                        sbk = B[3 + nsb % 2]; nsb += 1
                        pt = PT[npt % 3]; npt += 1
                        P.do("tensor", "matmul", out=sbk[0:rows, :], lhsT=kcmpT[:, g, ch * 128:ch * 128 + rows], rhs=qg,
                             start=True, stop=True)
                        P.do("scalar", "activation", out=pt[0:rows, :], in_=sbk[0:rows, :], func=AF.Exp)
                        P.do("gpsimd", "affine_select", out=pt[0:rows, :], in_=pt[0:rows, :], pattern=[[0, 4], [1, 128]],
                             compare_op=ALU.is_ge, fill=0.0, base=128 * T - 2048 * ch - 31, channel_multiplier=-16)
                        for h in range(4):
                            P.do("tensor", "matmul", out=B[1 + h // 2][:, (h % 2) * 129:(h % 2) * 129 + 129],
                                 lhsT=pt[0:rows, h * 128:(h + 1) * 128], rhs=vcmp[0:rows, ch, g, :],
                                 start=(ch == 0 and h % 2 == 0), stop=(ch == nch - 1 and h % 2 == 1))
                    for b2 in range(2):
                        P.do("vector", "tensor_scalar", out=lrec[:, 2 * b2:2 * b2 + 2], in0=B[1 + b2][:, 64:258:129],
                             scalar1=1e-20, scalar2=None, op0=ALU.max)
                    P.do("vector", "reciprocal", out=lrec[:], in_=lrec[:])
                    P.do("vector", "tensor_tensor", out=wsc[:], in0=lrec[:],
                         in1=gt[:].rearrange("p (h c) -> p h c", c=3)[:, 4 * g:4 * g + 4, 0], op=ALU.mult)
                    for h in range(4):
                        bo = B[1 + h // 2]
                        off = (h % 2) * 129
                        H = 4 * g + h
                        if h % 2 == 0:
                            P.do("vector", "tensor_tensor",
                                 out=oacc[:, H * 64:(H + 2) * 64].rearrange("p (h d) -> p h d", d=64),
                                 in0=bo[:, 0:258].rearrange("p (h d) -> p h d", d=129)[:, :, 0:64],
                                 in1=V(wsc, wsc[:, h:h + 2].ap.unsqueeze(2).to_broadcast([128, 2, 64])), op=ALU.mult)
                        if h == 0:
                            P.do("vector", "tensor_scalar", out=imp[:], in0=bo[:, off + 65:off + 129],
                                 scalar1=lrec[:, h:h + 1], scalar2=None, op0=ALU.mult)
                        else:
                            P.do("vector", "scalar_tensor_tensor", out=imp[:], in0=bo[:, off + 65:off + 129],
                                 scalar=lrec[:, h:h + 1], in1=imp[:], op0=ALU.mult, op1=ALU.add)
                    P.do("vector", "tensor_tensor", out=imp2[:], in0=imp[:], in1=M1[:], op=ALU.mult)
                    P.do("vector", "tensor_tensor", out=imp2[:], in0=imp2[:], in1=Cm[:], op=ALU.add)
                    P.do("vector", "max", out=m8[:, 0:8], in_=imp2[:])
                    P.do("vector", "match_replace", out=imp3[:], in_to_replace=m8[:, 0:8], in_values=imp2[:],
                         imm_value=-3e38)
                    P.do("vector", "max", out=m8[:, 8:16], in_=imp3[:])
                    P.do("vector", "tensor_scalar", out=selm[:, 64:128], in0=imp2[:], scalar1=m8[:, 15:16], scalar2=NEGM,
                         op0=ALU.is_lt, op1=ALU.mult)
                    P.do("tensor", "transpose", out=pTb[:, 0:128], in_=selm[:], identity=ident[:])
                    P.do("vector", "tensor_copy", out=qmask[:, 4 * g:4 * g + 4, :],
                         in_=V(B[0], pTb.ap[64:128, 0:128].unsqueeze(1).to_broadcast([64, 4, 128])))
                    kts = list(range(max(0, T - 4), T + 1))
                    for kt in kts:
                        sbk = B[3 + nsb % 2]; nsb += 1
                        pt = PT[npt % 3]; npt += 1
                        sl = kt % 5
                        P.do("tensor", "matmul", out=sbk[:], lhsT=kwT[:, g, sl * 128:(sl + 1) * 128], rhs=qg,
                             start=True, stop=True)
                        P.do("scalar", "activation", out=pt[:], in_=sbk[:], func=AF.Exp)
                        if kt == T:
                            P.do("gpsimd", "affine_select", out=pt[:], in_=pt[:], pattern=[[0, 4], [1, 128]],
                                 compare_op=ALU.is_ge, fill=0.0, base=0, channel_multiplier=-1)
                        if kt == T - 4:
                            P.do("gpsimd", "affine_select", out=pt[:], in_=pt[:], pattern=[[0, 4], [-1, 128]],
                                 compare_op=ALU.is_ge, fill=0.0, base=-1, channel_multiplier=1)
                        for h in range(4):
                            P.do("tensor", "matmul", out=B[6][:, h * 65:h * 65 + 65], lhsT=pt[:, h * 128:(h + 1) * 128],
                                 rhs=vwa[:, sl, g, :], start=(kt == kts[0] and h == 0), stop=(kt == T and h == 3))
                    _combine(P, B[6], g, 2, lrec, wsc, gt, oacc, otmp)
                for g in range(4):
                    qga = qaug[:, 4 * g:4 * g + 4, :]
                    bsel = B[5] if g % 2 == 0 else B[7]
                    for kt in range(T + 1):
                        sbk = B[3 + nsb % 2]; nsb += 1
                        pt = PT[npt % 3]; npt += 1
                        P.do("tensor", "matmul", out=sbk[:], lhsT=ksT[:, g, kt * 128:(kt + 1) * 128], rhs=qga,
                             start=True, stop=True, r=[qmask])
                        P.do("scalar", "activation", out=pt[:], in_=sbk[:], func=AF.Exp)
                        if kt == T:
                            P.do("gpsimd", "affine_select", out=pt[:], in_=pt[:], pattern=[[0, 4], [1, 128]],
                                 compare_op=ALU.is_ge, fill=0.0, base=0, channel_multiplier=-1)
                        for h in range(4):
                            P.do("tensor", "matmul", out=bsel[:, h * 65:h * 65 + 65], lhsT=pt[:, h * 128:(h + 1) * 128],
                                 rhs=vsa[:, kt, g, :], start=(kt == 0 and h == 0), stop=(kt == T and h == 3))
                    _combine(P, bsel, g, 1, lrec, wsc, gt, oacc, otmp)

                P.do("gpsimd", "tensor_tensor", out=og[:], in0=oacc[:], in1=zs[:], op=ALU.mult)
                for c in range(8):
                    P.do("tensor", "transpose", out=pTb[:, c * 128:(c + 1) * 128], in_=og[:, c * 128:(c + 1) * 128],
                         identity=ident[:])
                P.do("vector", "tensor_copy", out=ogT[:], in_=pTb)
                for i in range(2):
                    for fc in range(8):
                        P.do("tensor", "matmul", out=B[1 + i][:], lhsT=ogT[:, fc * 128:(fc + 1) * 128],
                             rhs=Wout[:, fc, i * 512:(i + 1) * 512], start=(fc == 0), stop=(fc == 7))
                P.do("scalar", "activation", out=sq[:, 0:512], in_=B[1][:], func=AF.Square, accum_out=ss[:, 1:2])
                P.do("scalar", "activation", out=sq[:, 512:1024], in_=B[2][:], func=AF.Square, accum_out=ss[:, 2:3])
                P.do("vector", "tensor_tensor", out=ss[:, 3:4], in0=ss[:, 1:2], in1=ss[:, 2:3], op=ALU.add)
                P.do("vector", "tensor_scalar", out=rstd[:, 1:2], in0=ss[:, 3:4], scalar1=1.0 / D, scalar2=EPS,
                     op0=ALU.mult, op1=ALU.add)
                P.do("scalar", "activation", out=rstd[:, 1:2], in_=rstd[:, 1:2], func=AF.Sqrt)
                P.do("vector", "reciprocal", out=rstd[:, 1:2], in_=rstd[:, 1:2])
                for i in range(2):
                    P.do("vector", "scalar_tensor_tensor", out=t1[:, i * 512:(i + 1) * 512], in0=B[1 + i][:],
                         scalar=rstd[:, 1:2], in1=gpost[:, i * 512:(i + 1) * 512], op0=ALU.mult, op1=ALU.mult)
                x_o = xo[T % 2]
                P.do("gpsimd", "tensor_tensor", out=x_o[:], in0=t1[:], in1=x_t[:], op=ALU.add)
                P.dma("sync", x_out[T * 128:(T + 1) * 128, :], x_o[:])
            P.flush()


def _combine(P, bo, g, c, lrec, wsc, gt, oacc, otmp):
    P.do("vector", "tensor_scalar", out=lrec[:], in0=bo[:, 64:260:65], scalar1=1e-20, scalar2=None, op0=ALU.max)
    P.do("vector", "reciprocal", out=lrec[:], in_=lrec[:])
    P.do("vector", "tensor_tensor", out=wsc[:], in0=lrec[:],
         in1=gt[:].rearrange("p (h c) -> p h c", c=3)[:, 4 * g:4 * g + 4, c], op=ALU.mult)
    bo3 = bo[:, 0:260].rearrange("p (h d) -> p h d", d=65)[:, :, 0:64]
    wbc = V(wsc, wsc[:].ap.unsqueeze(2).to_broadcast([128, 4, 64]))
    P.do("vector", "tensor_tensor", out=otmp[:].rearrange("p (h d) -> p h d", d=64), in0=bo3, in1=wbc, op=ALU.mult)
    P.do("gpsimd", "tensor_tensor", out=oacc[:, g * 256:(g + 1) * 256], in0=oacc[:, g * 256:(g + 1) * 256],
         in1=otmp[:], op=ALU.add)


FUSED = True


def _din(nc, name, shape, dt=F32):
    return nc.dram_tensor(name, list(shape), dt, kind="ExternalInput").ap()


def _gla_inputs(nc):
    return dict(pre=_din(nc, "g_pre", [1024]), post=_din(nc, "g_post", [1024]), w_in=_din(nc, "g_w_in", [1024, 3088]),
                w_up=_din(nc, "g_w_up", [16, 512]), b_gk=_din(nc, "g_b_gk", [512]), hnorm=_din(nc, "g_hnorm", [256]),
                w_out=_din(nc, "g_w_out", [1024, 1024]))


def _nsa_inputs(nc):
    return dict(pos=_din(nc, "n_pos", [4096], I32), pre=_din(nc, "n_pre", [1024]), post=_din(nc, "n_post", [1024]),
                w_in=_din(nc, "n_w_in", [1024, 3632]), b_gate=_din(nc, "n_b_gate", [48]),
                pe_k=_din(nc, "n_pe_k", [32, 64]), pe_v=_din(nc, "n_pe_v", [32, 64]),
                ck_w1=_din(nc, "n_ck_w1", [2048, 256]), ck_w2=_din(nc, "n_ck_w2", [256, 64]),
                cv_w1=_din(nc, "n_cv_w1", [2048, 256]), cv_w2=_din(nc, "n_cv_w2", [256, 64]),
                w_out=_din(nc, "n_w_out", [1024, 1024]))


def _emit_gla(P, nc, x, y, gi):
    gla_layer(P, nc, x, y, gi["pre"], gi["post"], gi["w_in"], gi["w_up"], gi["b_gk"], gi["hnorm"], gi["w_out"])


def _emit_nsa(P, nc, x, y, ni):
    nsa_layer(P, nc, x, y, ni["pos"], ni["pre"], ni["post"], ni["w_in"], ni["b_gate"], ni["pe_k"], ni["pe_v"],
              ni["ck_w1"], ni["ck_w2"], ni["cv_w1"], ni["cv_w2"], ni["w_out"])


def _gla_map(inp, b):
    return {"g_pre": inp["pre_norm"][0], "g_post": inp["post_norm"][0], "g_w_in": inp["gla_w_in"][0],
            "g_w_up": inp["gla_w_gk_up"][0], "g_b_gk": inp["gla_b_gk"][0], "g_hnorm": inp["gla_head_norm"][0],
            "g_w_out": inp["gla_w_out"][0]}


def _nsa_map(inp, b):
    return {"n_pos": np.ascontiguousarray(inp["positions"][b]), "n_pre": inp["pre_norm"][1], "n_post": inp["post_norm"][1],
            "n_w_in": inp["nsa_w_in"][0], "n_b_gate": inp["nsa_b_gate"][0], "n_pe_k": inp["nsa_pe_k"][0],
            "n_pe_v": inp["nsa_pe_v"][0], "n_ck_w1": inp["nsa_ck_w1"][0], "n_ck_w2": inp["nsa_ck_w2"][0],
            "n_cv_w1": inp["nsa_cv_w1"][0], "n_cv_w2": inp["nsa_cv_w2"][0], "n_w_out": inp["nsa_w_out"][0]}


def kernel(**inputs):
    inp = {k: np.ascontiguousarray(np.asarray(v)) for k, v in inputs.items()}
    n = 8
    cores = list(range(n))
    xs = [np.ascontiguousarray(inp["x"][b]) for b in range(n)]
    if FUSED:
        nc = bass.Bass("TRN2", target_bir_lowering=False)
        x = _din(nc, "x", [4096, 1024])
        y = nc.dram_tensor("y", [4096, 1024], F32, kind="ExternalOutput").ap()
        x1 = nc.dram_tensor("x1_stage", [4096, 1024], F32, kind="ExternalOutput").ap()
        gi = _gla_inputs(nc)
        ni = _nsa_inputs(nc)
        P = Prog(nc)
        _emit_gla(P, nc, x, x1, gi)
        _emit_nsa(P, nc, x1, y, ni)
        maps = [dict(x=xs[b], **_gla_map(inp, b), **_nsa_map(inp, b)) for b in range(n)]
        res = run_bass_kernel_spmd(nc, maps, core_ids=cores)
        return np.stack([np.asarray(res.results[b]["y"]) for b in range(n)], axis=0).astype(np.float32)
    nc1 = bass.Bass("TRN2", target_bir_lowering=False)
    x = _din(nc1, "x", [4096, 1024])
    y = nc1.dram_tensor("y", [4096, 1024], F32, kind="ExternalOutput").ap()
    gi = _gla_inputs(nc1)
    _emit_gla(Prog(nc1), nc1, x, y, gi)
    res1 = run_bass_kernel_spmd(nc1, [dict(x=xs[b], **_gla_map(inp, b)) for b in range(n)], core_ids=cores)
    x1s = [np.ascontiguousarray(np.asarray(res1.results[b]["y"])) for b in range(n)]
    nc2 = bass.Bass("TRN2", target_bir_lowering=False)
    x = _din(nc2, "x", [4096, 1024])
    y = nc2.dram_tensor("y", [4096, 1024], F32, kind="ExternalOutput").ap()
    ni = _nsa_inputs(nc2)
    _emit_nsa(Prog(nc2), nc2, x, y, ni)
    res2 = run_bass_kernel_spmd(nc2, [dict(x=x1s[b], **_nsa_map(inp, b)) for b in range(n)], core_ids=cores)
    return np.stack([np.asarray(res2.results[b]["y"]) for b in range(n)], axis=0).astype(np.float32)
```
